# Optimizing a Trainium2 kernel written in Bass

```python
import math
import jax
import jax.numpy as jnp
from jax import lax
import numpy as np

D_MODEL = 1024
BATCH = 8
SEQ = 8192
DEPTH = 2

DA_HEAD_DIM = 64
DA_V_DIM = 2 * DA_HEAD_DIM
DA_WIDTH = D_MODEL // 2
DA_HEADS = DA_WIDTH // DA_V_DIM
POOL_WIDTH = D_MODEL // 4
POOL_WINDOWS = (2, 4, 8, 16)
POOL_GROUPS = len(POOL_WINDOWS)
POOL_GROUP_DIM = POOL_WIDTH // POOL_GROUPS
RET_WIDTH = D_MODEL // 4
RET_HEAD_DIM = 64
RET_HEADS = RET_WIDTH // RET_HEAD_DIM
RET_CHUNK = 128
MIX_WIDTH = DA_WIDTH + POOL_WIDTH + RET_WIDTH
IN_SECTIONS = (DA_WIDTH, DA_WIDTH, DA_WIDTH, POOL_WIDTH, RET_WIDTH, RET_WIDTH, RET_WIDTH, RET_WIDTH)
IN_WIDTH = sum(IN_SECTIONS)
IN_SPLITS = tuple(int(s) for s in np.cumsum(IN_SECTIONS)[:-1])
BLOCK_Q = 128
REL_BUCKETS = 32
REL_MAX_DIST = 128
FFN_DIM = 2816
N_EXPERTS = 8
TOP_K = 2
EXPERT_DIM = 3584
MOE_BLOCK = 512
ALPHA = (2 * DEPTH) ** 0.25
BETA = (8 * DEPTH) ** -0.25
LN_EPS = 1e-5
NORM_EPS = 1e-6

kernel_name = 'hymba_style_diffattn_pool_retention_moe'


def layer_norm(x, g, b):
    xf = x.astype(jnp.float32)
    mu = jnp.mean(xf, axis=-1, keepdims=True)
    var = jnp.mean(jnp.square(xf - mu), axis=-1, keepdims=True)
    return ((xf - mu) * lax.rsqrt(var + LN_EPS) * g + b).astype(x.dtype)


def t5_bucket(dist):
    n = jnp.maximum(dist, 0)
    max_exact = REL_BUCKETS // 2
    nf = jnp.maximum(n, 1).astype(jnp.float32)
    large = max_exact + (jnp.log(nf / max_exact) / math.log(REL_MAX_DIST / max_exact)
                         * (REL_BUCKETS - max_exact)).astype(jnp.int32)
    large = jnp.minimum(large, REL_BUCKETS - 1)
    return jnp.where(n < max_exact, n, large)


def diff_attention(q, k, v, rel_bias, lam, lam_init, subln_g):
    B, S = q.shape[0], q.shape[1]
    nqb = S // BLOCK_Q
    scale = DA_HEAD_DIM ** -0.5
    kh = k.transpose(0, 2, 3, 1, 4)
    vh = v.transpose(0, 2, 1, 3)
    q_blocks = q.transpose(0, 2, 3, 1, 4).reshape(B, DA_HEADS, 2, nqb, BLOCK_Q, DA_HEAD_DIM)
    q_blocks = q_blocks.transpose(3, 0, 1, 2, 4, 5)
    table = rel_bias.astype(jnp.float32).reshape(REL_BUCKETS, DA_HEADS, 2)
    k_pos = jnp.arange(S)

    def one_block(args):
        qb, b_idx = args
        q_pos = b_idx * BLOCK_Q + jnp.arange(BLOCK_Q)
        dist = q_pos[:, None] - k_pos[None, :]
        bias = table[t5_bucket(dist)].transpose(2, 3, 0, 1)
        logits = jnp.einsum('bhmqd,bhmkd->bhmqk', qb, kh).astype(jnp.float32) * scale + bias
        logits = jnp.where(dist >= 0, logits, -1e30)
        p = jax.nn.softmax(logits, axis=-1)
        a = p[:, :, 0] - lam * p[:, :, 1]
        return jnp.einsum('bhqk,bhke->bhqe', a.astype(vh.dtype), vh)

    out = lax.map(one_block, (q_blocks, jnp.arange(nqb)))
    out = out.transpose(1, 0, 3, 2, 4).reshape(B, S, DA_HEADS, DA_V_DIM).astype(jnp.float32)
    out = out * lax.rsqrt(jnp.mean(jnp.square(out), axis=-1, keepdims=True) + NORM_EPS) * subln_g
    return (out * (1.0 - lam_init)).reshape(B, S, DA_WIDTH).astype(q.dtype)


def pool_mixer(p, pool_w, pool_scale):
    B, S, _ = p.shape
    pf = p.astype(jnp.float32).reshape(B, S, POOL_GROUPS, POOL_GROUP_DIM)
    csum = jnp.concatenate([jnp.zeros((B, 1, POOL_GROUPS, POOL_GROUP_DIM), jnp.float32),
                            jnp.cumsum(pf, axis=1)], axis=1)
    t = jnp.arange(S)
    outs = []
    for gi, w in enumerate(POOL_WINDOWS):
        lo = jnp.maximum(t + 1 - w, 0)
        cnt = jnp.minimum(t + 1, w).astype(jnp.float32)
        mean = (csum[:, 1:, gi] - csum[:, lo, gi]) / cnt[None, :, None]
        outs.append(mean - pf[:, :, gi])
    pooled = jnp.stack(outs, axis=2)
    mixed = jnp.einsum('bsgc,gcd->bsgd', pooled, pool_w.astype(jnp.float32))
    return (mixed.reshape(B, S, POOL_WIDTH) * pool_scale).astype(p.dtype)


def rotate(t, pos):
    half = t.shape[-1] // 2
    inv = 10000.0 ** (-jnp.linspace(0.0, 1.0, half, dtype=jnp.float32))
    ang = pos[:, None].astype(jnp.float32) * inv[None, :]
    cos = jnp.cos(ang)[None, :, None, :]
    sin = jnp.sin(ang)[None, :, None, :]
    t1 = t[..., :half].astype(jnp.float32)
    t2 = t[..., half:].astype(jnp.float32)
    return jnp.concatenate([t1 * cos - t2 * sin, t1 * sin + t2 * cos], axis=-1)


def retention(q, k, v, g, gn_g):
    dtype = q.dtype
    B, S, H, d = q.shape
    pos = jnp.arange(S)
    q = rotate(q, pos)
    k = rotate(k, pos) * (d ** -0.5)
    log_gamma = jnp.log(1.0 - 2.0 ** (-5.0 - jnp.arange(H, dtype=jnp.float32)))
    C = RET_CHUNK
    nc = S // C
    idx = jnp.arange(C, dtype=jnp.float32)
    rel = idx[:, None] - idx[None, :]
    intra = jnp.where(rel >= 0, jnp.exp(log_gamma[:, None, None] * jnp.maximum(rel, 0.0)), 0.0)
    q_decay = jnp.exp(log_gamma[:, None] * (idx + 1.0))[..., None]
    k_decay = jnp.exp(log_gamma[:, None] * (C - 1.0 - idx))[..., None]
    chunk_decay = jnp.exp(log_gamma * C)[:, None, None]

    def to_chunks(t):
        return t.astype(jnp.float32).reshape(B, nc, C, H, d).transpose(1, 0, 3, 2, 4)

    def step(state, inp):
        qc, kc, vc = inp
        inner = jnp.einsum('bhid,bhjd->bhij', qc, kc) * intra
        y = (jnp.einsum('bhij,bhje->bhie', inner, vc)
             + jnp.einsum('bhid,bhde->bhie', qc * q_decay, state))
        state = state * chunk_decay + jnp.einsum('bhjd,bhje->bhde', kc * k_decay, vc)
        return state, y

    state0 = jnp.zeros((B, H, d, d), jnp.float32)
    _, y = lax.scan(step, state0, (to_chunks(q), to_chunks(k), to_chunks(v)))
    y = y.transpose(1, 0, 3, 2, 4).reshape(B, S, H, d)
    mu = jnp.mean(y, axis=-1, keepdims=True)
    var = jnp.mean(jnp.square(y - mu), axis=-1, keepdims=True)
    y = ((y - mu) * lax.rsqrt(var + NORM_EPS)).reshape(B, S, H * d) * gn_g
    return (jax.nn.silu(g.reshape(B, S, H * d).astype(jnp.float32)) * y).astype(dtype)


def swiglu(h, w_gate, w_up, w_down):
    return jnp.dot(jax.nn.silu(jnp.dot(h, w_gate)) * jnp.dot(h, w_up), w_down)


def moe_swiglu(h, router_w, w_gate, w_up, w_down):
    B, S, D = h.shape
    n_tok = B * S
    n_assign = n_tok * TOP_K
    hf = h.reshape(n_tok, D)
    logits = jnp.dot(hf, router_w).astype(jnp.float32)
    top_vals, top_idx = lax.top_k(logits, TOP_K)
    gates = jax.nn.softmax(top_vals, axis=-1)
    flat_e = top_idx.reshape(-1)
    order = jnp.argsort(flat_e, stable=True)
    sorted_e = flat_e[order]
    tok_sorted = (order // TOP_K).astype(jnp.int32)
    counts = jnp.bincount(flat_e, length=N_EXPERTS)
    padded = (counts + MOE_BLOCK - 1) // MOE_BLOCK * MOE_BLOCK
    pad_end = jnp.cumsum(padded)
    pad_start = pad_end - padded
    start = jnp.cumsum(counts) - counts
    dest = pad_start[sorted_e] + jnp.arange(n_assign) - start[sorted_e]
    n_slots = (n_assign + MOE_BLOCK - 1) // MOE_BLOCK * MOE_BLOCK + N_EXPERTS * MOE_BLOCK
    n_blocks = n_slots // MOE_BLOCK
    buf_tok = jnp.full((n_slots,), n_tok, jnp.int32).at[dest].set(tok_sorted)
    block_e = jnp.minimum(jnp.searchsorted(pad_end, jnp.arange(n_blocks) * MOE_BLOCK, side='right'),
                          N_EXPERTS - 1)
    h_pad = jnp.concatenate([hf, jnp.zeros((1, D), hf.dtype)], axis=0)
    xb = h_pad[buf_tok].reshape(n_blocks, MOE_BLOCK, D)

    def expert_block(args):
        xblk, e = args
        return swiglu(xblk, w_gate[e], w_up[e], w_down[e])

    yb = lax.map(expert_block, (xb, block_e)).reshape(n_slots, D)
    contrib = yb[dest] * gates.reshape(-1)[order][:, None].astype(yb.dtype)
    out = jax.ops.segment_sum(contrib, tok_sorted, num_segments=n_tok)
    return out.reshape(B, S, D)


def setup_inputs(seed: int = 0) -> dict:
    key = jax.random.key(seed)
    ks = jax.random.split(key, 22)
    n_dense = (DEPTH + 1) // 2
    n_moe = DEPTH // 2
    nrm = jax.random.normal
    f32 = jnp.float32
    return {
        'x': nrm(ks[0], (BATCH, SEQ, D_MODEL), f32),
        'rel_bias': 0.5 * nrm(ks[1], (REL_BUCKETS, 2 * DA_HEADS), f32),
        'w_in': nrm(ks[2], (DEPTH, D_MODEL, IN_WIDTH), f32) * D_MODEL ** -0.5,
        'diff_lambda': 0.1 * nrm(ks[3], (DEPTH, 4, DA_HEAD_DIM), f32),
        'diff_subln_g': 1.0 + 0.1 * nrm(ks[4], (DEPTH, DA_V_DIM), f32),
        'pool_w': nrm(ks[5], (DEPTH, POOL_GROUPS, POOL_GROUP_DIM, POOL_GROUP_DIM), f32) * POOL_GROUP_DIM ** -0.5,
        'pool_scale': 1.0 + 0.1 * nrm(ks[6], (DEPTH, POOL_WIDTH), f32),
        'ret_gn_g': 1.0 + 0.1 * nrm(ks[7], (DEPTH, RET_WIDTH), f32),
        'w_out': nrm(ks[8], (DEPTH, MIX_WIDTH, D_MODEL), f32) * (MIX_WIDTH ** -0.5 * BETA),
        'ln1_g': 1.0 + 0.1 * nrm(ks[9], (DEPTH, D_MODEL), f32),
        'ln1_b': 0.02 * nrm(ks[10], (DEPTH, D_MODEL), f32),
        'ln2_g': 1.0 + 0.1 * nrm(ks[11], (DEPTH, D_MODEL), f32),
        'ln2_b': 0.02 * nrm(ks[12], (DEPTH, D_MODEL), f32),
        'ffn_w_gate': nrm(ks[13], (n_dense, D_MODEL, FFN_DIM), f32) * D_MODEL ** -0.5,
        'ffn_w_up': nrm(ks[14], (n_dense, D_MODEL, FFN_DIM), f32) * D_MODEL ** -0.5,
        'ffn_w_down': nrm(ks[15], (n_dense, FFN_DIM, D_MODEL), f32) * (FFN_DIM ** -0.5 * BETA),
        'router_w': nrm(ks[16], (n_moe, D_MODEL, N_EXPERTS), f32) * D_MODEL ** -0.5,
        'moe_w_gate': nrm(ks[17], (n_moe, N_EXPERTS, D_MODEL, EXPERT_DIM), f32) * D_MODEL ** -0.5,
        'moe_w_up': nrm(ks[18], (n_moe, N_EXPERTS, D_MODEL, EXPERT_DIM), f32) * D_MODEL ** -0.5,
        'moe_w_down': nrm(ks[19], (n_moe, N_EXPERTS, EXPERT_DIM, D_MODEL), f32) * (EXPERT_DIM ** -0.5 * BETA),
    }


def reference(x, rel_bias, w_in, diff_lambda, diff_subln_g, pool_w, pool_scale, ret_gn_g, w_out,
              ln1_g, ln1_b, ln2_g, ln2_b, ffn_w_gate, ffn_w_up, ffn_w_down,
              router_w, moe_w_gate, moe_w_up, moe_w_down):
    B, S, D = x.shape
    for l in range(DEPTH):
        proj = jnp.dot(x, w_in[l])
        dq, dk, dv, pp, rq, rk, rv, rg = jnp.split(proj, IN_SPLITS, axis=-1)
        lam_init = 0.8 - 0.6 * math.exp(-0.3 * l)
        lq1, lk1, lq2, lk2 = [diff_lambda[l, i].astype(jnp.float32) for i in range(4)]
        lam = jnp.exp(jnp.sum(lq1 * lk1)) - jnp.exp(jnp.sum(lq2 * lk2)) + lam_init
        y_da = diff_attention(dq.reshape(B, S, DA_HEADS, 2, DA_HEAD_DIM),
                              dk.reshape(B, S, DA_HEADS, 2, DA_HEAD_DIM),
                              dv.reshape(B, S, DA_HEADS, DA_V_DIM),
                              rel_bias, lam, lam_init, diff_subln_g[l])
        y_pool = pool_mixer(pp, pool_w[l], pool_scale[l])
        rshape = (B, S, RET_HEADS, RET_HEAD_DIM)
        y_ret = retention(rq.reshape(rshape), rk.reshape(rshape), rv.reshape(rshape),
                          rg.reshape(rshape), ret_gn_g[l])
        mix = jnp.dot(jnp.concatenate([y_da, y_pool, y_ret], axis=-1), w_out[l])
        x = layer_norm(ALPHA * x + mix, ln1_g[l], ln1_b[l])
        if l % 2 == 0:
            j = l // 2
            f = swiglu(x, ffn_w_gate[j], ffn_w_up[j], ffn_w_down[j])
        else:
            j = l // 2
            f = moe_swiglu(x, router_w[j], moe_w_gate[j], moe_w_up[j], moe_w_down[j])
        x = layer_norm(ALPHA * x + f, ln2_g[l], ln2_b[l])
    return x
```

```python
import math
from contextlib import ExitStack
import numpy as np
import ml_dtypes
import concourse.bass as bass
import concourse.mybir as mybir
from concourse.bass_utils import run_bass_kernel_spmd

F32 = mybir.dt.float32
BF16 = mybir.dt.bfloat16
AF = mybir.ActivationFunctionType
ALU = mybir.AluOpType
AX = mybir.AxisListType

D = 1024
DEPTH = 2
INW = 2816
INWX = 3328
FFN_DIM = 2816
N_EXPERTS = 8
EXPERT_DIM = 3584
ALPHA = (2 * DEPTH) ** 0.25
LN_EPS = 1e-5
NORM_EPS = 1e-6
NEG = -30000.0
ENGS = ("pe", "act", "dve", "pool", "sp")
PHASES = "0ABCD"
ASTOP = 0
BSTOP = 0
SPARSE = True
OVERLAP_CONV = True
ASUB = ""
LAYERS = "01"


class Prog:
    def __init__(self, nc):
        self.nc = nc
        self.streams = {e: [] for e in ENGS}
        self.cnt = {e: 0 for e in ENGS}
        self.esem = {}
        self.lanes = {}
        self.res = {}
        self.seen = {e: {} for e in ENGS}
        self._sem_ctx = []
        self.free_lanes = []
        self.free_sw = []
        self.nlane = 0

    def _new_sem(self, name):
        ctx = self.nc.semaphore(name)
        s = ctx.__enter__()
        self._sem_ctx.append(ctx)
        return s

    def eng_sem(self, e):
        if e not in self.esem:
            self.esem[e] = self._new_sem("se_" + e)
        return self.esem[e]

    def lane(self, key, sw=False):
        if key not in self.lanes:
            pool = self.free_sw if sw else self.free_lanes
            if pool:
                ent = pool.pop()
            else:
                self.nlane += 1
                ent = [self._new_sem("ln%d" % self.nlane), 0, sw]
            self.lanes[key] = ent
        return self.lanes[key]

    def op(self, eng, fns, reads=(), writes=(), lane=None):
        if callable(fns):
            fns = [fns]
        deps = {}

        def add(tok):
            if tok is None:
                return
            sem, val, peng = tok
            if peng == "pe" and eng == "pe" and lane is None:
                return
            k = id(sem)
            if k not in deps or deps[k][1] < val:
                deps[k] = (sem, val)

        for k in reads:
            st = self.res.get(k)
            if st:
                add(st[0])
        for k in writes:
            st = self.res.get(k)
            if st:
                add(st[0])
                for t in st[1]:
                    add(t)
        waits = []
        seen = self.seen[eng]
        for k, (sem, val) in deps.items():
            if seen.get(k, 0) >= val:
                continue
            seen[k] = val
            waits.append((sem, val))
        if lane is None:
            sem = self.eng_sem(eng)
            self.cnt[eng] += 1
            val = self.cnt[eng]
            inc = 1
            tok = (sem, val, eng)
        else:
            ln = self.lane(lane, sw=(eng == "pool"))
            ln[1] += 16
            sem, val, inc = ln[0], ln[1], 16
            tok = (sem, val, "dma")
        for k in reads:
            st = self.res.setdefault(k, [None, []])
            st[1].append(tok)
        for k in writes:
            self.res[k] = [tok, []]
        self.streams[eng].append((waits, fns, sem, inc))
        return tok

    def dma(self, eng, out, in_, reads=(), writes=(), lane=None):
        assert lane is not None
        return self.op(eng, lambda e: e.dma_start(out=out, in_=in_), reads=reads, writes=writes, lane=lane)

    def barrier(self):
        toks = []
        for e in ENGS:
            if self.cnt[e] > 0:
                toks.append((self.esem[e], self.cnt[e]))
        for k, (sem, c, _sw) in self.lanes.items():
            if c > 0:
                toks.append((sem, c))
        for e in ENGS:
            waits = []
            seen = self.seen[e]
            for sem, val in toks:
                if seen.get(id(sem), 0) >= val:
                    continue
                seen[id(sem)] = val
                waits.append((sem, val))
            if waits:
                self.streams[e].append((waits, [], None, 0))
        self.res = {}
        for ent in self.lanes.values():
            (self.free_sw if ent[2] else self.free_lanes).append(ent)
        self.lanes = {}

    def emit(self):
        nc = self.nc
        self.barrier()
        streams = self.streams
        self.streams = {e: [] for e in ENGS}
        with nc.Block() as block:
            def run(engobj, stream):
                for waits, fns, sem, inc in stream:
                    for s, v in waits:
                        engobj.wait_ge(s, v)
                    ins = None
                    for f in fns:
                        ins = f(engobj)
                    if ins is not None and sem is not None:
                        ins.then_inc(sem, inc)

            @block.tensor
            def _(t):
                run(t, streams["pe"])

            @block.scalar
            def _(t):
                run(t, streams["act"])

            @block.vector
            def _(t):
                run(t, streams["dve"])

            @block.gpsimd
            def _(t):
                run(t, streams["pool"])

            @block.sync
            def _(t):
                run(t, streams["sp"])

    def close(self):
        for ctx in reversed(self._sem_ctx):
            ctx.__exit__(None, None, None)


_UQ = [0]


def _uniq(n):
    _UQ[0] += 1
    return '%s_%d' % (n, _UQ[0])


def MM(out, lhsT, rhs, start=True, stop=True, skip=False):
    if skip:
        return lambda e: e.matmul(out, lhsT=lhsT, rhs=rhs, start=start, stop=stop, skip_group_check=True)
    return lambda e: e.matmul(out, lhsT=lhsT, rhs=rhs, start=start, stop=stop)


def TR(out, in_, ident):
    return lambda e: e.transpose(out=out, in_=in_, identity=ident)


def ACT(out, in_, func, **kw):
    return lambda e: e.activation(out=out, in_=in_, func=func, **kw)


def TT(out, a, b, op):
    return lambda e: e.tensor_tensor(out=out, in0=a, in1=b, op=op)


def TS(out, a, s1, s2, op0, op1=None):
    if op1 is None:
        return lambda e: e.tensor_scalar(out=out, in0=a, scalar1=s1, scalar2=None, op0=op0)
    return lambda e: e.tensor_scalar(out=out, in0=a, scalar1=s1, scalar2=s2, op0=op0, op1=op1)


def STT(out, a, s, b, op0, op1):
    return lambda e: e.scalar_tensor_tensor(out=out, in0=a, scalar=s, in1=b, op0=op0, op1=op1)


def CP(out, in_):
    return lambda e: e.tensor_copy(out=out, in_=in_)


def RED(out, in_, op):
    return lambda e: e.tensor_reduce(out=out, in_=in_, axis=AX.X, op=op)


def RCP(out, in_):
    return lambda e: e.reciprocal(out=out, in_=in_)


def MSET(ap, v):
    return lambda e: e.memset(ap, v)


def cast_op(p, i, out, in_, reads, writes):
    k = i % 3
    if k == 0:
        p.op("dve", CP(out, in_), reads=reads, writes=writes)
    elif k == 1:
        p.op("act", ACT(out, in_, AF.Copy), reads=reads, writes=writes)
    else:
        p.op("pool", CP(out, in_), reads=reads, writes=writes)


def build(S, dev=False):
    assert S % 1024 == 0
    nc = bass.Bass("TRN2", target_bir_lowering=False)
    NB = S // 128
    NT5 = S // 512

    NSLOT = 2 * S + N_EXPERTS * 512
    NBLK = NSLOT // 512

    def din(name, shape, dt=F32):
        return nc.dram_tensor(name, list(shape), dt, kind="ExternalInput").ap()

    def dscr(name, shape, dt):
        if dev:
            return nc.dram_tensor(name, list(shape), dt, kind="ExternalOutput").ap()
        return nc.dram_tensor(name, list(shape), dt).ap()

    x_in = din("x", [S, D])
    w_in = din("w_in", [DEPTH, D, INW])
    w_out = din("w_out", [DEPTH, D, D])
    router_w = din("router_w", [D, N_EXPERTS])
    if "0" in PHASES:
        ffn_wg = din("ffn_w_gate", [1, D, FFN_DIM])
        ffn_wu = din("ffn_w_up", [1, D, FFN_DIM])
        ffn_wd = din("ffn_w_down", [1, FFN_DIM, D])
        moe_wg = din("moe_w_gate", [N_EXPERTS, D, EXPERT_DIM])
        moe_wu = din("moe_w_up", [N_EXPERTS, D, EXPERT_DIM])
        moe_wd = din("moe_w_down", [N_EXPERTS, EXPERT_DIM, D])
    lam_rep = din("lam_rep", [DEPTH, 128, 256])
    subln_rep = din("subln_rep", [DEPTH, 128, 128])
    poolw = din("pool_w", [DEPTH, 4, 64, 64])
    pscale = din("pscale", [DEPTH, 128, 2])
    gn_rep = din("gn_rep", [DEPTH, 128, 256])
    ln_rep = din("ln_rep", [DEPTH, 4, 128, D])
    ident_d = din("ident", [128, 128])
    cos_d = din("cos_t", [128, S])
    sin_d = din("sin_t", [128, S])
    bt_d = din("bt", [128, 2, 8, 128])
    maskneg_d = din("maskneg", [128, 128])
    cb_d = din("cb", [128, 8])
    maskt_d = din("maskt", [128, 4, 128])
    qdec_d = din("qdec", [128, 2, 512])
    kdec_d = din("kdec", [128, 4, 64])
    cd_d = din("cd", [128, 2])
    bdmask_d = din("bdmask", [128, 128])
    invc_d = din("invc", [128, 2, 2, 512])
    ut_d = din("ut", [128, 128])
    kth_d = din("kth", [128, 32])
    bstart_d = din("bstart", [128, NBLK])
    su_d = din("su", [128, 32])
    cu_d = din("cu", [128, 32])

    out_d = nc.dram_tensor("out", [S, D], F32, kind="ExternalOutput").ap()

    QT = dscr("QT", [512, S], BF16)
    KT = dscr("KT", [512, S], BF16)
    VV = dscr("VV", [S, 512], BF16)
    RQT = dscr("RQT", [256, S], BF16)
    RKT = dscr("RKT", [256, S], BF16)
    RV = dscr("RV", [S, 256], BF16)
    RG = dscr("RG", [S, 256], F32)
    CATT = dscr("CATT", [D, S], BF16)
    X1S = dscr("X1S", [S, D], F32)
    X1F = nc.dram_tensor("X1F", [S, D], F32).ap()
    X1B = nc.dram_tensor("X1B", [S, D], BF16).ap()
    XB = nc.dram_tensor("XB", [NSLOT, D], BF16).ap()
    YB = nc.dram_tensor("YB", [NSLOT, D], F32).ap()
    NFC_D = FFN_DIM // 128
    NFC_M = EXPERT_DIM // 128
    WGU_D = nc.dram_tensor("WGU_D", [NFC_D, 128, 2048], BF16).ap()
    WD_D = nc.dram_tensor("WD_D", [NFC_D, 128, D], BF16).ap()
    WGU_M = nc.dram_tensor("WGU_M", [N_EXPERTS * NFC_M, 128, 2048], BF16).ap()
    WD_M = nc.dram_tensor("WD_M", [N_EXPERTS * 4, 128, 7, D], BF16).ap()

    p = Prog(nc)

    def make_conv_units():
        units = []

        def conv_set(wg, wu, wd, WGU, WDs, NE, F):
            NFC = F // 128
            for e in range(NE):
                for which, w in ((0, wg), (1, wu)):
                    for kc in range(8):
                        src = w[e, kc * 128:(kc + 1) * 128, :]
                        base = WGU[e * NFC:(e + 1) * NFC, :, which * 1024 + kc * 128: which * 1024 + (kc + 1) * 128]
                        dst = base.rearrange("c p j -> p c j")
                        units.append((src, F, dst, lambda s_: s_, lambda s_: s_.rearrange("p (c j) -> p c j", j=128)))
                wdv = wd[e].rearrange("(c p) d -> p c d", p=128)
                for c0 in range(0, NFC, 2):
                    src = wdv[:, c0:c0 + 2, :]
                    if NE == 1:
                        dst = WDs[e * NFC + c0: e * NFC + c0 + 2].rearrange("c p d -> p c d")
                    else:
                        dst = [WDs[e * 4 + (c0 + q) // 7, :, (c0 + q) % 7, :] for q in range(2)]
                    v2 = lambda s_: s_.rearrange("p (c d) -> p c d", d=D)
                    units.append((src, 2 * D, dst, v2, v2))

        conv_set(ffn_wg, ffn_wu, ffn_wd, WGU_D, WD_D, 1, FFN_DIM)
        n_dense = len(units)
        conv_set(moe_wg, moe_wu, moe_wd, WGU_M, WD_M, N_EXPERTS, EXPERT_DIM)
        return units, n_dense

    class ConvStream:
        def __init__(self, units, c32, c16):
            self.units = units
            self.c32 = c32
            self.c16 = c16
            self.i = 0
            self.pending = None

        def _store(self):
            if self.pending is None:
                return
            dst_ap, st_view, s16, b = self.pending
            self.pending = None
            if isinstance(dst_ap, list):
                v = st_view(s16)
                for q, d_ in enumerate(dst_ap):
                    p.dma("sp", d_, v[:, q, :], reads=[("c16", b)], lane=("c16", b))
            else:
                p.dma("sp", dst_ap, st_view(s16), reads=[("c16", b)], lane=("c16", b))

        def step(self):
            self._store()
            if self.i >= len(self.units):
                return False
            src_ap, n, dst_ap, ld_view, st_view = self.units[self.i]
            b = self.i % 2
            s32 = self.c32[:, b, 0:n]
            s16 = self.c16[:, b, 0:n]
            p.dma("sp", ld_view(s32), src_ap, writes=[("c32", b)], lane=("c32", b))
            if self.i % 3 == 2:
                p.op("pool", CP(s16, s32), reads=[("c32", b)], writes=[("c16", b)])
            else:
                p.op("dve", CP(s16, s32), reads=[("c32", b)], writes=[("c16", b)])
            self.pending = (dst_ap, st_view, s16, b)
            self.i += 1
            return True

        def flush(self):
            while self.step():
                pass
            self._store()

    def phase_convert():
        FMAX = EXPERT_DIM
        with ExitStack() as es:
            sb = lambda n, *a: es.enter_context(nc.sbuf_tensor(_uniq(n), *a))
            pst = lambda n, *a: es.enter_context(nc.psum_tensor(_uniq(n), *a))
            c32 = sb("c32", [128, 2, FMAX], F32)
            c16 = sb("c16", [128, 2, FMAX], BF16)
            cnt = [0]

            def unit(src_ap, n, dst_ap, ld_view, st_view):
                i = cnt[0]
                cnt[0] += 1
                b = i % 2
                s32 = c32[:, b, 0:n]
                s16 = c16[:, b, 0:n]
                p.dma("sp", ld_view(s32), src_ap, writes=[("c32", b)], lane=("c32", b))
                cast_op(p, i, s16, s32, reads=[("c32", b)], writes=[("c16", b)])
                if isinstance(dst_ap, list):
                    v = st_view(s16)
                    for q, d_ in enumerate(dst_ap):
                        p.dma("sp", d_, v[:, q, :], reads=[("c16", b)], lane=("c16", b))
                else:
                    p.dma("sp", dst_ap, st_view(s16), reads=[("c16", b)], lane=("c16", b))

            def conv_set(wg, wu, wd, WGU, WDs, NE, F):
                NFC = F // 128
                for e in range(NE):
                    for which, w in ((0, wg), (1, wu)):
                        for kc in range(8):
                            src = w[e, kc * 128:(kc + 1) * 128, :]
                            base = WGU[e * NFC:(e + 1) * NFC, :, which * 1024 + kc * 128: which * 1024 + (kc + 1) * 128]
                            dst = base.rearrange("c p j -> p c j")
                            unit(src, F, dst, lambda s: s, lambda s: s.rearrange("p (c j) -> p c j", j=128))
                    wdv = wd[e].rearrange("(c p) d -> p c d", p=128)
                    for c0 in range(0, NFC, 2):
                        src = wdv[:, c0:c0 + 2, :]
                        if NE == 1:
                            dst = WDs[e * NFC + c0: e * NFC + c0 + 2].rearrange("c p d -> p c d")
                        else:
                            dst = [WDs[e * 4 + (c0 + q) // 7, :, (c0 + q) % 7, :] for q in range(2)]
                        v2 = lambda s: s.rearrange("p (c d) -> p c d", d=D)
                        unit(src, 2 * D, dst, v2, v2)

            conv_set(ffn_wg, ffn_wu, ffn_wd, WGU_D, WD_D, 1, FFN_DIM)
            conv_set(moe_wg, moe_wu, moe_wd, WGU_M, WD_M, N_EXPERTS, EXPERT_DIM)
            p.emit()

    def phase_A(l, xin):
        with ExitStack() as es:
            sb = lambda n, *a: es.enter_context(nc.sbuf_tensor(_uniq(n), *a))
            pst = lambda n, *a: es.enter_context(nc.psum_tensor(_uniq(n), *a))
            WIN = sb("WIN", [128, 8, INWX], BF16)
            WST = sb("WST", [128, INW], F32)
            IDN = sb("IDN", [128, 128], F32)
            XIN = sb("XIN", [128, 6, D], F32)
            XT = sb("XT", [128, 2, 8, 512], BF16)
            QKO = sb("QKO", [128, 2, 8, 512], BF16)
            VO = sb("VO", [128, 2, 4, 512], BF16)
            RVO = sb("RVO", [128, 2, 4, 256], BF16)
            RGO = sb("RGO", [128, 2, 4, 256], F32)
            RQKO = sb("RQKO", [128, 2, 4, 512], BF16)
            PB = sb("PB", [128, 2, 528], F32)
            LA = sb("LA", [128, 2, 528], F32)
            LB = sb("LB", [128, 2, 528], F32)
            PTMP = sb("PTMP", [128, 512], F32)
            PLD = sb("PLD", [128, 2, 512], BF16)
            PO = sb("PO", [128, 2, 2, 512], BF16)
            CS = sb("CS", [128, 2, 2, 512], F32)
            T1 = sb("T1", [128, 2, 512], F32)
            T2 = sb("T2", [128, 2, 512], F32)
            INVC = sb("INVC", [128, 2, 2, 512], F32)
            PW32 = sb("PW32", [128, 2, 128], F32)
            PWBD = sb("PWBD", [128, 2, 128], BF16)
            PSC = sb("PSC", [128, 2], F32)
            TRP0 = pst("TRP0", [128, 512], F32)
            TRP1 = pst("TRP1", [128, 512], F32)
            MMA = pst("MMA", [128, 512], F32)
            MMB = pst("MMB", [128, 512], F32)
            MMC = pst("MMC", [128, 512], F32)
            MMD = pst("MMD", [128, 512], F32)
            PMX = pst("PMX", [128, 512], F32)
            TRP = [TRP0, TRP1]
            MMP = [MMA, MMB, MMC, MMD]
            p.dma("sp", IDN[:], ident_d, writes=["IDN"], lane="IDN")
            p.dma("sp", INVC[:], invc_d, writes=["INVC"], lane="INVC")
            p.dma("sp", PSC[:], pscale[l], writes=["PSC"], lane="PSC")
            p.op("dve", MSET(PW32[:], 0.0), writes=["PW32"])
            for c in range(2):
                for hh in range(2):
                    g = 2 * c + hh
                    p.dma("sp", PW32[64 * hh:64 * hh + 64, c, 64 * hh:64 * hh + 64], poolw[l, g],
                          reads=[], writes=["PW32"], lane=("PW32", g))
            p.op("dve", CP(PWBD[:], PW32[:]), reads=["PW32"], writes=["PWBD"])
            for kc in range(8):
                p.dma("sp", WST[:], w_in[l, kc * 128:(kc + 1) * 128, :], writes=["WST"], lane="WST")
                cast_op(p, kc, WIN[:, kc, 0:INW], WST[:], reads=["WST"], writes=[("WIN", kc)])
                src = WST[:, 1792:2304].rearrange("p (h t f) -> p h t f", h=8, t=2)
                dst = WIN[:, kc, INW:INWX].rearrange("p (h t f) -> p h t f", h=8, t=2)
                p.op("act", ACT(dst[:, :, 0, :], src[:, :, 1, :], AF.Copy, scale=-1.0), reads=["WST"], writes=[("WINa", kc)])
                p.op("dve", CP(dst[:, :, 1, :], src[:, :, 0, :]), reads=["WST"], writes=[("WINb", kc)])
            WINK = [("WIN", kc) for kc in range(8)] + [("WINa", kc) for kc in range(8)] + [("WINb", kc) for kc in range(8)]
            p.op("pool", MSET(PB[:, :, 0:16], 0.0), writes=["PBh"])
            if ASTOP == 1:
                p.emit()
                return

            xv = xin.rearrange("(n p) d -> n p d", p=128)
            QTv = QT.rearrange("(c p) s -> p c s", p=128)
            KTv = KT.rearrange("(c p) s -> p c s", p=128)
            VVv = VV.rearrange("(n p) f -> p n f", p=128)
            RVv = RV.rearrange("(n p) f -> p n f", p=128)
            RGv = RG.rearrange("(n p) f -> p n f", p=128)
            RQTv = RQT.rearrange("(c p) s -> p c s", p=128)
            RKTv = RKT.rearrange("(c p) s -> p c s", p=128)
            CATv = CATT.rearrange("(c p) s -> p c s", p=128)

            mmi = [0]
            evi = [0]
            xi = [0]
            deferred = []

            def evac(out, in_, reads, writes):
                evi[0] += 1
                if evi[0] % 2:
                    p.op("act", ACT(out, in_, AF.Copy), reads=[], writes=list(writes) + list(reads))
                else:
                    p.op("dve", CP(out, in_), reads=[], writes=list(writes) + list(reads))

            for t in range(NT5):
                b = t % 2
                t0 = t * 512
                p.dma("sp", CS[:, b, 0, :], cos_d[:, t0:t0 + 512], writes=[("CS", b, 0)], lane=("CS", b, 0))
                p.dma("sp", CS[:, b, 1, :], sin_d[:, t0:t0 + 512], writes=[("CS", b, 1)], lane=("CS", b, 1))
                xsl = []
                for j in range(4):
                    s = xi[0] % 6
                    xi[0] += 1
                    p.dma("sp", XIN[:, s, :], xv[t * 4 + j], writes=[("XIN", s)], lane=("XIN", s))
                    xsl.append(s)
                for kc in range(8):
                    tp = TRP[kc % 2]
                    tk = ("TRP", kc % 2)
                    fns = [TR(tp[:, j * 128:(j + 1) * 128], XIN[:, xsl[j], kc * 128:(kc + 1) * 128], IDN[:]) for j in range(4)]
                    p.op("pe", fns, reads=["IDN"] + [("XIN", s) for s in xsl], writes=[tk])
                    evac(XT[:, b, kc, :], tp[:], reads=[tk], writes=[("XT", b, kc)])
                XTK = [("XT", b, kc) for kc in range(8)]
                if ASTOP == 2:
                    continue

                def fm_group(col0):
                    i = mmi[0] % 4
                    mmi[0] += 1
                    ps = MMP[i]
                    fns = [MM(ps[:], WIN[:, kc, col0:col0 + 128], XT[:, b, kc, :], start=(kc == 0), stop=(kc == 7)) for kc in range(8)]
                    p.op("pe", fns, reads=XTK + WINK, writes=[("MMP", i)])
                    return ps, ("MMP", i)

                def tm_group(j, col0):
                    i = mmi[0] % 4
                    mmi[0] += 1
                    ps = MMP[i]
                    fns = [MM(ps[:], XT[:, b, kc, j * 128:(j + 1) * 128], WIN[:, kc, col0:col0 + 512], start=(kc == 0), stop=(kc == 7)) for kc in range(8)]
                    p.op("pe", fns, reads=XTK + WINK, writes=[("MMP", i)])
                    return ps, ("MMP", i)

                for c in range(8):
                    ps, k = fm_group(c * 128)
                    evac(QKO[:, b, c, :], ps[:], reads=[k], writes=[("QKO", b, c)])
                while deferred:
                    deferred.pop(0)()
                if ASUB != "nostore":
                    p.dma("sp", QTv[:, :, t0:t0 + 512], QKO[:, b, 0:4, :], reads=[("QKO", b, c) for c in range(4)], lane=("QKOq", b))
                    p.dma("sp", KTv[:, :, t0:t0 + 512], QKO[:, b, 4:8, :], reads=[("QKO", b, c) for c in range(4, 8)], lane=("QKOk", b))
                if ASTOP == 3:
                    continue
                for j in range(4):
                    ps, k = tm_group(j, 1024)
                    p.op("act", ACT(VO[:, b, j, :], ps[:], AF.Copy), writes=[k, ("VO", b, j)])
                if ASUB != "novst":
                    p.dma("sp", VVv[:, t * 4:(t + 1) * 4, :], VO[:, b], reads=[("VO", b, j) for j in range(4)], lane=("VO", b))
                if ASUB in ("novst", "onlydv"):
                    continue
                for j in range(4):
                    ps, k = tm_group(j, 2304)
                    p.op("dve", CP(RVO[:, b, j, :], ps[:, 0:256]), writes=[k, ("RVO", b, j)])
                    p.op("act", ACT(RGO[:, b, j, :], ps[:, 256:512], AF.Copy if ASUB == "nosilu" else AF.Silu), writes=[k, ("RGO", b, j)])
                p.dma("sp", RVv[:, t * 4:(t + 1) * 4, :], RVO[:, b], reads=[("RVO", b, j) for j in range(4)], lane=("RVO", b))
                p.dma("sp", RGv[:, t * 4:(t + 1) * 4, :], RGO[:, b], reads=[("RGO", b, j) for j in range(4)], lane=("RGO", b))
                if ASTOP == 4:
                    continue
                for qk in range(2):
                    for c in range(2):
                        psA, kA = fm_group(1792 + qk * 256 + c * 128)
                        psB, kB = fm_group(INW + qk * 256 + c * 128)
                        p.op("dve", TT(T1[:, c, :], psA[:], CS[:, b, 0, :], ALU.mult), reads=[("CS", b, 0)], writes=[kA, ("T1", c)])
                        p.op("dve", TT(T2[:, c, :], psB[:], CS[:, b, 1, :], ALU.mult), reads=[("CS", b, 1)], writes=[kB, ("T2", c)])
                        p.op("pool", TT(RQKO[:, b, qk * 2 + c, :], T1[:, c, :], T2[:, c, :], ALU.add),
                             reads=[("T1", c), ("T2", c)], writes=[("RQKO", b, qk * 2 + c)])
                p.dma("sp", RQTv[:, :, t0:t0 + 512], RQKO[:, b, 0:2, :], reads=[("RQKO", b, 0), ("RQKO", b, 1)], lane=("RQKOq", b))
                p.dma("sp", RKTv[:, :, t0:t0 + 512], RQKO[:, b, 2:4, :], reads=[("RQKO", b, 2), ("RQKO", b, 3)], lane=("RQKOk", b))
                if ASTOP == 5:
                    continue
                for c in range(2):
                    ps, k = fm_group(1536 + c * 128)
                    p.op("act", ACT(PB[:, c, 16:528], ps[:], AF.Copy), writes=[k, ("PB", c)])
                PBK = [("PB", 0), ("PB", 1), "PBh"]
                p.op("pool", TT(LA[:, :, 1:528], PB[:, :, 1:528], PB[:, :, 0:527], ALU.add), reads=PBK, writes=["LA"])
                iv = INVC[:, 0 if t == 0 else 1]

                def pool_out(lv, c, hh):
                    sl = slice(64 * hh, 64 * hh + 64)
                    p.op("pool", TT(PTMP[sl, :], lv[sl, c, 16:528], iv[sl, c, :], ALU.mult), reads=["LA", "LB", "INVC"], writes=[("PTMP", hh)])
                    p.op("pool", TT(PLD[sl, c, :], PTMP[sl, :], PB[sl, c, 16:528], ALU.subtract),
                         reads=[("PTMP", hh)] + PBK, writes=[("PLD", c, hh)])

                pool_out(LA, 0, 0)
                p.op("pool", TT(LB[:, :, 3:528], LA[:, :, 3:528], LA[:, :, 1:526], ALU.add), reads=["LA"], writes=["LB"])
                pool_out(LB, 0, 1)
                p.op("pool", TT(LA[:, :, 7:528], LB[:, :, 7:528], LB[:, :, 3:524], ALU.add), reads=["LB"], writes=["LA"])
                pool_out(LA, 1, 0)
                p.op("pool", TT(LB[:, :, 15:528], LA[:, :, 15:528], LA[:, :, 7:520], ALU.add), reads=["LA"], writes=["LB"])
                pool_out(LB, 1, 1)
                p.op("pool", CP(PB[:, :, 0:16], PB[:, :, 512:528]), reads=[("PB", 0), ("PB", 1)] + [("PLD", c, hh) for c in range(2) for hh in range(2)], writes=["PBh"])
                def mix(b=b, t0=t0):
                    for c in range(2):
                        p.op("pe", MM(PMX[:], PWBD[:, c, :], PLD[:, c, :]), reads=["PWBD", ("PLD", c, 0), ("PLD", c, 1)], writes=["PMX"])
                        p.op("dve", TS(PO[:, b, c, :], PMX[:], PSC[:, c:c + 1], None, ALU.mult), reads=["PSC"], writes=["PMX", ("PO", b, c)])
                    p.dma("sp", CATv[:, 4:6, t0:t0 + 512], PO[:, b], reads=[("PO", b, 0), ("PO", b, 1)], lane=("PO", b))
                deferred.append(mix)
            while deferred:
                deferred.pop(0)()
            p.emit()

    def phase_B(l, conv_units=None):
        lam_init = 0.8 - 0.6 * math.exp(-0.3 * l)
        scale = 64 ** -0.5
        with ExitStack() as es:
            sb = lambda n, *a: es.enter_context(nc.sbuf_tensor(_uniq(n), *a))
            pst = lambda n, *a: es.enter_context(nc.psum_tensor(_uniq(n), *a))
            KTh2 = sb("KTh", [128, 2, S], BF16)
            Vh2 = sb("Vh", [128, 2, NB, 132], BF16)
            cstream = None
            if conv_units:
                c32 = sb("c32", [128, 2, EXPERT_DIM], F32)
                c16 = sb("c16", [128, 2, EXPERT_DIM], BF16)
                cstream = ConvStream(conv_units, c32, c16)
                n_steps_total = sum(2 * (4 * q + 4) for q in range(NT5)) * 4
                conv_every = max(1, n_steps_total // (len(conv_units) + 1))
            step_ctr = [0]
            QZ = sb("QZ", [128, 2, 2, 512], BF16)
            PT = sb("PT", [128, 6, 512], BF16)
            TMP = sb("TMP", [128, 2, 128], F32)
            BT = sb("BT", [128, 2, 8, 128], F32)
            MNEG = sb("MNEG", [128, 128], F32)
            CB = sb("CB", [128, 8], F32)
            LAMT = sb("LAMT", [128, 256], F32)
            LPR = sb("LPR", [128, 2, 64], F32)
            LS = sb("LS", [128, 2], F32)
            LE = sb("LE", [128, 2], F32)
            NLAM = sb("NLAM", [128, 1], F32)
            SG = sb("SG", [128, 128], F32)
            IDB = sb("IDB", [128, 128], BF16)
            IDF = sb("IDF", [128, 128], F32)
            RR = sb("RR", [128, 2, 4, 2], F32)
            RL = sb("RL", [128, 2, 4], F32)
            OO = sb("OO", [128, 2, 128], F32)
            JUNK = sb("JUNK", [128, 128], F32)
            SS = sb("SS", [128, 2, 4], F32)
            SD = sb("SD", [128, 2, 4], F32)
            RS = sb("RS", [128, 2, 4], F32)
            EPSN = sb("EPSN", [128, 1], F32)
            YT = sb("YT", [128, 2, 128], BF16)
            YO = sb("YO", [128, 2, 512], BF16)
            ST0 = pst("ST0", [128, 512], F32)
            ST1 = pst("ST1", [128, 512], F32)
            ST2 = pst("ST2", [128, 512], F32)
            ACC = pst("ACC", [128, 4, 512], F32)
            TRB = pst("TRB", [128, 512], F32)
            STP = [ST0, ST1, ST2]
            p.dma("sp", BT[:], bt_d, writes=["BT"], lane="BT")
            p.dma("sp", MNEG[:], maskneg_d, writes=["MNEG"], lane="MNEG")
            p.dma("sp", CB[:], cb_d, writes=["CB"], lane="CB")
            p.dma("sp", LAMT[:], lam_rep[l], writes=["LAMT"], lane="LAMT")
            p.dma("sp", SG[:], subln_rep[l], writes=["SG"], lane="SG")
            p.dma("sp", IDF[:], ident_d, writes=["IDF"], lane="IDF")
            p.op("dve", CP(IDB[:], IDF[:]), reads=["IDF"], writes=["IDB"])
            p.op("dve", MSET(EPSN[:], NORM_EPS), writes=["EPSN"])
            for hm in range(8):
                p.op("dve", TT(BT[:, 0, hm, :], BT[:, 0, hm, :], MNEG[:], ALU.add), reads=["BT", "MNEG"], writes=["BT"])
            LV = LAMT[:].rearrange("p (a b f) -> p a b f", a=2, b=2)
            p.op("dve", TT(LPR[:], LV[:, :, 0, :], LV[:, :, 1, :], ALU.mult), reads=["LAMT"], writes=["LPR"])
            p.op("dve", RED(LS[:], LPR[:], ALU.add), reads=["LPR"], writes=["LS"])
            p.op("act", ACT(LE[:], LS[:], AF.Exp), reads=["LS"], writes=["LE"])
            p.op("dve", TT(NLAM[:], LE[:, 1:2], LE[:, 0:1], ALU.subtract), reads=["LE"], writes=["NLAM"])
            p.op("dve", TS(NLAM[:], NLAM[:], -lam_init, None, ALU.add), reads=["NLAM"], writes=["NLAM"])
            p.op("dve", TS(SG[:], SG[:], 1.0 - lam_init, None, ALU.mult), reads=["SG"], writes=["SG"])
            p.op("pool", MSET(Vh2[:, :, :, 128:132], 1.0), writes=["Vh1"])
            p.op("pool", MSET(QZ[:], 0.0), writes=[("QTt", 0), ("QTt", 1)])
            if BSTOP == 1:
                p.emit()
                return

            KTv = KT.rearrange("(h p) s -> h p s", p=128)
            QTv = QT.rearrange("(h p) s -> h p s", p=128)
            VVv = VV.rearrange("(n p) f -> p n f", p=128)
            CATv = CATT.rearrange("(c p) s -> c p s", p=128)
            sti = [0]
            pti = [0]
            tmi = [0]
            gi = [0]

            for h in range(4):
                hb = h % 2
                KTh = KTh2[:, hb, :]
                Vh = Vh2[:, hb]
                if h == 0:
                    p.dma("sp", KTh2[:, 0, :], KTv[0], writes=[("KTh", 0)], lane=("KTh", 0))
                    p.dma("sp", Vh2[:, 0, :, 0:128], VVv[:, :, 0:128], writes=[("Vh", 0)], lane=("Vh", 0))
                if h + 1 < 4:
                    nb_ = (h + 1) % 2
                    p.dma("sp", KTh2[:, nb_, :], KTv[h + 1], writes=[("KTh", nb_)], lane=("KTh", nb_))
                    p.dma("sp", Vh2[:, nb_, :, 0:128], VVv[:, :, (h + 1) * 128:(h + 2) * 128], writes=[("Vh", nb_)], lane=("Vh", nb_))
                for qg in range(NT5):
                    g = gi[0] % 2
                    gi[0] += 1
                    t0 = qg * 512
                    p.dma("sp", QZ[0:64, g, 0, :], QTv[h, 0:64, t0:t0 + 512], writes=[("QTt", g)], lane=("QTt", g, 0))
                    p.dma("sp", QZ[64:128, g, 1, :], QTv[h, 64:128, t0:t0 + 512], reads=[("QTt", g)], writes=[("QTtb", g)], lane=("QTt", g, 1))
                    nkb = 4 * qg + 4
                    steps = [(kb, m) for kb in range(nkb) for m in range(2)]
                    first_pv = {}

                    def qk(step):
                        kb, m = step
                        i = sti[0] % 3
                        sti[0] += 1
                        p.op("pe", MM(STP[i][:], KTh[:, kb * 128:(kb + 1) * 128], QZ[:, g, m, :]),
                             reads=[("KTh", hb), ("QTt", g), ("QTtb", g)], writes=[("ST", i)])
                        return i

                    def softmax_pv(step, i):
                        kb, m = step
                        if BSTOP == 2:
                            return
                        hm = 2 * h + m
                        st = STP[i]
                        pi = pti[0] % 6
                        pti[0] += 1
                        jmin = max(0, kb - 4 * qg)
                        jc = max(0, kb - 4 * qg + 2)
                        wk = []
                        for j in range(jmin, min(jc, 4)):
                            rel = 4 * qg + j - kb
                            case = 0 if rel == 0 else 1
                            ti = tmi[0] % 2
                            tmi[0] += 1
                            p.op("dve", STT(TMP[:, ti, :], st[:, j * 128:(j + 1) * 128], scale, BT[:, case, hm, :], ALU.mult, ALU.add),
                                 reads=["BT"], writes=[("ST", i), ("TMP", ti)])
                            p.op("act", ACT(PT[:, pi, j * 128:(j + 1) * 128], TMP[:, ti, :], AF.Exp),
                                 reads=[("TMP", ti)], writes=[("PT", pi, j)])
                        if jc < 4:
                            p.op("act", ACT(PT[:, pi, jc * 128:512], st[:, jc * 128:512], AF.Exp, bias=CB[:, hm:hm + 1], scale=scale),
                                 reads=["CB"], writes=[("ST", i)] + [("PT", pi, j) for j in range(jc, 4)])
                        if BSTOP == 3:
                            return
                        fns = []
                        for j in range(jmin, 4):
                            st_flag = j not in first_pv
                            first_pv[j] = True
                            last = (kb == 4 * qg + j) and m == 1
                            fns.append(MM(ACC[:, j, m * 132:m * 132 + 129], PT[:, pi, j * 128:(j + 1) * 128], Vh[:, kb, 0:129],
                                          start=st_flag, stop=last, skip=True))
                        p.op("pe", fns, reads=[("PT", pi, j) for j in range(jmin, 4)] + [("Vh", hb), "Vh1"], writes=[("ACC", j) for j in range(jmin, 4)])

                    LOOK = 2
                    pend = []
                    for s_i, step in enumerate(steps):
                        pend.append((step, qk(step)))
                        if len(pend) > LOOK:
                            st_, i_ = pend.pop(0)
                            softmax_pv(st_, i_)
                        step_ctr[0] += 1
                        if cstream is not None and step_ctr[0] % conv_every == 0:
                            cstream.step()
                    while pend:
                        st_, i_ = pend.pop(0)
                        softmax_pv(st_, i_)

                    if BSTOP in (2, 3, 4):
                        continue
                    lv = ACC[:, :, 128:264:132]
                    p.op("dve", RCP(RR[:, g], lv), writes=[("ACC", j) for j in range(4)] + [("RR", g)])
                    p.op("dve", TS(RL[:, g, :], RR[:, g, :, 1], NLAM[:, 0:1], None, ALU.mult), reads=[("RR", g), "NLAM"], writes=[("RL", g)])
                    for j in range(4):
                        o = j % 2
                        p.op("dve", TS(OO[:, o, :], ACC[:, j, 0:128], RR[:, g, j, 0:1], None, ALU.mult), reads=[("RR", g)], writes=[("ACC", j), ("OO", o)])
                        p.op("dve", STT(OO[:, o, :], ACC[:, j, 132:260], RL[:, g, j:j + 1], OO[:, o, :], ALU.mult, ALU.add),
                             reads=[("RL", g), ("OO", o)], writes=[("ACC", j), ("OO", o)])
                        p.op("dve", lambda e, o=o, g=g, j=j: e.scalar_tensor_tensor(out=JUNK[:], in0=OO[:, o, :], scalar=1.0, in1=OO[:, o, :], op0=ALU.mult, op1=ALU.mult,
                                                                                     accum_out=SS[:, g, j:j + 1]),
                             reads=[("OO", o)], writes=["JUNK", ("SS", g, j)])
                        p.op("act", ACT(SD[:, g, j:j + 1], SS[:, g, j:j + 1], AF.Ln, bias=EPSN[:], scale=1.0 / 128.0),
                             reads=[("SS", g, j), "EPSN"], writes=[("SD", g, j)])
                        p.op("act", ACT(RS[:, g, j:j + 1], SD[:, g, j:j + 1], AF.Exp, scale=-0.5), reads=[("SD", g, j)], writes=[("RS", g, j)])
                        p.op("dve", STT(YT[:, o, :], OO[:, o, :], RS[:, g, j:j + 1], SG[:], ALU.mult, ALU.mult),
                             reads=[("OO", o), ("RS", g, j), "SG"], writes=[("YT", o)])
                        p.op("pe", MM(TRB[:, j * 128:(j + 1) * 128], YT[:, o, :], IDB[:]), reads=[("YT", o), "IDB"], writes=["TRB"])
                    p.op("act", ACT(YO[:, g, :], TRB[:, 0:512], AF.Copy), writes=["TRB", ("YO", g)])
                    p.dma("sp", CATv[h, :, t0:t0 + 512], YO[:, g, :], reads=[("YO", g)], lane=("YO", g))
            if cstream is not None:
                cstream.flush()
            p.emit()

    def phase_C(l):
        with ExitStack() as es:
            sb = lambda n, *a: es.enter_context(nc.sbuf_tensor(_uniq(n), *a))
            pst = lambda n, *a: es.enter_context(nc.psum_tensor(_uniq(n), *a))
            RQ = sb("RQ", [128, 2, 512], BF16)
            RK = sb("RK", [128, 2, 512], BF16)
            QD = sb("QD", [128, 2, 512], BF16)
            RVt = sb("RVt", [128, 2, 4, 128], BF16)
            RGt = sb("RGt", [128, 2, 4, 128], F32)
            MASKT = sb("MASKT", [128, 4, 128], F32)
            QDEC = sb("QDEC", [128, 2, 512], F32)
            KDEC = sb("KDEC", [128, 4, 64], F32)
            CDt = sb("CDt", [128, 2], F32)
            BDM = sb("BDM", [128, 128], F32)
            GN = sb("GN", [128, 256], F32)
            IDB = sb("IDB2", [128, 128], BF16)
            IDF = sb("IDF2", [128, 128], F32)
            IND = sb("IND", [128, 2, 2, 128], BF16)
            KD = sb("KD", [128, 2, 128], BF16)
            SBD = sb("SBD", [128, 2, 128], F32)
            SBDb = sb("SBDb", [128, 2, 128], BF16)
            TMPU = sb("TMPU", [128, 128], F32)
            YSQ = sb("YSQ", [128, 512], F32)
            YS = sb("YS", [128, 512], F32)
            SM = sb("SM", [128, 8], F32)
            SQ = sb("SQ", [128, 8], F32)
            MN = sb("MN", [128, 8], F32)
            VR = sb("VR", [128, 8], F32)
            SDv = sb("SDv", [128, 8], F32)
            RSv = sb("RSv", [128, 8], F32)
            EPS2 = sb("EPS2", [128, 1], F32)
            YR = sb("YR", [128, 4, 128], BF16)
            YRO = sb("YRO", [128, 2, 512], BF16)
            INP0 = pst("INP0", [128, 512], F32)
            INP1 = pst("INP1", [128, 512], F32)
            INPS = [INP0, INP1]
            TKP = pst("TKP", [128, 512], F32)
            YP0 = pst("YP0", [128, 512], F32)
            YP1 = pst("YP1", [128, 512], F32)
            UP = pst("UP", [128, 512], F32)
            TRC = pst("TRC", [128, 512], F32)
            YPS = [YP0, YP1]
            p.dma("sp", MASKT[:], maskt_d, writes=["MASKT"], lane="MASKT")
            p.dma("sp", QDEC[:], qdec_d, writes=["QDEC"], lane="QDEC")
            p.dma("sp", KDEC[:], kdec_d, writes=["KDEC"], lane="KDEC")
            p.dma("sp", CDt[:], cd_d, writes=["CDt"], lane="CDt")
            p.dma("sp", BDM[:], bdmask_d, writes=["BDM"], lane="BDM")
            p.dma("sp", GN[:], gn_rep[l], writes=["GN"], lane="GN")
            p.dma("sp", IDF[:], ident_d, writes=["IDF"], lane="IDF")
            p.op("dve", CP(IDB[:], IDF[:]), reads=["IDF"], writes=["IDB"])
            p.op("dve", MSET(EPS2[:], NORM_EPS), writes=["EPS2"])
            RQTv = RQT.rearrange("(c p) s -> c p s", p=128)
            RKTv = RKT.rearrange("(c p) s -> c p s", p=128)
            RVv = RV.rearrange("(n p) f -> p n f", p=128)
            RGv = RG.rearrange("(n p) f -> p n f", p=128)
            CATv = CATT.rearrange("(c p) s -> c p s", p=128)
            bi = [0]
            ci = [0]
            for c in range(2):
                p.op("dve", MSET(SBD[:, 0, :], 0.0), writes=[("SBD", 0)])
                p.op("dve", MSET(SBDb[:, 0, :], 0.0), writes=[("SBDb", 0)])
                cur = 0
                for tb in range(NT5):
                    b = bi[0] % 2
                    bi[0] += 1
                    t0 = tb * 512
                    p.dma("sp", RQ[:, b, :], RQTv[c, :, t0:t0 + 512], writes=[("RQ", b)], lane=("RQ", b))
                    p.dma("sp", RK[:, b, :], RKTv[c, :, t0:t0 + 512], writes=[("RK", b)], lane=("RK", b))
                    p.dma("sp", RVt[:, b], RVv[:, tb * 4:(tb + 1) * 4, c * 128:(c + 1) * 128], writes=[("RVt", b)], lane=("RVt", b))
                    p.dma("sp", RGt[:, b], RGv[:, tb * 4:(tb + 1) * 4, c * 128:(c + 1) * 128], writes=[("RGt", b)], lane=("RGt", b))
                    p.op("pool", TT(QD[:, b, :], RQ[:, b, :], QDEC[:, c, :], ALU.mult), reads=[("RQ", b), "QDEC"], writes=[("QD", b)])
                    yp = YPS[b]
                    state = {"cur": cur}

                    def S1(j):
                        cc = j % 2
                        js = slice(j * 128, (j + 1) * 128)
                        for hh in range(2):
                            p.op("pe", MM(INPS[hh][:, cc * 128:(cc + 1) * 128], RK[64 * hh:64 * hh + 64, b, js], RQ[64 * hh:64 * hh + 64, b, js]),
                                 reads=[("RK", b), ("RQ", b)], writes=[("INP", hh)])
                        p.op("pe", MM(TKP[:, cc * 128:(cc + 1) * 128], RK[:, b, js], IDB[:]), reads=[("RK", b), "IDB"], writes=["TKP"])

                    def S2(j):
                        cc = j % 2
                        for hh in range(2):
                            p.op("dve", TT(IND[:, cc, hh, :], INPS[hh][:, cc * 128:(cc + 1) * 128], MASKT[:, 2 * c + hh, :], ALU.mult),
                                 reads=["MASKT"], writes=[("INP", hh), ("IND", cc, hh)])
                        p.op("dve", TT(KD[:, cc, :], TKP[:, cc * 128:(cc + 1) * 128], KDEC[:, 2 * c:2 * c + 2, :].rearrange("p a b -> p (a b)"), ALU.mult),
                             reads=["KDEC"], writes=["TKP", ("KD", cc)])

                    def S3(j):
                        cc = j % 2
                        js = slice(j * 128, (j + 1) * 128)
                        cur_ = state["cur"]
                        fns = [MM(yp[:, js], QD[:, b, js], SBDb[:, cur_, :], start=True, stop=False)]
                        for hh in range(2):
                            fns.append(MM(yp[:, j * 128 + 64 * hh: j * 128 + 64 * hh + 64], IND[:, cc, hh, :], RVt[:, b, j, 64 * hh:64 * hh + 64],
                                          start=False, stop=(hh == 1)))
                        p.op("pe", fns, reads=[("QD", b), ("SBDb", cur_), ("IND", cc, 0), ("IND", cc, 1), ("RVt", b)], writes=[("YPB", b)])
                        p.op("pe", MM(UP[:, cc * 128:(cc + 1) * 128], KD[:, cc, :], RVt[:, b, j, :]), reads=[("KD", cc), ("RVt", b)], writes=["UPB"])

                    def S4(j):
                        cc = j % 2
                        cur_ = state["cur"]
                        nxt = 1 - cur_
                        p.op("dve", TT(TMPU[:], UP[:, cc * 128:(cc + 1) * 128], BDM[:], ALU.mult), reads=["BDM"], writes=["UPB", "TMPU"])
                        p.op("dve", STT(SBD[:, nxt, :], SBD[:, cur_, :], CDt[:, c:c + 1], TMPU[:], ALU.mult, ALU.add),
                             reads=[("SBD", cur_), "CDt", "TMPU"], writes=[("SBD", nxt)])
                        p.op("act", ACT(SBDb[:, nxt, :], SBD[:, nxt, :], AF.Copy), reads=[("SBD", nxt)], writes=[("SBDb", nxt)])
                        state["cur"] = nxt

                    for n_ in range(6):
                        if n_ < 4:
                            S1(n_)
                        if 0 <= n_ - 1 < 4:
                            S2(n_ - 1)
                        if 0 <= n_ - 2 < 4:
                            S3(n_ - 2)
                            S4(n_ - 2)
                    cur = state["cur"]
                    YK = [("YPB", b)]
                    y3 = yp[:].rearrange("p (g e) -> p g e", e=64)
                    p.op("dve", RED(SM[:], y3, ALU.add), writes=YK + ["SM"])
                    p.op("act", ACT(YSQ[:], yp[:], AF.Square), writes=YK + ["YSQ"])
                    p.op("dve", RED(SQ[:], YSQ[:].rearrange("p (g e) -> p g e", e=64), ALU.add), reads=["YSQ"], writes=["SQ"])
                    p.op("dve", TS(MN[:], SM[:], 1.0 / 64.0, None, ALU.mult), reads=["SM"], writes=["MN"])
                    p.op("dve", TT(VR[:], MN[:], MN[:], ALU.mult), reads=["MN"], writes=["VR"])
                    p.op("dve", STT(VR[:], SQ[:], 1.0 / 64.0, VR[:], ALU.mult, ALU.subtract), reads=["SQ", "VR"], writes=["VR"])
                    p.op("act", ACT(SDv[:], VR[:], AF.Sqrt, bias=EPS2[:], scale=1.0), reads=["VR", "EPS2"], writes=["SDv"])
                    p.op("dve", RCP(RSv[:], SDv[:]), reads=["SDv"], writes=["RSv"])
                    ys3 = YS[:].rearrange("p (g e) -> p g e", e=64)
                    p.op("dve", TT(ys3, y3, MN[:].unsqueeze(2).to_broadcast([128, 8, 64]), ALU.subtract), reads=["MN"], writes=YK + ["YS"])
                    p.op("dve", TT(ys3, ys3, RSv[:].unsqueeze(2).to_broadcast([128, 8, 64]), ALU.mult), reads=["YS", "RSv"], writes=["YS"])
                    ys4 = YS[:].rearrange("p (j f) -> p j f", f=128)
                    p.op("pool", TT(ys4, ys4, GN[:, c * 128:(c + 1) * 128].unsqueeze(1).to_broadcast([128, 4, 128]), ALU.mult), reads=["YS", "GN"], writes=["YS"])
                    p.op("pool", TT(YR[:], ys4, RGt[:, b], ALU.mult), reads=["YS", ("RGt", b)], writes=["YR"])
                    fns = [MM(TRC[:, j * 128:(j + 1) * 128], YR[:, j, :], IDB[:]) for j in range(4)]
                    p.op("pe", fns, reads=["YR", "IDB"], writes=["TRC"])
                    p.op("act", ACT(YRO[:, b, :], TRC[:, 0:512], AF.Copy), writes=["TRC", ("YRO", b)])
                    p.dma("sp", CATv[6 + c, :, t0:t0 + 512], YRO[:, b, :], reads=[("YRO", b)], lane=("YRO", b))
            p.emit()

    def phase_D(l, xin, xout, moe):
        T = 1024
        NTT = T // 128
        NE = N_EXPERTS if moe else 1
        NFC = NFC_M if moe else NFC_D
        WGU = WGU_M if moe else WGU_D
        WDs = WD_M if moe else WD_D
        if moe:
            groups = [(0, 7), (7, 14), (14, 21), (21, 28)]
        else:
            groups = [(0, 6), (6, 12), (12, 17), (17, 22)]
        GMAX = 7
        with ExitStack() as es:
            sb = lambda n, *a: es.enter_context(nc.sbuf_tensor(_uniq(n), *a))
            pst = lambda n, *a: es.enter_context(nc.psum_tensor(_uniq(n), *a))
            WOUT = sb("WOUT", [128, 8, D], BF16)
            WO32 = sb("WO32", [128, D], F32)
            LNP = sb("LNP", [128, 4, D], F32)
            IDF = sb("IDF3", [128, 128], F32)
            X1 = sb("X1", [128, NTT, D], F32)
            X1T = sb("X1T", [128, 8, T], BF16)
            X1TF = sb("X1TF", [128, 2, 8, 16], F32)
            RW = sb("RW", [128, 8, N_EXPERTS], F32)
            HT = sb("HT", [128, GMAX, T], BF16)
            WD = sb("WD", [128, GMAX, D], BF16)
            WGUr = sb("WGUr", [128, 3, 2048], BF16)
            CTt = sb("CTt", [128, 4, 8, 128], BF16)
            Xt = sb("Xt", [128, 4, D], F32)
            Yt = sb("Yt", [128, 4, D], F32)
            XN = sb("XN", [128, 4, D], F32)
            BST = sb("BST", [128, 4, 12], F32)
            MV = sb("MV", [128, 4, 2], F32)
            SDl = sb("SDl", [128, 4], F32)
            RSl = sb("RSl", [128, 4], F32)
            EPSL = sb("EPSL", [128, 1], F32)
            SGt = sb("SGt", [128, 2, 512], F32)
            LGS = sb("LGS", [128, 8], F32)
            M1 = sb("M1", [128, 4], F32)
            EQ1 = sb("EQ1", [128, 8], F32)
            EQ2 = sb("EQ2", [128, 8], F32)
            LG2 = sb("LG2", [128, 8], F32)
            GT = sb("GT", [128, NTT, 8], F32)
            OUTt = sb("OUTt", [128, 2, D], F32)
            G0 = pst("G0", [128, 512], F32)
            G1 = pst("G1", [128, 512], F32)
            U0 = pst("U0", [128, 512], F32)
            U1 = pst("U1", [128, 512], F32)
            O0 = pst("O0", [128, 512], F32)
            O1 = pst("O1", [128, 512], F32)
            TR0 = pst("TR0", [128, 512], F32)
            TR1 = pst("TR1", [128, 512], F32)
            GP = [G0, G1]
            UPp = [U0, U1]
            OP = [O0, O1]
            TRp = [TR0, TR1]
            p.dma("sp", IDF[:], ident_d, writes=["IDF"], lane="IDF")
            p.dma("sp", LNP[:], ln_rep[l].rearrange("a p d -> p a d"), writes=["LNP"], lane="LNP")
            p.op("dve", MSET(EPSL[:], LN_EPS), writes=["EPSL"])
            for kc in range(8):
                p.dma("sp", WO32[:], w_out[l, kc * 128:(kc + 1) * 128, :], writes=["WO32"], lane="WO32")
                cast_op(p, kc, WOUT[:, kc, :], WO32[:], reads=["WO32"], writes=[("WOUT", kc)])
            WOK = [("WOUT", kc) for kc in range(8)]
            if moe:
                p.dma("sp", RW[:], router_w.rearrange("(c p) e -> p c e", p=128), writes=["RW"], lane="RW")
            xv = xin.rearrange("(n p) d -> n p d", p=128)
            ov = xout.rearrange("(n p) d -> n p d", p=128)
            CATv = CATT.rearrange("(c p) s -> p c s", p=128)
            oi = [0]
            ri = [0]
            gui = [0]

            def layer_norm(src, key_src, dst, key_dst, gi_, bi_, s, part=0):
                if part in (0, 1):
                    p.op("dve", lambda e: e.bn_stats(out=BST[:, s, 0:6], in_=src[:, 0:512]), reads=[key_src], writes=[("BST", s, 0)])
                    p.op("dve", lambda e: e.bn_stats(out=BST[:, s, 6:12], in_=src[:, 512:1024]), reads=[key_src], writes=[("BST", s, 1)])
                    p.op("dve", lambda e: e.bn_aggr(out=MV[:, s, :], in_=BST[:, s, :]), reads=[("BST", s, 0), ("BST", s, 1)], writes=[("MV", s)])
                    p.op("act", ACT(SDl[:, s:s + 1], MV[:, s, 1:2], AF.Ln, bias=EPSL[:], scale=1.0), reads=[("MV", s), "EPSL"], writes=[("SDl", s)])
                    p.op("act", ACT(RSl[:, s:s + 1], SDl[:, s:s + 1], AF.Exp, scale=-0.5), reads=[("SDl", s)], writes=[("RSl", s)])
                if part == 1:
                    return
                p.op("dve", TS(XN[:, s, :], src, MV[:, s, 0:1], RSl[:, s:s + 1], ALU.subtract, ALU.mult),
                     reads=[key_src, ("MV", s), ("RSl", s)], writes=[("XN", s)])
                p.op("dve", TT(XN[:, s, :], XN[:, s, :], LNP[:, gi_, :], ALU.mult), reads=[("XN", s), "LNP"], writes=[("XN", s)])
                p.op("dve", TT(dst, XN[:, s, :], LNP[:, bi_, :], ALU.add), reads=[("XN", s), "LNP"], writes=[key_dst])

            for st in range(S // T):
                tb = st * T
                def d1L(i):
                    s = i % 4
                    n = st * NTT + i
                    p.dma("sp", CTt[:, s], CATv[:, :, tb + i * 128: tb + (i + 1) * 128], writes=[("CTt", s)], lane=("CTt", s))
                    p.dma("sp", Xt[:, s, :], xv[n], writes=[("Xt", s)], lane=("Xt", s))

                def d1A(i):
                    s = i % 4
                    for half in range(2):
                        o = oi[0] % 2
                        oi[0] += 1
                        fns = [MM(OP[o][:], CTt[:, s, kc, :], WOUT[:, kc, half * 512:(half + 1) * 512], start=(kc == 0), stop=(kc == 7)) for kc in range(8)]
                        p.op("pe", fns, reads=[("CTt", s)] + WOK, writes=[("OP", o)])
                        p.op("dve", STT(Yt[:, s, half * 512:(half + 1) * 512], Xt[:, s, half * 512:(half + 1) * 512], ALPHA, OP[o][:], ALU.mult, ALU.add),
                             reads=[("Xt", s)], writes=[("OP", o), ("Yt", s)])

                def d1C(i):
                    for q4 in range(2):
                        r = ri[0] % 2
                        ri[0] += 1
                        fns = [TR(TRp[r][:, k4 * 128:(k4 + 1) * 128], X1[:, i, (q4 * 4 + k4) * 128:(q4 * 4 + k4 + 1) * 128], IDF[:]) for k4 in range(4)]
                        p.op("pe", fns, reads=[("X1", i), "IDF"], writes=[("TRp", r)])
                        src = TRp[r][:].rearrange("p (k t) -> p k t", t=128)
                        p.op("act", ACT(X1T[:, q4 * 4:(q4 + 1) * 4, i * 128:(i + 1) * 128], src, AF.Copy), writes=[("TRp", r), ("X1T", i, q4)])
                    p.op("act", ACT(X1[:, i, :], X1[:, i, :], AF.Copy, scale=ALPHA), reads=[("X1", i)], writes=[("X1", i)])

                if not moe:
                    d1L(0)
                    d1L(1)
                    for n_ in range(NTT + 3):
                        if n_ + 2 < NTT:
                            d1L(n_ + 2)
                        if n_ < NTT:
                            d1A(n_)
                        if 0 <= n_ - 1 < NTT:
                            i_ = n_ - 1
                            layer_norm(Yt[:, i_ % 4, :], ("Yt", i_ % 4), X1[:, i_, :], ("X1", i_), 0, 1, i_ % 4, part=1)
                        if 0 <= n_ - 2 < NTT:
                            i_ = n_ - 2
                            layer_norm(Yt[:, i_ % 4, :], ("Yt", i_ % 4), X1[:, i_, :], ("X1", i_), 0, 1, i_ % 4, part=2)
                        if 0 <= n_ - 3 < NTT:
                            d1C(n_ - 3)
                for i in (range(NTT) if moe else []):
                    s = i % 2
                    n = st * NTT + i
                    p.dma("sp", CTt[:, s], CATv[:, :, tb + i * 128: tb + (i + 1) * 128], writes=[("CTt", s)], lane=("CTt", s))
                    p.dma("sp", Xt[:, s, :], xv[n], writes=[("Xt", s)], lane=("Xt", s))
                    for half in range(2):
                        o = oi[0] % 2
                        oi[0] += 1
                        fns = [MM(OP[o][:], CTt[:, s, kc, :], WOUT[:, kc, half * 512:(half + 1) * 512], start=(kc == 0), stop=(kc == 7)) for kc in range(8)]
                        p.op("pe", fns, reads=[("CTt", s)] + WOK, writes=[("OP", o)])
                        p.op("dve", STT(Yt[:, s, half * 512:(half + 1) * 512], Xt[:, s, half * 512:(half + 1) * 512], ALPHA, OP[o][:], ALU.mult, ALU.add),
                             reads=[("Xt", s)], writes=[("OP", o), ("Yt", s)])
                    layer_norm(Yt[:, s, :], ("Yt", s), X1[:, i, :], ("X1", i), 0, 1, s)
                    for q4 in range(2):
                        r = ri[0] % 2
                        ri[0] += 1
                        fns = [TR(TRp[r][:, k4 * 128:(k4 + 1) * 128], X1[:, i, (q4 * 4 + k4) * 128:(q4 * 4 + k4 + 1) * 128], IDF[:]) for k4 in range(4)]
                        p.op("pe", fns, reads=[("X1", i), "IDF"], writes=[("TRp", r)])
                        src = TRp[r][:].rearrange("p (k t) -> p k t", t=128)
                        p.op("act", ACT(X1T[:, q4 * 4:(q4 + 1) * 4, i * 128:(i + 1) * 128], src, AF.Copy), writes=[("TRp", r), ("X1T", i, q4)])
                        if moe:
                            p.op("dve", CP(X1TF[:, s, q4 * 4:(q4 + 1) * 4, :], src), writes=[("TRp", r), ("X1TF", s, q4)])
                    if moe:
                        o = oi[0] % 2
                        oi[0] += 1
                        fns = [MM(OP[o][:, 0:8], X1TF[:, s, kc, :], RW[:, kc, :], start=(kc == 0), stop=(kc == 7)) for kc in range(8)]
                        p.op("pe", fns, reads=[("X1TF", s, 0), ("X1TF", s, 1), "RW"], writes=[("OP", o)])
                        p.op("dve", CP(LGS[:], OP[o][:, 0:8]), writes=[("OP", o), "LGS"])
                        p.op("dve", RED(M1[:, 0:1], LGS[:], ALU.max), reads=["LGS"], writes=["M1a"])
                        p.op("dve", TS(EQ1[:], LGS[:], M1[:, 0:1], None, ALU.is_equal), reads=["LGS", "M1a"], writes=["EQ1"])
                        p.op("dve", STT(LG2[:], EQ1[:], -1e30, LGS[:], ALU.mult, ALU.add), reads=["EQ1", "LGS"], writes=["LG2"])
                        p.op("dve", RED(M1[:, 1:2], LG2[:], ALU.max), reads=["LG2"], writes=["M1b"])
                        p.op("dve", TS(EQ2[:], LG2[:], M1[:, 1:2], None, ALU.is_equal), reads=["LG2", "M1b"], writes=["EQ2"])
                        p.op("dve", TT(M1[:, 2:3], M1[:, 1:2], M1[:, 0:1], ALU.subtract), reads=["M1a", "M1b"], writes=["M1c"])
                        p.op("act", ACT(M1[:, 2:3], M1[:, 2:3], AF.Sigmoid), reads=["M1c"], writes=["M1c"])
                        p.op("dve", TS(M1[:, 3:4], M1[:, 2:3], -1.0, 1.0, ALU.mult, ALU.add), reads=["M1c"], writes=["M1d"])
                        p.op("dve", TS(EQ1[:], EQ1[:], M1[:, 3:4], None, ALU.mult), reads=["EQ1", "M1d"], writes=["EQ1"])
                        p.op("dve", STT(GT[:, i, :], EQ2[:], M1[:, 2:3], EQ1[:], ALU.mult, ALU.add), reads=["EQ2", "M1c", "EQ1"], writes=[("GT", i)])
                    p.op("act", ACT(X1[:, i, :], X1[:, i, :], AF.Copy, scale=ALPHA), reads=[("X1", i)], writes=[("X1", i)])
                X1TK = [("X1T", i, q4) for i in range(NTT) for q4 in range(2)]
                for e in range(NE):
                    for (f0, f1) in groups:
                        ng = f1 - f0
                        for fl in range(ng):
                            fc = f0 + fl
                            r3 = gui[0] % 3
                            gui[0] += 1
                            p.dma("sp", WGUr[:, r3, :], WGU[e * NFC + fc], writes=[("WGUr", r3)], lane=("WGUr", r3))
                            if fl == 1:
                                p.dma("sp", WD[:, 0:ng, :], WDs[e * NFC + f0: e * NFC + f1].rearrange("c p d -> p c d"), writes=["WD"], lane="WD")
                            wv = WGUr[:, r3, :].rearrange("p (w k j) -> p w k j", w=2, k=8)
                            for th in range(2):
                                gp, up = GP[th], UPp[th]
                                fns = [MM(gp[:], wv[:, 0, kc, :], X1T[:, kc, th * 512:(th + 1) * 512], start=(kc == 0), stop=(kc == 7)) for kc in range(8)]
                                fns += [MM(up[:], wv[:, 1, kc, :], X1T[:, kc, th * 512:(th + 1) * 512], start=(kc == 0), stop=(kc == 7)) for kc in range(8)]
                                p.op("pe", fns, reads=[("WGUr", r3)] + X1TK, writes=[("GP", th), ("UP", th)])
                                p.op("act", ACT(SGt[:, th, :], gp[:], AF.Silu), writes=[("GP", th), ("SGt", th)])
                                p.op("dve", TT(HT[:, fl, th * 512:(th + 1) * 512], SGt[:, th, :], up[:], ALU.mult),
                                     reads=[("SGt", th)], writes=[("UP", th), ("HT", fl, th)])
                        HTK = [("HT", fl, th) for fl in range(ng) for th in range(2)]
                        for i in range(NTT):
                            for half in range(2):
                                o = oi[0] % 2
                                oi[0] += 1
                                fns = [MM(OP[o][:], HT[:, fl, i * 128:(i + 1) * 128], WD[:, fl, half * 512:(half + 1) * 512], start=(fl == 0), stop=(fl == ng - 1))
                                       for fl in range(ng)]
                                p.op("pe", fns, reads=HTK + ["WD"], writes=[("OP", o)])
                                acc = X1[:, i, half * 512:(half + 1) * 512]
                                sc = GT[:, i, e:e + 1] if moe else 1.0
                                p.op("dve", STT(acc, OP[o][:], sc, acc, ALU.mult, ALU.add), reads=[("X1", i), ("GT", i)], writes=[("OP", o), ("X1", i)])
                for n_ in range(NTT + 1):
                    if n_ < NTT:
                        layer_norm(X1[:, n_, :], ("X1", n_), OUTt[:, n_ % 2, :], ("OUTt", n_ % 2), 2, 3, n_ % 4, part=1)
                    if n_ - 1 >= 0:
                        i = n_ - 1
                        layer_norm(X1[:, i, :], ("X1", i), OUTt[:, i % 2, :], ("OUTt", i % 2), 2, 3, i % 4, part=2)
                        p.dma("sp", ov[st * NTT + i], OUTt[:, i % 2, :], reads=[("OUTt", i % 2)], lane=("OUTt", i % 2))
            p.emit()


    def phase_E(l, xin, xout):
        I32 = mybir.dt.int32
        U32 = mybir.dt.uint32
        NFC = NFC_M
        NTL = S // 128
        xv = xin.rearrange("(n p) d -> n p d", p=128)
        ov = xout.rearrange("(n p) d -> n p d", p=128)
        x1fv = X1F.rearrange("(n p) d -> n p d", p=128)
        x1bv = X1B.rearrange("(n p) d -> n p d", p=128)
        CATv = CATT.rearrange("(c p) s -> p c s", p=128)

        def ln_ops(src, key_src, dst, key_dst, LNP, gi_, bi_, s, BST, MV, SDl, RSl, EPSL, XN, part=0):
            if part in (0, 1):
                p.op("dve", lambda e: e.bn_stats(out=BST[:, s, 0:6], in_=src[:, 0:512]), reads=[key_src], writes=[("BST", s, 0)])
                p.op("dve", lambda e: e.bn_stats(out=BST[:, s, 6:12], in_=src[:, 512:1024]), reads=[key_src], writes=[("BST", s, 1)])
                p.op("dve", lambda e: e.bn_aggr(out=MV[:, s, :], in_=BST[:, s, :]), reads=[("BST", s, 0), ("BST", s, 1)], writes=[("MV", s)])
                p.op("act", ACT(SDl[:, s:s + 1], MV[:, s, 1:2], AF.Ln, bias=EPSL[:], scale=1.0), reads=[("MV", s), "EPSL"], writes=[("SDl", s)])
                p.op("act", ACT(RSl[:, s:s + 1], SDl[:, s:s + 1], AF.Exp, scale=-0.5), reads=[("SDl", s)], writes=[("RSl", s)])
            if part == 1:
                return
            p.op("dve", TS(XN[:, s, :], src, MV[:, s, 0:1], RSl[:, s:s + 1], ALU.subtract, ALU.mult),
                 reads=[key_src, ("MV", s), ("RSl", s)], writes=[("XN", s)])
            p.op("dve", TT(XN[:, s, :], XN[:, s, :], LNP[:, gi_, :], ALU.mult), reads=[("XN", s), "LNP"], writes=[("XN", s)])
            p.op("dve", TT(dst, XN[:, s, :], LNP[:, bi_, :], ALU.add), reads=[("XN", s), "LNP"], writes=[key_dst])

        RT = nc.dram_tensor(_uniq("RTAB"), [128, NTL * 4 + NBLK * 32], I32).ap()

        with ExitStack() as es:
            sb = lambda n, *a: es.enter_context(nc.sbuf_tensor(_uniq(n), *a))
            pst = lambda n, *a: es.enter_context(nc.psum_tensor(_uniq(n), *a))
            WOUT = sb("WOUT", [128, 8, D], BF16)
            WO32 = sb("WO32", [128, D], F32)
            LNP = sb("LNP", [128, 4, D], F32)
            IDF = sb("IDF", [128, 128], F32)
            RW = sb("RW", [128, 8, N_EXPERTS], F32)
            UT32 = sb("UT32", [128, 128], F32)
            UTB = sb("UTB", [128, 128], BF16)
            ONB = sb("ONB", [128, 128], BF16)
            KTH = sb("KTH", [128, 32], F32)
            BSTART = sb("BSTART", [128, NBLK], F32)
            SU = sb("SU", [128, 32], F32)
            CU = sb("CU", [128, 32], F32)
            CTt = sb("CTt", [128, 4, 8, 128], BF16)
            Xt = sb("Xt", [128, 4, D], F32)
            Yt = sb("Yt", [128, 4, D], F32)
            XN = sb("XN", [128, 4, D], F32)
            X1t = sb("X1t", [128, 4, D], F32)
            X1Bt = sb("X1Bt", [128, 4, D], BF16)
            X1TF = sb("X1TF", [128, 4, 8, 128], F32)
            BST = sb("BST", [128, 4, 12], F32)
            MV = sb("MV", [128, 4, 2], F32)
            SDl = sb("SDl", [128, 4], F32)
            RSl = sb("RSl", [128, 4], F32)
            EPSL = sb("EPSL", [128, 1], F32)
            LGSa = sb("LGS", [128, 4, 8], F32)
            LG2a = sb("LG2", [128, 4, 8], F32)
            M1a_ = sb("M1", [128, 4, 4], F32)
            MSKa = sb("MSK", [128, 4, 8], F32)
            MSKB = sb("MSKB", [128, 4, 8], BF16)
            EQ1A = sb("EQ1A", [128, NTL, 8], F32)
            EQ2A = sb("EQ2A", [128, NTL, 8], F32)
            RANKA = sb("RANKA", [128, NTL, 8], F32)
            GA = sb("GA", [128, NTL, 2], F32)
            CARRY = sb("CARRY", [128, 8], F32)
            CMPK = sb("CMPK", [128, 8, 32], F32)
            NBK = sb("NBK", [128, 8], F32)
            PADDED = sb("PADDED", [128, 8], F32)
            PSTART = sb("PSTART", [128, 8], F32)
            PEND = sb("PEND", [128, 8], F32)
            TMPA = sb("TMPA", [128, NTL, 8], F32)
            SLOTF = sb("SLOTF", [128, NTL, 2], F32)
            RTI = sb("RTI", [128, NTL * 4 + NBLK * 32], I32)
            CMPB = sb("CMPB", [128, NBLK, 8], F32)
            EB = sb("EB", [128, NBLK], F32)
            OFFU = sb("OFFU", [128, NBLK, 32], F32)
            O0 = pst("O0", [128, 512], F32)
            O1 = pst("O1", [128, 512], F32)
            TR0 = pst("TR0", [128, 512], F32)
            TR1 = pst("TR1", [128, 512], F32)
            RKP = pst("RKP", [128, 512], F32)
            OP = [O0, O1]
            TRp = [TR0, TR1]
            p.dma("sp", IDF[:], ident_d, writes=["IDF"], lane="IDF")
            p.dma("sp", LNP[:], ln_rep[l].rearrange("a p d -> p a d"), writes=["LNP"], lane="LNP")
            p.dma("sp", RW[:], router_w.rearrange("(c p) e -> p c e", p=128), writes=["RW"], lane="RW")
            p.dma("sp", UT32[:], ut_d, writes=["UT32"], lane="UT32")
            p.dma("sp", KTH[:], kth_d, writes=["KTH"], lane="KTH")
            p.dma("sp", BSTART[:], bstart_d, writes=["BSTART"], lane="BSTART")
            p.dma("sp", SU[:], su_d, writes=["SU"], lane="SU")
            p.dma("sp", CU[:], cu_d, writes=["CU"], lane="CU")
            p.op("dve", CP(UTB[:], UT32[:]), reads=["UT32"], writes=["UTB"])
            p.op("dve", MSET(ONB[:], 1.0), writes=["ONB"])
            p.op("dve", MSET(EPSL[:], LN_EPS), writes=["EPSL"])
            p.op("dve", MSET(CARRY[:], 0.0), writes=["CARRY"])
            for kc in range(8):
                p.dma("sp", WO32[:], w_out[l, kc * 128:(kc + 1) * 128, :], writes=["WO32"], lane="WO32")
                cast_op(p, kc, WOUT[:, kc, :], WO32[:], reads=["WO32"], writes=[("WOUT", kc)])
            WOK = [("WOUT", kc) for kc in range(8)]
            oi = [0]
            ri = [0]
            def stL(i):
                s = i % 4
                p.dma("sp", CTt[:, s], CATv[:, :, i * 128:(i + 1) * 128], writes=[("CTt", s)], lane=("CTt", s))
                p.dma("sp", Xt[:, s, :], xv[i], writes=[("Xt", s)], lane=("Xt", s))

            def stA(i):
                s = i % 4
                for half in range(2):
                    o = oi[0] % 2
                    oi[0] += 1
                    fns = [MM(OP[o][:], CTt[:, s, kc, :], WOUT[:, kc, half * 512:(half + 1) * 512], start=(kc == 0), stop=(kc == 7)) for kc in range(8)]
                    p.op("pe", fns, reads=[("CTt", s)] + WOK, writes=[("OP", o)])
                    p.op("dve", STT(Yt[:, s, half * 512:(half + 1) * 512], Xt[:, s, half * 512:(half + 1) * 512], ALPHA, OP[o][:], ALU.mult, ALU.add),
                         reads=[("Xt", s)], writes=[("OP", o), ("Yt", s)])

            def stB1(i):
                s = i % 4
                ln_ops(Yt[:, s, :], ("Yt", s), X1t[:, s, :], ("X1t", s), LNP, 0, 1, s, BST, MV, SDl, RSl, EPSL, XN, part=1)

            def stB(i):
                s = i % 4
                ln_ops(Yt[:, s, :], ("Yt", s), X1t[:, s, :], ("X1t", s), LNP, 0, 1, s, BST, MV, SDl, RSl, EPSL, XN, part=2)
                p.dma("sp", x1fv[i], X1t[:, s, :], reads=[("X1t", s)], lane=("X1tf", s))
                p.op("act", ACT(X1Bt[:, s, :], X1t[:, s, :], AF.Copy), reads=[("X1t", s)], writes=[("X1Bt", s)])
                p.dma("sp", x1bv[i], X1Bt[:, s, :], reads=[("X1Bt", s)], lane=("X1Bt", s))

            def stC(i):
                s = i % 4
                for q4 in range(2):
                    r = ri[0] % 2
                    ri[0] += 1
                    fns = [TR(TRp[r][:, k4 * 128:(k4 + 1) * 128], X1t[:, s, (q4 * 4 + k4) * 128:(q4 * 4 + k4 + 1) * 128], IDF[:]) for k4 in range(4)]
                    p.op("pe", fns, reads=[("X1t", s), "IDF"], writes=[("TRp", r)])
                    src = TRp[r][:].rearrange("p (k t) -> p k t", t=128)
                    p.op("dve", CP(X1TF[:, s, q4 * 4:(q4 + 1) * 4, :], src), writes=[("TRp", r), ("X1TF", s, q4)])
                o = oi[0] % 2
                oi[0] += 1
                fns = [MM(OP[o][:, 0:8], X1TF[:, s, kc, :], RW[:, kc, :], start=(kc == 0), stop=(kc == 7)) for kc in range(8)]
                p.op("pe", fns, reads=[("X1TF", s, 0), ("X1TF", s, 1), "RW"], writes=[("OP", o)])
                p.op("dve", CP(LGSa[:, s, :], OP[o][:, 0:8]), writes=[("OP", o), ("LGS", s)])

            def stD(i):
                s = i % 4
                LGS = LGSa[:, s, :]
                LG2 = LG2a[:, s, :]
                M1 = M1a_[:, s, :]
                MSK = MSKa[:, s, :]
                p.op("dve", RED(M1[:, 0:1], LGS, ALU.max), reads=[("LGS", s)], writes=[("M1a", s)])
                p.op("dve", TS(EQ1A[:, i, :], LGS, M1[:, 0:1], None, ALU.is_equal), reads=[("LGS", s), ("M1a", s)], writes=[("EQ1", i)])
                p.op("dve", STT(LG2, EQ1A[:, i, :], -1e30, LGS, ALU.mult, ALU.add), reads=[("EQ1", i), ("LGS", s)], writes=[("LG2", s)])
                p.op("dve", RED(M1[:, 1:2], LG2, ALU.max), reads=[("LG2", s)], writes=[("M1b", s)])
                p.op("dve", TS(EQ2A[:, i, :], LG2, M1[:, 1:2], None, ALU.is_equal), reads=[("LG2", s), ("M1b", s)], writes=[("EQ2", i)])
                p.op("dve", TT(M1[:, 2:3], M1[:, 1:2], M1[:, 0:1], ALU.subtract), reads=[("M1a", s), ("M1b", s)], writes=[("M1c", s)])
                p.op("act", ACT(M1[:, 3:4], M1[:, 2:3], AF.Exp, scale=1.0), reads=[("M1c", s)], writes=[("M1d", s)])

            def stD2(i):
                s = i % 4
                M1 = M1a_[:, s, :]
                MSK = MSKa[:, s, :]
                TQ = LG2a[:, s, 0:1]
                p.op("dve", TS(TQ, M1[:, 3:4], 1.0, None, ALU.add), reads=[("M1d", s), ("LG2", s)], writes=[("LG2", s)])
                p.op("dve", RCP(TQ, TQ), reads=[("LG2", s)], writes=[("LG2", s)])
                p.op("dve", TT(GA[:, i, 1:2], M1[:, 3:4], TQ, ALU.mult), reads=[("M1d", s), ("LG2", s)], writes=[("GA2", i)])
                p.op("dve", TS(GA[:, i, 0:1], GA[:, i, 1:2], -1.0, 1.0, ALU.mult, ALU.add), reads=[("GA2", i)], writes=[("GA1", i)])
                p.op("dve", TT(MSK, EQ1A[:, i, :], EQ2A[:, i, :], ALU.add), reads=[("EQ1", i), ("EQ2", i)], writes=[("MSK", s)])
                p.op("dve", CP(MSKB[:, s, :], MSK), reads=[("MSK", s)], writes=[("MSKB", s)])
                p.op("pe", [MM(RKP[:, 0:8], UTB[:], MSKB[:, s, :]), MM(RKP[:, 8:16], ONB[:], MSKB[:, s, :])], reads=["UTB", "ONB", ("MSKB", s)], writes=["RKP"])

            def stD3(i):
                p.op("dve", TT(RANKA[:, i, :], RKP[:, 0:8], CARRY[:], ALU.add), reads=["CARRY"], writes=["RKP", ("RANK", i)])
                p.op("dve", TT(CARRY[:], RKP[:, 8:16], CARRY[:], ALU.add), reads=["CARRY"], writes=["RKP", "CARRY"])

            rbank = {}
            stL(0)
            stL(1)
            for n in range(NTL + 6):
                if n + 2 < NTL:
                    stL(n + 2)
                if n < NTL:
                    stA(n)
                if 0 <= n - 1 < NTL:
                    stB1(n - 1)
                if 0 <= n - 2 < NTL:
                    stB(n - 2)
                if 0 <= n - 3 < NTL:
                    stC(n - 3)
                if 0 <= n - 4 < NTL:
                    stD(n - 4)
                if 0 <= n - 6 < NTL:
                    stD3(n - 6)
                if 0 <= n - 5 < NTL:
                    stD2(n - 5)
            AK = [("EQ1", i) for i in range(NTL)] + [("EQ2", i) for i in range(NTL)] + [("RANK", i) for i in range(NTL)]
            p.op("dve", TT(CMPK[:], CARRY[:].unsqueeze(2).to_broadcast([128, 8, 32]), KTH[:].unsqueeze(1).to_broadcast([128, 8, 32]), ALU.is_gt),
                 reads=["CARRY", "KTH"], writes=["CMPK"])
            p.op("dve", RED(NBK[:], CMPK[:], ALU.add), reads=["CMPK"], writes=["NBK"])
            p.op("dve", TS(PADDED[:], NBK[:], 512.0, None, ALU.mult), reads=["NBK"], writes=["PADDED"])
            p.op("dve", MSET(PSTART[:, 0:1], 0.0), writes=["PSTART"])
            for e_ in range(1, 8):
                p.op("dve", TT(PSTART[:, e_:e_ + 1], PSTART[:, e_ - 1:e_], PADDED[:, e_ - 1:e_], ALU.add), reads=["PSTART", "PADDED"], writes=["PSTART"])
            p.op("dve", TT(PEND[:], PSTART[:], PADDED[:], ALU.add), reads=["PSTART", "PADDED"], writes=["PEND"])
            p.op("dve", TT(RANKA[:], RANKA[:], PSTART[:].unsqueeze(1).to_broadcast([128, NTL, 8]), ALU.add), reads=AK + ["PSTART"], writes=["DEST"])
            p.op("dve", TT(TMPA[:], RANKA[:], EQ1A[:], ALU.mult), reads=["DEST"] + AK, writes=["TMPA"])
            p.op("dve", RED(SLOTF[:, :, 0], TMPA[:], ALU.add), reads=["TMPA"], writes=["SLOT0"])
            p.op("dve", TT(TMPA[:], RANKA[:], EQ2A[:], ALU.mult), reads=["DEST", "SLOT0"] + AK, writes=["TMPA"])
            p.op("dve", RED(SLOTF[:, :, 1], TMPA[:], ALU.add), reads=["TMPA"], writes=["SLOT1"])
            p.op("dve", CP(RTI[:, 0:NTL * 2], SLOTF[:].rearrange("p a b -> p (a b)")), reads=["SLOT0", "SLOT1"], writes=["RTIa"])
            GK = [("GA1", i) for i in range(NTL)] + [("GA2", i) for i in range(NTL)]
            p.op("dve", CP(RTI[:, NTL * 2:NTL * 4].bitcast(F32), GA[:].rearrange("p a b -> p (a b)")), reads=GK, writes=["RTIb"])
            p.op("dve", TT(CMPB[:], PEND[:].unsqueeze(1).to_broadcast([128, NBLK, 8]), BSTART[:].unsqueeze(2).to_broadcast([128, NBLK, 8]), ALU.is_le),
                 reads=["PEND", "BSTART"], writes=["CMPB"])
            p.op("dve", RED(EB[:], CMPB[:], ALU.add), reads=["CMPB"], writes=["EB"])
            p.op("dve", TS(EB[:], EB[:], 7.0, None, ALU.min), reads=["EB"], writes=["EB"])
            p.op("dve", TT(OFFU[:], EB[:].unsqueeze(2).to_broadcast([128, NBLK, 32]), SU[:].unsqueeze(1).to_broadcast([128, NBLK, 32]), ALU.mult),
                 reads=["EB", "SU"], writes=["OFFU"])
            p.op("dve", TT(OFFU[:], OFFU[:], CU[:].unsqueeze(1).to_broadcast([128, NBLK, 32]), ALU.add), reads=["OFFU", "CU"], writes=["OFFU"])
            p.op("dve", CP(RTI[:, NTL * 4:], OFFU[:].rearrange("p a b -> p (a b)")), reads=["OFFU"], writes=["RTIc"])
            p.dma("sp", RT, RTI[:], reads=["RTIa", "RTIb", "RTIc"], lane="RT")
            p.emit()

        with ExitStack() as es:
            sb = lambda n, *a: es.enter_context(nc.sbuf_tensor(_uniq(n), *a))
            RTI = sb("RTI", [128, NTL * 4 + NBLK * 32], I32)
            ZT = sb("ZT", [128, 8, D], BF16)
            XR = sb("XR", [128, 4, D], BF16)
            p.dma("sp", RTI[:], RT, writes=["RTI"], lane="RTI")
            p.op("dve", MSET(ZT[:], 0.0), writes=["ZT"])
            xbz = XB.rearrange("(a j p) d -> a p j d", p=128, j=8)
            for a in range(NSLOT // 1024):
                p.dma("sp", xbz[a], ZT[:], reads=["ZT"], writes=[("XBz", a)], lane=("XBz", a % 4))
            for i in range(NTL):
                s = i % 4
                p.dma("sp", XR[:, s, :], x1bv[i], writes=[("XR", s)], lane=("XR", s))
                for k in range(2):
                    idx = RTI[:, 2 * i + k: 2 * i + k + 1].bitcast(U32)
                    p.op("pool", lambda e, idx=idx, s=s: e.indirect_dma_start(out=XB, out_offset=bass.IndirectOffsetOnAxis(ap=idx, axis=0), in_=XR[:, s, :], in_offset=None),
                         reads=["RTI", ("XR", s)] + [("XBz", a) for a in range(NSLOT // 1024)], writes=[("XBs", i, k)], lane=("XRs", s, k))
            p.emit()

        with ExitStack() as es:
            sb = lambda n, *a: es.enter_context(nc.sbuf_tensor(_uniq(n), *a))
            pst = lambda n, *a: es.enter_context(nc.psum_tensor(_uniq(n), *a))
            RTI = sb("RTI", [128, NTL * 4 + NBLK * 32], I32)
            IDF = sb("IDF", [128, 128], F32)
            IDB = sb("IDB", [128, 128], BF16)
            XBt = sb("XBt", [128, 2, 4, D], BF16)
            XBT = sb("XBT", [128, 8, 512], BF16)
            HT = sb("HT", [128, NFC, 512], BF16)
            WD = sb("WD", [128, NFC, D], BF16)
            WGUr = sb("WGUr", [128, 4, 2048], BF16)
            SGt = sb("SGt", [128, 2, 512], F32)
            YBt = sb("YBt", [128, 2, 4, D], F32)
            G0 = pst("G0", [128, 512], F32)
            G1 = pst("G1", [128, 512], F32)
            U0 = pst("U0", [128, 512], F32)
            U1 = pst("U1", [128, 512], F32)
            O0 = pst("O0", [128, 512], F32)
            O1 = pst("O1", [128, 512], F32)
            TR0 = pst("TR0", [128, 512], F32)
            TR1 = pst("TR1", [128, 512], F32)
            GP = [G0, G1]
            UPp = [U0, U1]
            OP = [O0, O1]
            TRp = [TR0, TR1]
            p.dma("sp", RTI[:], RT, writes=["RTI"], lane="RTI")
            p.dma("sp", IDF[:], ident_d, writes=["IDF"], lane="IDF")
            p.op("dve", CP(IDB[:], IDF[:]), reads=["IDF"], writes=["IDB"])
            xbv = XB.rearrange("(b j p) d -> b p j d", p=128, j=4)
            ybv = YB.rearrange("(b j p) d -> b p j d", p=128, j=4)
            regs = []
            OFF0 = NTL * 4
            gui = [0]
            oi = [0]
            ri = [0]
            gi2 = [0]
            evi = [0]

            WGUrows = WGU_M.rearrange("c p f -> (c p) f")
            WDrows = WD_M.rearrange("g p c d -> (g p) (c d)")

            def dyn_dma(out_ap, rows_ap, b, slot, reads, writes, lane):
                idx = RTI[:, OFF0 + b * 32 + slot: OFF0 + b * 32 + slot + 1].bitcast(U32)
                p.op("pool", lambda e: e.indirect_dma_start(out=out_ap, out_offset=None, in_=rows_ap, in_offset=bass.IndirectOffsetOnAxis(ap=idx, axis=0)),
                     reads=["RTI"] + list(reads), writes=writes, lane=lane)

            regs_alloc = []
            dyn_cnt = [0]
            for b in range(NBLK):
                s = b % 2
                p.dma("sp", XBt[:, s], xbv[b], writes=[("XBt", s)], lane=("XBt", s))
                for kc in range(8):
                    r = ri[0] % 2
                    ri[0] += 1
                    fns = [MM(TRp[r][:, j * 128:(j + 1) * 128], XBt[:, s, j, kc * 128:(kc + 1) * 128], IDB[:]) for j in range(4)]
                    p.op("pe", fns, reads=[("XBt", s), "IDB"], writes=[("TRp", r)])
                    evi[0] += 1
                    if evi[0] % 2:
                        p.op("act", ACT(XBT[:, kc, :], TRp[r][:], AF.Copy), writes=[("TRp", r), ("XBT", kc)])
                    else:
                        p.op("dve", CP(XBT[:, kc, :], TRp[r][:]), writes=[("TRp", r), ("XBT", kc)])
                XBTK = [("XBT", kc) for kc in range(8)]
                for fc in range(NFC):
                    r4 = gui[0] % 4
                    gui[0] += 1
                    dyn_dma(WGUr[:, r4, :], WGUrows, b, fc, [], [("WGUr", r4)], ("WGUr", r4))
                    if fc % 7 == 1:
                        g7 = fc // 7
                        dyn_dma(WD[:, g7 * 7:(g7 + 1) * 7, :].rearrange("p c d -> p (c d)"), WDrows, b, 28 + g7, [], [("WD", g7)], ("WD", g7))
                    wv = WGUr[:, r4, :].rearrange("p (w k j) -> p w k j", w=2, k=8)
                    th = gi2[0] % 2
                    gi2[0] += 1
                    gp, up = GP[th], UPp[th]
                    fns = [MM(gp[:], wv[:, 0, kc, :], XBT[:, kc, :], start=(kc == 0), stop=(kc == 7)) for kc in range(8)]
                    fns += [MM(up[:], wv[:, 1, kc, :], XBT[:, kc, :], start=(kc == 0), stop=(kc == 7)) for kc in range(8)]
                    p.op("pe", fns, reads=[("WGUr", r4)] + XBTK, writes=[("GP", th), ("UP", th)])
                    p.op("act", ACT(SGt[:, th, :], gp[:], AF.Silu), writes=[("GP", th), ("SGt", th)])
                    p.op("dve", TT(HT[:, fc, :], SGt[:, th, :], up[:], ALU.mult), reads=[("SGt", th)], writes=[("UP", th), ("HT", fc)])
                HTK = [("HT", fc) for fc in range(NFC)]
                WDK = [("WD", g7) for g7 in range(4)]
                for j in range(4):
                    for half in range(2):
                        o = oi[0] % 2
                        oi[0] += 1
                        fns = [MM(OP[o][:], HT[:, fc, j * 128:(j + 1) * 128], WD[:, fc, half * 512:(half + 1) * 512], start=(fc == 0), stop=(fc == NFC - 1)) for fc in range(NFC)]
                        p.op("pe", fns, reads=HTK + WDK, writes=[("OP", o)])
                        dst = YBt[:, s, j, half * 512:(half + 1) * 512]
                        if o == 0:
                            p.op("act", ACT(dst, OP[o][:], AF.Copy), writes=[("OP", o), ("YBt", s, j, half)])
                        else:
                            p.op("dve", CP(dst, OP[o][:]), writes=[("OP", o), ("YBt", s, j, half)])
                p.dma("sp", ybv[b], YBt[:, s], reads=[("YBt", s, j, h2) for j in range(4) for h2 in range(2)], lane=("YBt", s))
            p.emit()

        with ExitStack() as es:
            sb = lambda n, *a: es.enter_context(nc.sbuf_tensor(_uniq(n), *a))
            RTI = sb("RTI", [128, NTL * 4 + NBLK * 32], I32)
            LNP = sb("LNP", [128, 4, D], F32)
            Y12 = sb("Y12", [128, 4, 2, D], F32)
            X1t = sb("X1t", [128, 4, D], F32)
            ACCt = sb("ACCt", [128, 4, D], F32)
            XN = sb("XN", [128, 4, D], F32)
            OUTt = sb("OUTt", [128, 4, D], F32)
            BST = sb("BST", [128, 4, 12], F32)
            MV = sb("MV", [128, 4, 2], F32)
            SDl = sb("SDl", [128, 4], F32)
            RSl = sb("RSl", [128, 4], F32)
            EPSL = sb("EPSL", [128, 1], F32)
            p.dma("sp", RTI[:], RT, writes=["RTI"], lane="RTI")
            p.dma("sp", LNP[:], ln_rep[l].rearrange("a p d -> p a d"), writes=["LNP"], lane="LNP")
            p.op("dve", MSET(EPSL[:], LN_EPS), writes=["EPSL"])
            GAf = RTI[:, NTL * 2:NTL * 4].bitcast(F32)
            def e4_load(i):
                s = i % 4
                p.dma("sp", X1t[:, s, :], x1fv[i], writes=[("X1t", s)], lane=("X1t", s))
                for k in range(2):
                    idx = RTI[:, 2 * i + k: 2 * i + k + 1].bitcast(U32)
                    p.op("pool", lambda e, idx=idx, s=s, k=k: e.indirect_dma_start(out=Y12[:, s, k, :], out_offset=None, in_=YB, in_offset=bass.IndirectOffsetOnAxis(ap=idx, axis=0)),
                         reads=["RTI"], writes=[("Y12", s, k)], lane=("Y12", s, k))

            for n in range(NTL + 2):
                if n < NTL:
                    e4_load(n)
                i = n - 2
                if i < 0:
                    continue
                s = i % 4
                p.op("act", ACT(ACCt[:, s, :], X1t[:, s, :], AF.Copy, scale=ALPHA), reads=[("X1t", s)], writes=[("ACCt", s)])
                for k in range(2):
                    p.op("dve", STT(ACCt[:, s, :], Y12[:, s, k, :], GAf[:, 2 * i + k: 2 * i + k + 1], ACCt[:, s, :], ALU.mult, ALU.add),
                         reads=[("Y12", s, k), "RTI", ("ACCt", s)], writes=[("ACCt", s)])
                ln_ops(ACCt[:, s, :], ("ACCt", s), OUTt[:, s, :], ("OUTt", s), LNP, 2, 3, s, BST, MV, SDl, RSl, EPSL, XN)
                p.dma("sp", ov[i], OUTt[:, s, :], reads=[("OUTt", s)], lane=("OUTt", s))
            p.emit()

    conv_split = [None, None]
    if "0" in PHASES:
        if OVERLAP_CONV and "B" in PHASES:
            units, n_dense = make_conv_units()
            if LAYERS == "01":
                n0 = n_dense + (len(units) - n_dense) // 2
                conv_split = [units[:n0], units[n0:]]
            else:
                conv_split = [units, units]
        else:
            phase_convert()
    for l in range(DEPTH):
        if str(l) not in LAYERS:
            continue
        xin = x_in if l == 0 else X1S
        xout = X1S if l == 0 else out_d
        if "A" in PHASES:
            phase_A(l, xin)
        if "B" in PHASES:
            phase_B(l, conv_split[l])
        if "C" in PHASES:
            phase_C(l)
        if "D" in PHASES:
            if l % 2 == 1 and SPARSE:
                phase_E(l, xin, xout)
            else:
                phase_D(l, xin, xout, moe=(l % 2 == 1))
    p.close()
    return nc


def _t5_bucket(dist):
    n = np.maximum(dist, 0)
    max_exact = 16
    nf = np.maximum(n, 1).astype(np.float32)
    large = max_exact + (np.log(nf / max_exact) / math.log(128 / max_exact) * (32 - max_exact)).astype(np.int32)
    large = np.minimum(large, 31)
    return np.where(n < max_exact, n, large)


def host_constants(S):
    c = {}
    c["ident"] = np.eye(128, dtype=np.float32)
    half = 32
    inv = (10000.0 ** (-np.linspace(0.0, 1.0, half, dtype=np.float32))).astype(np.float32)
    pos = np.arange(S, dtype=np.float32)
    ang = pos[None, :] * inv[:, None]
    fidx = (np.arange(128) % 64) % 32
    c["cos_t"] = np.cos(ang)[fidx].astype(np.float32)
    c["sin_t"] = np.sin(ang)[fidx].astype(np.float32)
    kk = np.arange(128)[:, None]
    qq = np.arange(128)[None, :]
    c["maskneg"] = np.where(qq >= kk, 0.0, NEG).astype(np.float32)
    H = 4
    log_gamma = np.log(1.0 - 2.0 ** (-5.0 - np.arange(H, dtype=np.float64)))
    idx = np.arange(128, dtype=np.float64)
    rel = idx[None, :] - idx[:, None]
    maskt = np.zeros((128, 4, 128), np.float64)
    for h in range(H):
        maskt[:, h, :] = np.where(rel >= 0, np.exp(log_gamma[h] * np.maximum(rel, 0.0)), 0.0) * 0.125
    c["maskt"] = maskt.astype(np.float32)
    qdec = np.zeros((128, 2, 512), np.float64)
    cd = np.zeros((128, 2), np.float64)
    for cpair in range(2):
        for hh in range(2):
            h = 2 * cpair + hh
            qd = np.exp(log_gamma[h] * (idx + 1.0))
            qdec[64 * hh:64 * hh + 64, cpair, :] = np.tile(qd, 4)[None, :]
            cd[64 * hh:64 * hh + 64, cpair] = np.exp(log_gamma[h] * 128.0)
    c["qdec"] = qdec.astype(np.float32)
    c["cd"] = cd.astype(np.float32)
    kdec = np.zeros((128, 4, 64), np.float64)
    for h in range(H):
        kdec[:, h, :] = (np.exp(log_gamma[h] * (127.0 - idx)) * 0.125)[:, None]
    c["kdec"] = kdec.astype(np.float32)
    bd = np.zeros((128, 128), np.float32)
    bd[0:64, 0:64] = 1.0
    bd[64:128, 64:128] = 1.0
    c["bdmask"] = bd
    invc = np.zeros((128, 2, 2, 512), np.float32)
    wins = (2, 4, 8, 16)
    t = np.arange(512)
    for g, w in enumerate(wins):
        cch, hh = g // 2, g % 2
        invc[64 * hh:64 * hh + 64, 0, cch, :] = (1.0 / np.minimum(t + 1, w).astype(np.float32))[None, :]
        invc[64 * hh:64 * hh + 64, 1, cch, :] = np.float32(1.0 / w)
    c["invc"] = invc
    tp = np.arange(128)
    c["ut"] = (tp[:, None] < tp[None, :]).astype(np.float32)
    c["kth"] = np.broadcast_to((512.0 * np.arange(32, dtype=np.float32))[None, :], (128, 32)).copy()
    nblk = (2 * S + N_EXPERTS * 512) // 512
    c["bstart"] = np.broadcast_to((512.0 * np.arange(nblk, dtype=np.float32))[None, :], (128, nblk)).copy()
    su = np.zeros(32, np.float32)
    cu = np.zeros((128, 32), np.float32)
    su[0:28] = 28.0 * 128.0
    cu[:, 0:28] = 128.0 * np.arange(28)[None, :] + np.arange(128)[:, None]
    su[28:32] = 4.0 * 128.0
    cu[:, 28:32] = 128.0 * np.arange(4)[None, :] + np.arange(128)[:, None]
    c["su"] = np.broadcast_to(su[None, :], (128, 32)).copy()
    c["cu"] = cu
    return c


def host_layout(inputs, S):
    f = lambda a: np.ascontiguousarray(np.asarray(a, dtype=np.float32))
    sh = {}
    for k in ("w_in", "w_out", "ffn_w_gate", "ffn_w_up", "ffn_w_down", "pool_w"):
        sh[k] = f(inputs[k])
    sh["router_w"] = f(inputs["router_w"][0])
    sh["moe_w_gate"] = f(inputs["moe_w_gate"][0])
    sh["moe_w_up"] = f(inputs["moe_w_up"][0])
    sh["moe_w_down"] = f(inputs["moe_w_down"][0])
    lam = f(inputs["diff_lambda"]).reshape(DEPTH, 1, 256)
    sh["lam_rep"] = f(np.broadcast_to(lam, (DEPTH, 128, 256)))
    sh["subln_rep"] = f(np.broadcast_to(f(inputs["diff_subln_g"])[:, None, :], (DEPTH, 128, 128)))
    sh["pscale"] = f(f(inputs["pool_scale"]).reshape(DEPTH, 2, 128).transpose(0, 2, 1))
    sh["gn_rep"] = f(np.broadcast_to(f(inputs["ret_gn_g"])[:, None, :], (DEPTH, 128, 256)))
    ln = np.stack([f(inputs["ln1_g"]), f(inputs["ln1_b"]), f(inputs["ln2_g"]), f(inputs["ln2_b"])], axis=1)
    sh["ln_rep"] = f(np.broadcast_to(ln[:, :, None, :], (DEPTH, 4, 128, D)))
    rb = f(inputs["rel_bias"])
    kk = np.arange(128)[:, None]
    qq = np.arange(128)[None, :]
    b0 = _t5_bucket(qq - kk)
    b1 = _t5_bucket(128 + qq - kk)
    bt = np.stack([rb[b0], rb[b1]], axis=1)
    sh["bt"] = f(bt.transpose(0, 1, 3, 2))
    sh["cb"] = f(np.broadcast_to(rb[31][None, :], (128, 8)))
    sh.update(host_constants(S))
    if "0" not in PHASES:
        for k in ("ffn_w_gate", "ffn_w_up", "ffn_w_down", "moe_w_gate", "moe_w_up", "moe_w_down"):
            sh.pop(k)
    return sh


_CACHE = {}


def run(inputs, S, dev=False):
    B = inputs["x"].shape[0]
    key = (S, dev)
    if key not in _CACHE:
        _CACHE[key] = build(S, dev)
    nc = _CACHE[key]
    shared = host_layout(inputs, S)
    x = np.asarray(inputs["x"], dtype=np.float32)
    in_maps = []
    for b in range(B):
        m = dict(shared)
        m["x"] = np.ascontiguousarray(x[b])
        in_maps.append(m)
    res = run_bass_kernel_spmd(nc, in_maps, core_ids=list(range(B)))
    return res


def kernel(**inputs):
    S = inputs["x"].shape[1]
    res = run(inputs, S)
    out = np.stack([np.asarray(r["out"], dtype=np.float32) for r in res.results], axis=0)
    return out
```

```python
import math
from contextlib import ExitStack
import numpy as np
import ml_dtypes
import concourse.bass as bass
import concourse.mybir as mybir
from concourse.bass_utils import run_bass_kernel_spmd

F32 = mybir.dt.float32
BF16 = mybir.dt.bfloat16
AF = mybir.ActivationFunctionType
ALU = mybir.AluOpType
AX = mybir.AxisListType

D = 1024
DEPTH = 2
INW = 2816
INWX = 3328
FFN_DIM = 2816
N_EXPERTS = 8
EXPERT_DIM = 3584
ALPHA = (2 * DEPTH) ** 0.25
LN_EPS = 1e-5
NORM_EPS = 1e-6
NEG = -30000.0
ENGS = ("pe", "act", "dve", "pool", "sp")
PHASES = "0ABCD"
ASTOP = 0
BSTOP = 0
SPARSE = True
OVERLAP_CONV = True
ASUB = ""
LAYERS = "01"


class Prog:
    def __init__(self, nc):
        self.nc = nc
        self.streams = {e: [] for e in ENGS}
        self.cnt = {e: 0 for e in ENGS}
        self.esem = {}
        self.lanes = {}
        self.res = {}
        self.seen = {e: {} for e in ENGS}
        self._sem_ctx = []
        self.free_lanes = []
        self.free_sw = []
        self.nlane = 0

    def _new_sem(self, name):
        ctx = self.nc.semaphore(name)
        s = ctx.__enter__()
        self._sem_ctx.append(ctx)
        return s

    def eng_sem(self, e):
        if e not in self.esem:
            self.esem[e] = self._new_sem("se_" + e)
        return self.esem[e]

    def lane(self, key, sw=False):
        if key not in self.lanes:
            pool = self.free_sw if sw else self.free_lanes
            if pool:
                ent = pool.pop()
            else:
                self.nlane += 1
                ent = [self._new_sem("ln%d" % self.nlane), 0, sw]
            self.lanes[key] = ent
        return self.lanes[key]

    def op(self, eng, fns, reads=(), writes=(), lane=None):
        if callable(fns):
            fns = [fns]
        deps = {}

        def add(tok):
            if tok is None:
                return
            sem, val, peng = tok
            if peng == "pe" and eng == "pe" and lane is None:
                return
            k = id(sem)
            if k not in deps or deps[k][1] < val:
                deps[k] = (sem, val)

        for k in reads:
            st = self.res.get(k)
            if st:
                add(st[0])
        for k in writes:
            st = self.res.get(k)
            if st:
                add(st[0])
                for t in st[1]:
                    add(t)
        waits = []
        seen = self.seen[eng]
        for k, (sem, val) in deps.items():
            if seen.get(k, 0) >= val:
                continue
            seen[k] = val
            waits.append((sem, val))
        if lane is None:
            sem = self.eng_sem(eng)
            self.cnt[eng] += 1
            val = self.cnt[eng]
            inc = 1
            tok = (sem, val, eng)
        else:
            ln = self.lane(lane, sw=(eng == "pool"))
            ln[1] += 16
            sem, val, inc = ln[0], ln[1], 16
            tok = (sem, val, "dma")
        for k in reads:
            st = self.res.setdefault(k, [None, []])
            st[1].append(tok)
        for k in writes:
            self.res[k] = [tok, []]
        self.streams[eng].append((waits, fns, sem, inc))
        return tok

    def dma(self, eng, out, in_, reads=(), writes=(), lane=None):
        assert lane is not None
        return self.op(eng, lambda e: e.dma_start(out=out, in_=in_), reads=reads, writes=writes, lane=lane)

    def barrier(self):
        toks = []
        for e in ENGS:
            if self.cnt[e] > 0:
                toks.append((self.esem[e], self.cnt[e]))
        for k, (sem, c, _sw) in self.lanes.items():
            if c > 0:
                toks.append((sem, c))
        for e in ENGS:
            waits = []
            seen = self.seen[e]
            for sem, val in toks:
                if seen.get(id(sem), 0) >= val:
                    continue
                seen[id(sem)] = val
                waits.append((sem, val))
            if waits:
                self.streams[e].append((waits, [], None, 0))
        self.res = {}
        for ent in self.lanes.values():
            (self.free_sw if ent[2] else self.free_lanes).append(ent)
        self.lanes = {}

    def emit(self):
        nc = self.nc
        self.barrier()
        streams = self.streams
        self.streams = {e: [] for e in ENGS}
        with nc.Block() as block:
            def run(engobj, stream):
                for waits, fns, sem, inc in stream:
                    for s, v in waits:
                        engobj.wait_ge(s, v)
                    ins = None
                    for f in fns:
                        ins = f(engobj)
                    if ins is not None and sem is not None:
                        ins.then_inc(sem, inc)

            @block.tensor
            def _(t):
                run(t, streams["pe"])

            @block.scalar
            def _(t):
                run(t, streams["act"])

            @block.vector
            def _(t):
                run(t, streams["dve"])

            @block.gpsimd
            def _(t):
                run(t, streams["pool"])

            @block.sync
            def _(t):
                run(t, streams["sp"])

    def close(self):
        for ctx in reversed(self._sem_ctx):
            ctx.__exit__(None, None, None)


_UQ = [0]


def _uniq(n):
    _UQ[0] += 1
    return '%s_%d' % (n, _UQ[0])


def MM(out, lhsT, rhs, start=True, stop=True, skip=False):
    if skip:
        return lambda e: e.matmul(out, lhsT=lhsT, rhs=rhs, start=start, stop=stop, skip_group_check=True)
    return lambda e: e.matmul(out, lhsT=lhsT, rhs=rhs, start=start, stop=stop)


def TR(out, in_, ident):
    return lambda e: e.transpose(out=out, in_=in_, identity=ident)


def ACT(out, in_, func, **kw):
    return lambda e: e.activation(out=out, in_=in_, func=func, **kw)


def TT(out, a, b, op):
    return lambda e: e.tensor_tensor(out=out, in0=a, in1=b, op=op)


def TS(out, a, s1, s2, op0, op1=None):
    if op1 is None:
        return lambda e: e.tensor_scalar(out=out, in0=a, scalar1=s1, scalar2=None, op0=op0)
    return lambda e: e.tensor_scalar(out=out, in0=a, scalar1=s1, scalar2=s2, op0=op0, op1=op1)


def STT(out, a, s, b, op0, op1):
    return lambda e: e.scalar_tensor_tensor(out=out, in0=a, scalar=s, in1=b, op0=op0, op1=op1)


def CP(out, in_):
    return lambda e: e.tensor_copy(out=out, in_=in_)


def RED(out, in_, op):
    return lambda e: e.tensor_reduce(out=out, in_=in_, axis=AX.X, op=op)


def RCP(out, in_):
    return lambda e: e.reciprocal(out=out, in_=in_)


def MSET(ap, v):
    return lambda e: e.memset(ap, v)


def cast_op(p, i, out, in_, reads, writes):
    k = i % 3
    if k == 0:
        p.op("dve", CP(out, in_), reads=reads, writes=writes)
    elif k == 1:
        p.op("act", ACT(out, in_, AF.Copy), reads=reads, writes=writes)
    else:
        p.op("pool", CP(out, in_), reads=reads, writes=writes)


def build(S, dev=False):
    assert S % 1024 == 0
    nc = bass.Bass("TRN2", target_bir_lowering=False)
    NB = S // 128
    NT5 = S // 512

    NSLOT = 2 * S + N_EXPERTS * 512
    NBLK = NSLOT // 512

    def din(name, shape, dt=F32):
        return nc.dram_tensor(name, list(shape), dt, kind="ExternalInput").ap()

    def dscr(name, shape, dt):
        if dev:
            return nc.dram_tensor(name, list(shape), dt, kind="ExternalOutput").ap()
        return nc.dram_tensor(name, list(shape), dt).ap()

    x_in = din("x", [S, D])
    w_in = din("w_in", [DEPTH, D, INW])
    w_out = din("w_out", [DEPTH, D, D])
    router_w = din("router_w", [D, N_EXPERTS])
    if "0" in PHASES:
        ffn_wg = din("ffn_w_gate", [1, D, FFN_DIM])
        ffn_wu = din("ffn_w_up", [1, D, FFN_DIM])
        ffn_wd = din("ffn_w_down", [1, FFN_DIM, D])
        moe_wg = din("moe_w_gate", [N_EXPERTS, D, EXPERT_DIM])
        moe_wu = din("moe_w_up", [N_EXPERTS, D, EXPERT_DIM])
        moe_wd = din("moe_w_down", [N_EXPERTS, EXPERT_DIM, D])
    lam_rep = din("lam_rep", [DEPTH, 128, 256])
    subln_rep = din("subln_rep", [DEPTH, 128, 128])
    poolw = din("pool_w", [DEPTH, 4, 64, 64])
    pscale = din("pscale", [DEPTH, 128, 2])
    gn_rep = din("gn_rep", [DEPTH, 128, 256])
    ln_rep = din("ln_rep", [DEPTH, 4, 128, D])
    ident_d = din("ident", [128, 128])
    cos_d = din("cos_t", [128, S])
    sin_d = din("sin_t", [128, S])
    bt_d = din("bt", [128, 2, 8, 128])
    maskneg_d = din("maskneg", [128, 128])
    cb_d = din("cb", [128, 8])
    maskt_d = din("maskt", [128, 4, 128])
    qdec_d = din("qdec", [128, 2, 512])
    kdec_d = din("kdec", [128, 4, 64])
    cd_d = din("cd", [128, 2])
    bdmask_d = din("bdmask", [128, 128])
    invc_d = din("invc", [128, 2, 2, 512])
    ut_d = din("ut", [128, 128])
    kth_d = din("kth", [128, 32])
    bstart_d = din("bstart", [128, NBLK])
    su_d = din("su", [128, 32])
    cu_d = din("cu", [128, 32])

    out_d = nc.dram_tensor("out", [S, D], F32, kind="ExternalOutput").ap()

    QT = dscr("QT", [512, S], BF16)
    KT = dscr("KT", [512, S], BF16)
    VV = dscr("VV", [S, 512], BF16)
    RQT = dscr("RQT", [256, S], BF16)
    RKT = dscr("RKT", [256, S], BF16)
    RV = dscr("RV", [S, 256], BF16)
    RG = dscr("RG", [S, 256], F32)
    CATT = dscr("CATT", [D, S], BF16)
    X1S = dscr("X1S", [S, D], F32)
    X1F = nc.dram_tensor("X1F", [S, D], F32).ap()
    X1B = nc.dram_tensor("X1B", [S, D], BF16).ap()
    XB = nc.dram_tensor("XB", [NSLOT, D], BF16).ap()
    YB = nc.dram_tensor("YB", [NSLOT, D], F32).ap()
    NFC_D = FFN_DIM // 128
    NFC_M = EXPERT_DIM // 128
    WGU_D = nc.dram_tensor("WGU_D", [NFC_D, 128, 2048], BF16).ap()
    WD_D = nc.dram_tensor("WD_D", [NFC_D, 128, D], BF16).ap()
    WGU_M = nc.dram_tensor("WGU_M", [N_EXPERTS * NFC_M, 128, 2048], BF16).ap()
    WD_M = nc.dram_tensor("WD_M", [N_EXPERTS * 4, 128, 7, D], BF16).ap()

    p = Prog(nc)

    def make_conv_units():
        units = []

        def conv_set(wg, wu, wd, WGU, WDs, NE, F):
            NFC = F // 128
            for e in range(NE):
                for which, w in ((0, wg), (1, wu)):
                    for kc in range(8):
                        src = w[e, kc * 128:(kc + 1) * 128, :]
                        base = WGU[e * NFC:(e + 1) * NFC, :, which * 1024 + kc * 128: which * 1024 + (kc + 1) * 128]
                        dst = base.rearrange("c p j -> p c j")
                        units.append((src, F, dst, lambda s_: s_, lambda s_: s_.rearrange("p (c j) -> p c j", j=128)))
                wdv = wd[e].rearrange("(c p) d -> p c d", p=128)
                for c0 in range(0, NFC, 2):
                    src = wdv[:, c0:c0 + 2, :]
                    if NE == 1:
                        dst = WDs[e * NFC + c0: e * NFC + c0 + 2].rearrange("c p d -> p c d")
                    else:
                        dst = [WDs[e * 4 + (c0 + q) // 7, :, (c0 + q) % 7, :] for q in range(2)]
                    v2 = lambda s_: s_.rearrange("p (c d) -> p c d", d=D)
                    units.append((src, 2 * D, dst, v2, v2))

        conv_set(ffn_wg, ffn_wu, ffn_wd, WGU_D, WD_D, 1, FFN_DIM)
        n_dense = len(units)
        conv_set(moe_wg, moe_wu, moe_wd, WGU_M, WD_M, N_EXPERTS, EXPERT_DIM)
        return units, n_dense

    class ConvStream:
        def __init__(self, units, c32, c16):
            self.units = units
            self.c32 = c32
            self.c16 = c16
            self.i = 0
            self.pending = None

        def _store(self):
            if self.pending is None:
                return
            dst_ap, st_view, s16, b = self.pending
            self.pending = None
            if isinstance(dst_ap, list):
                v = st_view(s16)
                for q, d_ in enumerate(dst_ap):
                    p.dma("sp", d_, v[:, q, :], reads=[("c16", b)], lane=("c16", b))
            else:
                p.dma("sp", dst_ap, st_view(s16), reads=[("c16", b)], lane=("c16", b))

        def step(self):
            self._store()
            if self.i >= len(self.units):
                return False
            src_ap, n, dst_ap, ld_view, st_view = self.units[self.i]
            b = self.i % 2
            s32 = self.c32[:, b, 0:n]
            s16 = self.c16[:, b, 0:n]
            p.dma("sp", ld_view(s32), src_ap, writes=[("c32", b)], lane=("c32", b))
            if self.i % 3 == 2:
                p.op("pool", CP(s16, s32), reads=[("c32", b)], writes=[("c16", b)])
            else:
                p.op("dve", CP(s16, s32), reads=[("c32", b)], writes=[("c16", b)])
            self.pending = (dst_ap, st_view, s16, b)
            self.i += 1
            return True

        def flush(self):
            while self.step():
                pass
            self._store()

    def phase_convert():
        FMAX = EXPERT_DIM
        with ExitStack() as es:
            sb = lambda n, *a: es.enter_context(nc.sbuf_tensor(_uniq(n), *a))
            pst = lambda n, *a: es.enter_context(nc.psum_tensor(_uniq(n), *a))
            c32 = sb("c32", [128, 2, FMAX], F32)
            c16 = sb("c16", [128, 2, FMAX], BF16)
            cnt = [0]

            def unit(src_ap, n, dst_ap, ld_view, st_view):
                i = cnt[0]
                cnt[0] += 1
                b = i % 2
                s32 = c32[:, b, 0:n]
                s16 = c16[:, b, 0:n]
                p.dma("sp", ld_view(s32), src_ap, writes=[("c32", b)], lane=("c32", b))
                cast_op(p, i, s16, s32, reads=[("c32", b)], writes=[("c16", b)])
                if isinstance(dst_ap, list):
                    v = st_view(s16)
                    for q, d_ in enumerate(dst_ap):
                        p.dma("sp", d_, v[:, q, :], reads=[("c16", b)], lane=("c16", b))
                else:
                    p.dma("sp", dst_ap, st_view(s16), reads=[("c16", b)], lane=("c16", b))

            def conv_set(wg, wu, wd, WGU, WDs, NE, F):
                NFC = F // 128
                for e in range(NE):
                    for which, w in ((0, wg), (1, wu)):
                        for kc in range(8):
                            src = w[e, kc * 128:(kc + 1) * 128, :]
                            base = WGU[e * NFC:(e + 1) * NFC, :, which * 1024 + kc * 128: which * 1024 + (kc + 1) * 128]
                            dst = base.rearrange("c p j -> p c j")
                            unit(src, F, dst, lambda s: s, lambda s: s.rearrange("p (c j) -> p c j", j=128))
                    wdv = wd[e].rearrange("(c p) d -> p c d", p=128)
                    for c0 in range(0, NFC, 2):
                        src = wdv[:, c0:c0 + 2, :]
                        if NE == 1:
                            dst = WDs[e * NFC + c0: e * NFC + c0 + 2].rearrange("c p d -> p c d")
                        else:
                            dst = [WDs[e * 4 + (c0 + q) // 7, :, (c0 + q) % 7, :] for q in range(2)]
                        v2 = lambda s: s.rearrange("p (c d) -> p c d", d=D)
                        unit(src, 2 * D, dst, v2, v2)

            conv_set(ffn_wg, ffn_wu, ffn_wd, WGU_D, WD_D, 1, FFN_DIM)
            conv_set(moe_wg, moe_wu, moe_wd, WGU_M, WD_M, N_EXPERTS, EXPERT_DIM)
            p.emit()

    def phase_A(l, xin):
        with ExitStack() as es:
            sb = lambda n, *a: es.enter_context(nc.sbuf_tensor(_uniq(n), *a))
            pst = lambda n, *a: es.enter_context(nc.psum_tensor(_uniq(n), *a))
            WIN = sb("WIN", [128, 8, INWX], BF16)
            WST = sb("WST", [128, INW], F32)
            IDN = sb("IDN", [128, 128], F32)
            XIN = sb("XIN", [128, 6, D], F32)
            XT = sb("XT", [128, 2, 8, 512], BF16)
            QKO = sb("QKO", [128, 2, 8, 512], BF16)
            VO = sb("VO", [128, 2, 4, 512], BF16)
            RVO = sb("RVO", [128, 2, 4, 256], BF16)
            RGO = sb("RGO", [128, 2, 4, 256], F32)
            RQKO = sb("RQKO", [128, 2, 4, 512], BF16)
            PB = sb("PB", [128, 2, 528], F32)
            LA = sb("LA", [128, 2, 528], F32)
            LB = sb("LB", [128, 2, 528], F32)
            PTMP = sb("PTMP", [128, 512], F32)
            PLD = sb("PLD", [128, 2, 512], BF16)
            PO = sb("PO", [128, 2, 2, 512], BF16)
            CS = sb("CS", [128, 2, 2, 512], F32)
            T1 = sb("T1", [128, 2, 512], F32)
            T2 = sb("T2", [128, 2, 512], F32)
            INVC = sb("INVC", [128, 2, 2, 512], F32)
            PW32 = sb("PW32", [128, 2, 128], F32)
            PWBD = sb("PWBD", [128, 2, 128], BF16)
            PSC = sb("PSC", [128, 2], F32)
            TRP0 = pst("TRP0", [128, 512], F32)
            TRP1 = pst("TRP1", [128, 512], F32)
            MMA = pst("MMA", [128, 512], F32)
            MMB = pst("MMB", [128, 512], F32)
            MMC = pst("MMC", [128, 512], F32)
            MMD = pst("MMD", [128, 512], F32)
            PMX = pst("PMX", [128, 512], F32)
            TRP = [TRP0, TRP1]
            MMP = [MMA, MMB, MMC, MMD]
            p.dma("sp", IDN[:], ident_d, writes=["IDN"], lane="IDN")
            p.dma("sp", INVC[:], invc_d, writes=["INVC"], lane="INVC")
            p.dma("sp", PSC[:], pscale[l], writes=["PSC"], lane="PSC")
            p.op("dve", MSET(PW32[:], 0.0), writes=["PW32"])
            for c in range(2):
                for hh in range(2):
                    g = 2 * c + hh
                    p.dma("sp", PW32[64 * hh:64 * hh + 64, c, 64 * hh:64 * hh + 64], poolw[l, g],
                          reads=[], writes=["PW32"], lane=("PW32", g))
            p.op("dve", CP(PWBD[:], PW32[:]), reads=["PW32"], writes=["PWBD"])
            for kc in range(8):
                p.dma("sp", WST[:], w_in[l, kc * 128:(kc + 1) * 128, :], writes=["WST"], lane="WST")
                cast_op(p, kc, WIN[:, kc, 0:INW], WST[:], reads=["WST"], writes=[("WIN", kc)])
                src = WST[:, 1792:2304].rearrange("p (h t f) -> p h t f", h=8, t=2)
                dst = WIN[:, kc, INW:INWX].rearrange("p (h t f) -> p h t f", h=8, t=2)
                p.op("act", ACT(dst[:, :, 0, :], src[:, :, 1, :], AF.Copy, scale=-1.0), reads=["WST"], writes=[("WINa", kc)])
                p.op("dve", CP(dst[:, :, 1, :], src[:, :, 0, :]), reads=["WST"], writes=[("WINb", kc)])
            WINK = [("WIN", kc) for kc in range(8)] + [("WINa", kc) for kc in range(8)] + [("WINb", kc) for kc in range(8)]
            p.op("pool", MSET(PB[:, :, 0:16], 0.0), writes=["PBh"])
            if ASTOP == 1:
                p.emit()
                return

            xv = xin.rearrange("(n p) d -> n p d", p=128)
            QTv = QT.rearrange("(c p) s -> p c s", p=128)
            KTv = KT.rearrange("(c p) s -> p c s", p=128)
            VVv = VV.rearrange("(n p) f -> p n f", p=128)
            RVv = RV.rearrange("(n p) f -> p n f", p=128)
            RGv = RG.rearrange("(n p) f -> p n f", p=128)
            RQTv = RQT.rearrange("(c p) s -> p c s", p=128)
            RKTv = RKT.rearrange("(c p) s -> p c s", p=128)
            CATv = CATT.rearrange("(c p) s -> p c s", p=128)

            mmi = [0]
            evi = [0]
            xi = [0]
            deferred = []

            def evac(out, in_, reads, writes):
                evi[0] += 1
                if evi[0] % 2:
                    p.op("act", ACT(out, in_, AF.Copy), reads=[], writes=list(writes) + list(reads))
                else:
                    p.op("dve", CP(out, in_), reads=[], writes=list(writes) + list(reads))

            for t in range(NT5):
                b = t % 2
                t0 = t * 512
                p.dma("sp", CS[:, b, 0, :], cos_d[:, t0:t0 + 512], writes=[("CS", b, 0)], lane=("CS", b, 0))
                p.dma("sp", CS[:, b, 1, :], sin_d[:, t0:t0 + 512], writes=[("CS", b, 1)], lane=("CS", b, 1))
                xsl = []
                for j in range(4):
                    s = xi[0] % 6
                    xi[0] += 1
                    p.dma("sp", XIN[:, s, :], xv[t * 4 + j], writes=[("XIN", s)], lane=("XIN", s))
                    xsl.append(s)
                for kc in range(8):
                    tp = TRP[kc % 2]
                    tk = ("TRP", kc % 2)
                    fns = [TR(tp[:, j * 128:(j + 1) * 128], XIN[:, xsl[j], kc * 128:(kc + 1) * 128], IDN[:]) for j in range(4)]
                    p.op("pe", fns, reads=["IDN"] + [("XIN", s) for s in xsl], writes=[tk])
                    evac(XT[:, b, kc, :], tp[:], reads=[tk], writes=[("XT", b, kc)])
                XTK = [("XT", b, kc) for kc in range(8)]
                if ASTOP == 2:
                    continue

                def fm_group(col0):
                    i = mmi[0] % 4
                    mmi[0] += 1
                    ps = MMP[i]
                    fns = [MM(ps[:], WIN[:, kc, col0:col0 + 128], XT[:, b, kc, :], start=(kc == 0), stop=(kc == 7)) for kc in range(8)]
                    p.op("pe", fns, reads=XTK + WINK, writes=[("MMP", i)])
                    return ps, ("MMP", i)

                def tm_group(j, col0):
                    i = mmi[0] % 4
                    mmi[0] += 1
                    ps = MMP[i]
                    fns = [MM(ps[:], XT[:, b, kc, j * 128:(j + 1) * 128], WIN[:, kc, col0:col0 + 512], start=(kc == 0), stop=(kc == 7)) for kc in range(8)]
                    p.op("pe", fns, reads=XTK + WINK, writes=[("MMP", i)])
                    return ps, ("MMP", i)

                for c in range(8):
                    ps, k = fm_group(c * 128)
                    evac(QKO[:, b, c, :], ps[:], reads=[k], writes=[("QKO", b, c)])
                while deferred:
                    deferred.pop(0)()
                if ASUB != "nostore":
                    p.dma("sp", QTv[:, :, t0:t0 + 512], QKO[:, b, 0:4, :], reads=[("QKO", b, c) for c in range(4)], lane=("QKOq", b))
                    p.dma("sp", KTv[:, :, t0:t0 + 512], QKO[:, b, 4:8, :], reads=[("QKO", b, c) for c in range(4, 8)], lane=("QKOk", b))
                if ASTOP == 3:
                    continue
                for j in range(4):
                    ps, k = tm_group(j, 1024)
                    p.op("act", ACT(VO[:, b, j, :], ps[:], AF.Copy), writes=[k, ("VO", b, j)])
                if ASUB != "novst":
                    p.dma("sp", VVv[:, t * 4:(t + 1) * 4, :], VO[:, b], reads=[("VO", b, j) for j in range(4)], lane=("VO", b))
                if ASUB in ("novst", "onlydv"):
                    continue
                for j in range(4):
                    ps, k = tm_group(j, 2304)
                    p.op("dve", CP(RVO[:, b, j, :], ps[:, 0:256]), writes=[k, ("RVO", b, j)])
                    p.op("act", ACT(RGO[:, b, j, :], ps[:, 256:512], AF.Copy if ASUB == "nosilu" else AF.Silu), writes=[k, ("RGO", b, j)])
                p.dma("sp", RVv[:, t * 4:(t + 1) * 4, :], RVO[:, b], reads=[("RVO", b, j) for j in range(4)], lane=("RVO", b))
                p.dma("sp", RGv[:, t * 4:(t + 1) * 4, :], RGO[:, b], reads=[("RGO", b, j) for j in range(4)], lane=("RGO", b))
                if ASTOP == 4:
                    continue
                for qk in range(2):
                    for c in range(2):
                        psA, kA = fm_group(1792 + qk * 256 + c * 128)
                        psB, kB = fm_group(INW + qk * 256 + c * 128)
                        p.op("dve", TT(T1[:, c, :], psA[:], CS[:, b, 0, :], ALU.mult), reads=[("CS", b, 0)], writes=[kA, ("T1", c)])
                        p.op("dve", TT(T2[:, c, :], psB[:], CS[:, b, 1, :], ALU.mult), reads=[("CS", b, 1)], writes=[kB, ("T2", c)])
                        p.op("pool", TT(RQKO[:, b, qk * 2 + c, :], T1[:, c, :], T2[:, c, :], ALU.add),
                             reads=[("T1", c), ("T2", c)], writes=[("RQKO", b, qk * 2 + c)])
                p.dma("sp", RQTv[:, :, t0:t0 + 512], RQKO[:, b, 0:2, :], reads=[("RQKO", b, 0), ("RQKO", b, 1)], lane=("RQKOq", b))
                p.dma("sp", RKTv[:, :, t0:t0 + 512], RQKO[:, b, 2:4, :], reads=[("RQKO", b, 2), ("RQKO", b, 3)], lane=("RQKOk", b))
                if ASTOP == 5:
                    continue
                for c in range(2):
                    ps, k = fm_group(1536 + c * 128)
                    p.op("act", ACT(PB[:, c, 16:528], ps[:], AF.Copy), writes=[k, ("PB", c)])
                PBK = [("PB", 0), ("PB", 1), "PBh"]
                p.op("pool", TT(LA[:, :, 1:528], PB[:, :, 1:528], PB[:, :, 0:527], ALU.add), reads=PBK, writes=["LA"])
                iv = INVC[:, 0 if t == 0 else 1]

                def pool_out(lv, c, hh):
                    sl = slice(64 * hh, 64 * hh + 64)
                    p.op("pool", TT(PTMP[sl, :], lv[sl, c, 16:528], iv[sl, c, :], ALU.mult), reads=["LA", "LB", "INVC"], writes=[("PTMP", hh)])
                    p.op("pool", TT(PLD[sl, c, :], PTMP[sl, :], PB[sl, c, 16:528], ALU.subtract),
                         reads=[("PTMP", hh)] + PBK, writes=[("PLD", c, hh)])

                pool_out(LA, 0, 0)
                p.op("pool", TT(LB[:, :, 3:528], LA[:, :, 3:528], LA[:, :, 1:526], ALU.add), reads=["LA"], writes=["LB"])
                pool_out(LB, 0, 1)
                p.op("pool", TT(LA[:, :, 7:528], LB[:, :, 7:528], LB[:, :, 3:524], ALU.add), reads=["LB"], writes=["LA"])
                pool_out(LA, 1, 0)
                p.op("pool", TT(LB[:, :, 15:528], LA[:, :, 15:528], LA[:, :, 7:520], ALU.add), reads=["LA"], writes=["LB"])
                pool_out(LB, 1, 1)
                p.op("pool", CP(PB[:, :, 0:16], PB[:, :, 512:528]), reads=[("PB", 0), ("PB", 1)] + [("PLD", c, hh) for c in range(2) for hh in range(2)], writes=["PBh"])
                def mix(b=b, t0=t0):
                    for c in range(2):
                        p.op("pe", MM(PMX[:], PWBD[:, c, :], PLD[:, c, :]), reads=["PWBD", ("PLD", c, 0), ("PLD", c, 1)], writes=["PMX"])
                        p.op("dve", TS(PO[:, b, c, :], PMX[:], PSC[:, c:c + 1], None, ALU.mult), reads=["PSC"], writes=["PMX", ("PO", b, c)])
                    p.dma("sp", CATv[:, 4:6, t0:t0 + 512], PO[:, b], reads=[("PO", b, 0), ("PO", b, 1)], lane=("PO", b))
                deferred.append(mix)
            while deferred:
                deferred.pop(0)()
            p.emit()

    def phase_B(l, conv_units=None):
        lam_init = 0.8 - 0.6 * math.exp(-0.3 * l)
        scale = 64 ** -0.5
        with ExitStack() as es:
            sb = lambda n, *a: es.enter_context(nc.sbuf_tensor(_uniq(n), *a))
            pst = lambda n, *a: es.enter_context(nc.psum_tensor(_uniq(n), *a))
            KTh2 = sb("KTh", [128, 2, S], BF16)
            Vh2 = sb("Vh", [128, 2, NB, 132], BF16)
            cstream = None
            if conv_units:
                c32 = sb("c32", [128, 2, EXPERT_DIM], F32)
                c16 = sb("c16", [128, 2, EXPERT_DIM], BF16)
                cstream = ConvStream(conv_units, c32, c16)
                n_steps_total = sum(2 * (4 * q + 4) for q in range(NT5)) * 4
                conv_every = max(1, n_steps_total // (len(conv_units) + 1))
            step_ctr = [0]
            QZ = sb("QZ", [128, 2, 2, 512], BF16)
            PT = sb("PT", [128, 6, 512], BF16)
            TMP = sb("TMP", [128, 2, 128], F32)
            BT = sb("BT", [128, 2, 8, 128], F32)
            MNEG = sb("MNEG", [128, 128], F32)
            CB = sb("CB", [128, 8], F32)
            LAMT = sb("LAMT", [128, 256], F32)
            LPR = sb("LPR", [128, 2, 64], F32)
            LS = sb("LS", [128, 2], F32)
            LE = sb("LE", [128, 2], F32)
            NLAM = sb("NLAM", [128, 1], F32)
            SG = sb("SG", [128, 128], F32)
            IDB = sb("IDB", [128, 128], BF16)
            IDF = sb("IDF", [128, 128], F32)
            RR = sb("RR", [128, 2, 4, 2], F32)
            RL = sb("RL", [128, 2, 4], F32)
            OO = sb("OO", [128, 2, 128], F32)
            JUNK = sb("JUNK", [128, 128], F32)
            SS = sb("SS", [128, 2, 4], F32)
            SD = sb("SD", [128, 2, 4], F32)
            RS = sb("RS", [128, 2, 4], F32)
            EPSN = sb("EPSN", [128, 1], F32)
            YT = sb("YT", [128, 2, 128], BF16)
            YO = sb("YO", [128, 2, 512], BF16)
            ST0 = pst("ST0", [128, 512], F32)
            ST1 = pst("ST1", [128, 512], F32)
            ST2 = pst("ST2", [128, 512], F32)
            ACC = pst("ACC", [128, 4, 512], F32)
            TRB = pst("TRB", [128, 512], F32)
            STP = [ST0, ST1, ST2]
            p.dma("sp", BT[:], bt_d, writes=["BT"], lane="BT")
            p.dma("sp", MNEG[:], maskneg_d, writes=["MNEG"], lane="MNEG")
            p.dma("sp", CB[:], cb_d, writes=["CB"], lane="CB")
            p.dma("sp", LAMT[:], lam_rep[l], writes=["LAMT"], lane="LAMT")
            p.dma("sp", SG[:], subln_rep[l], writes=["SG"], lane="SG")
            p.dma("sp", IDF[:], ident_d, writes=["IDF"], lane="IDF")
            p.op("dve", CP(IDB[:], IDF[:]), reads=["IDF"], writes=["IDB"])
            p.op("dve", MSET(EPSN[:], NORM_EPS), writes=["EPSN"])
            for hm in range(8):
                p.op("dve", TT(BT[:, 0, hm, :], BT[:, 0, hm, :], MNEG[:], ALU.add), reads=["BT", "MNEG"], writes=["BT"])
            LV = LAMT[:].rearrange("p (a b f) -> p a b f", a=2, b=2)
            p.op("dve", TT(LPR[:], LV[:, :, 0, :], LV[:, :, 1, :], ALU.mult), reads=["LAMT"], writes=["LPR"])
            p.op("dve", RED(LS[:], LPR[:], ALU.add), reads=["LPR"], writes=["LS"])
            p.op("act", ACT(LE[:], LS[:], AF.Exp), reads=["LS"], writes=["LE"])
            p.op("dve", TT(NLAM[:], LE[:, 1:2], LE[:, 0:1], ALU.subtract), reads=["LE"], writes=["NLAM"])
            p.op("dve", TS(NLAM[:], NLAM[:], -lam_init, None, ALU.add), reads=["NLAM"], writes=["NLAM"])
            p.op("dve", TS(SG[:], SG[:], 1.0 - lam_init, None, ALU.mult), reads=["SG"], writes=["SG"])
            p.op("pool", MSET(Vh2[:, :, :, 128:132], 1.0), writes=["Vh1"])
            p.op("pool", MSET(QZ[:], 0.0), writes=[("QTt", 0), ("QTt", 1)])
            if BSTOP == 1:
                p.emit()
                return

            KTv = KT.rearrange("(h p) s -> h p s", p=128)
            QTv = QT.rearrange("(h p) s -> h p s", p=128)
            VVv = VV.rearrange("(n p) f -> p n f", p=128)
            CATv = CATT.rearrange("(c p) s -> c p s", p=128)
            sti = [0]
            pti = [0]
            tmi = [0]
            gi = [0]

            for h in range(4):
                hb = h % 2
                KTh = KTh2[:, hb, :]
                Vh = Vh2[:, hb]
                if h == 0:
                    p.dma("sp", KTh2[:, 0, :], KTv[0], writes=[("KTh", 0)], lane=("KTh", 0))
                    p.dma("sp", Vh2[:, 0, :, 0:128], VVv[:, :, 0:128], writes=[("Vh", 0)], lane=("Vh", 0))
                if h + 1 < 4:
                    nb_ = (h + 1) % 2
                    p.dma("sp", KTh2[:, nb_, :], KTv[h + 1], writes=[("KTh", nb_)], lane=("KTh", nb_))
                    p.dma("sp", Vh2[:, nb_, :, 0:128], VVv[:, :, (h + 1) * 128:(h + 2) * 128], writes=[("Vh", nb_)], lane=("Vh", nb_))
                for qg in range(NT5):
                    g = gi[0] % 2
                    gi[0] += 1
                    t0 = qg * 512
                    p.dma("sp", QZ[0:64, g, 0, :], QTv[h, 0:64, t0:t0 + 512], writes=[("QTt", g)], lane=("QTt", g, 0))
                    p.dma("sp", QZ[64:128, g, 1, :], QTv[h, 64:128, t0:t0 + 512], reads=[("QTt", g)], writes=[("QTtb", g)], lane=("QTt", g, 1))
                    nkb = 4 * qg + 4
                    steps = [(kb, m) for kb in range(nkb) for m in range(2)]
                    first_pv = {}

                    def qk(step):
                        kb, m = step
                        i = sti[0] % 3
                        sti[0] += 1
                        p.op("pe", MM(STP[i][:], KTh[:, kb * 128:(kb + 1) * 128], QZ[:, g, m, :]),
                             reads=[("KTh", hb), ("QTt", g), ("QTtb", g)], writes=[("ST", i)])
                        return i

                    def softmax_pv(step, i):
                        kb, m = step
                        if BSTOP == 2:
                            return
                        hm = 2 * h + m
                        st = STP[i]
                        pi = pti[0] % 6
                        pti[0] += 1
                        jmin = max(0, kb - 4 * qg)
                        jc = max(0, kb - 4 * qg + 2)
                        wk = []
                        for j in range(jmin, min(jc, 4)):
                            rel = 4 * qg + j - kb
                            case = 0 if rel == 0 else 1
                            ti = tmi[0] % 2
                            tmi[0] += 1
                            p.op("dve", STT(TMP[:, ti, :], st[:, j * 128:(j + 1) * 128], scale, BT[:, case, hm, :], ALU.mult, ALU.add),
                                 reads=["BT"], writes=[("ST", i), ("TMP", ti)])
                            p.op("act", ACT(PT[:, pi, j * 128:(j + 1) * 128], TMP[:, ti, :], AF.Exp),
                                 reads=[("TMP", ti)], writes=[("PT", pi, j)])
                        if jc < 4:
                            p.op("act", ACT(PT[:, pi, jc * 128:512], st[:, jc * 128:512], AF.Exp, bias=CB[:, hm:hm + 1], scale=scale),
                                 reads=["CB"], writes=[("ST", i)] + [("PT", pi, j) for j in range(jc, 4)])
                        if BSTOP == 3:
                            return
                        fns = []
                        for j in range(jmin, 4):
                            st_flag = j not in first_pv
                            first_pv[j] = True
                            last = (kb == 4 * qg + j) and m == 1
                            fns.append(MM(ACC[:, j, m * 132:m * 132 + 129], PT[:, pi, j * 128:(j + 1) * 128], Vh[:, kb, 0:129],
                                          start=st_flag, stop=last, skip=True))
                        p.op("pe", fns, reads=[("PT", pi, j) for j in range(jmin, 4)] + [("Vh", hb), "Vh1"], writes=[("ACC", j) for j in range(jmin, 4)])

                    LOOK = 2
                    pend = []
                    for s_i, step in enumerate(steps):
                        pend.append((step, qk(step)))
                        if len(pend) > LOOK:
                            st_, i_ = pend.pop(0)
                            softmax_pv(st_, i_)
                        step_ctr[0] += 1
                        if cstream is not None and step_ctr[0] % conv_every == 0:
                            cstream.step()
                    while pend:
                        st_, i_ = pend.pop(0)
                        softmax_pv(st_, i_)

                    if BSTOP in (2, 3, 4):
                        continue
                    lv = ACC[:, :, 128:264:132]
                    p.op("dve", RCP(RR[:, g], lv), writes=[("ACC", j) for j in range(4)] + [("RR", g)])
                    p.op("dve", TS(RL[:, g, :], RR[:, g, :, 1], NLAM[:, 0:1], None, ALU.mult), reads=[("RR", g), "NLAM"], writes=[("RL", g)])
                    for j in range(4):
                        o = j % 2
                        p.op("dve", TS(OO[:, o, :], ACC[:, j, 0:128], RR[:, g, j, 0:1], None, ALU.mult), reads=[("RR", g)], writes=[("ACC", j), ("OO", o)])
                        p.op("dve", STT(OO[:, o, :], ACC[:, j, 132:260], RL[:, g, j:j + 1], OO[:, o, :], ALU.mult, ALU.add),
                             reads=[("RL", g), ("OO", o)], writes=[("ACC", j), ("OO", o)])
                        p.op("dve", lambda e, o=o, g=g, j=j: e.scalar_tensor_tensor(out=JUNK[:], in0=OO[:, o, :], scalar=1.0, in1=OO[:, o, :], op0=ALU.mult, op1=ALU.mult,
                                                                                     accum_out=SS[:, g, j:j + 1]),
                             reads=[("OO", o)], writes=["JUNK", ("SS", g, j)])
                        p.op("act", ACT(SD[:, g, j:j + 1], SS[:, g, j:j + 1], AF.Ln, bias=EPSN[:], scale=1.0 / 128.0),
                             reads=[("SS", g, j), "EPSN"], writes=[("SD", g, j)])
                        p.op("act", ACT(RS[:, g, j:j + 1], SD[:, g, j:j + 1], AF.Exp, scale=-0.5), reads=[("SD", g, j)], writes=[("RS", g, j)])
                        p.op("dve", STT(YT[:, o, :], OO[:, o, :], RS[:, g, j:j + 1], SG[:], ALU.mult, ALU.mult),
                             reads=[("OO", o), ("RS", g, j), "SG"], writes=[("YT", o)])
                        p.op("pe", MM(TRB[:, j * 128:(j + 1) * 128], YT[:, o, :], IDB[:]), reads=[("YT", o), "IDB"], writes=["TRB"])
                    p.op("act", ACT(YO[:, g, :], TRB[:, 0:512], AF.Copy), writes=["TRB", ("YO", g)])
                    p.dma("sp", CATv[h, :, t0:t0 + 512], YO[:, g, :], reads=[("YO", g)], lane=("YO", g))
            if cstream is not None:
                cstream.flush()
            p.emit()

    def phase_C(l):
        with ExitStack() as es:
            sb = lambda n, *a: es.enter_context(nc.sbuf_tensor(_uniq(n), *a))
            pst = lambda n, *a: es.enter_context(nc.psum_tensor(_uniq(n), *a))
            RQ = sb("RQ", [128, 2, 512], BF16)
            RK = sb("RK", [128, 2, 512], BF16)
            QD = sb("QD", [128, 2, 512], BF16)
            RVt = sb("RVt", [128, 2, 4, 128], BF16)
            RGt = sb("RGt", [128, 2, 4, 128], F32)
            MASKT = sb("MASKT", [128, 4, 128], F32)
            QDEC = sb("QDEC", [128, 2, 512], F32)
            KDEC = sb("KDEC", [128, 4, 64], F32)
            CDt = sb("CDt", [128, 2], F32)
            BDM = sb("BDM", [128, 128], F32)
            GN = sb("GN", [128, 256], F32)
            IDB = sb("IDB2", [128, 128], BF16)
            IDF = sb("IDF2", [128, 128], F32)
            IND = sb("IND", [128, 2, 2, 128], BF16)
            KD = sb("KD", [128, 2, 128], BF16)
            SBD = sb("SBD", [128, 2, 128], F32)
            SBDb = sb("SBDb", [128, 2, 128], BF16)
            TMPU = sb("TMPU", [128, 128], F32)
            YSQ = sb("YSQ", [128, 512], F32)
            YS = sb("YS", [128, 512], F32)
            SM = sb("SM", [128, 8], F32)
            SQ = sb("SQ", [128, 8], F32)
            MN = sb("MN", [128, 8], F32)
            VR = sb("VR", [128, 8], F32)
            SDv = sb("SDv", [128, 8], F32)
            RSv = sb("RSv", [128, 8], F32)
            EPS2 = sb("EPS2", [128, 1], F32)
            YR = sb("YR", [128, 4, 128], BF16)
            YRO = sb("YRO", [128, 2, 512], BF16)
            INP0 = pst("INP0", [128, 512], F32)
            INP1 = pst("INP1", [128, 512], F32)
            INPS = [INP0, INP1]
            TKP = pst("TKP", [128, 512], F32)
            YP0 = pst("YP0", [128, 512], F32)
            YP1 = pst("YP1", [128, 512], F32)
            UP = pst("UP", [128, 512], F32)
            TRC = pst("TRC", [128, 512], F32)
            YPS = [YP0, YP1]
            p.dma("sp", MASKT[:], maskt_d, writes=["MASKT"], lane="MASKT")
            p.dma("sp", QDEC[:], qdec_d, writes=["QDEC"], lane="QDEC")
            p.dma("sp", KDEC[:], kdec_d, writes=["KDEC"], lane="KDEC")
            p.dma("sp", CDt[:], cd_d, writes=["CDt"], lane="CDt")
            p.dma("sp", BDM[:], bdmask_d, writes=["BDM"], lane="BDM")
            p.dma("sp", GN[:], gn_rep[l], writes=["GN"], lane="GN")
            p.dma("sp", IDF[:], ident_d, writes=["IDF"], lane="IDF")
            p.op("dve", CP(IDB[:], IDF[:]), reads=["IDF"], writes=["IDB"])
            p.op("dve", MSET(EPS2[:], NORM_EPS), writes=["EPS2"])
            RQTv = RQT.rearrange("(c p) s -> c p s", p=128)
            RKTv = RKT.rearrange("(c p) s -> c p s", p=128)
            RVv = RV.rearrange("(n p) f -> p n f", p=128)
            RGv = RG.rearrange("(n p) f -> p n f", p=128)
            CATv = CATT.rearrange("(c p) s -> c p s", p=128)
            bi = [0]
            ci = [0]
            for c in range(2):
                p.op("dve", MSET(SBD[:, 0, :], 0.0), writes=[("SBD", 0)])
                p.op("dve", MSET(SBDb[:, 0, :], 0.0), writes=[("SBDb", 0)])
                cur = 0
                for tb in range(NT5):
                    b = bi[0] % 2
                    bi[0] += 1
                    t0 = tb * 512
                    p.dma("sp", RQ[:, b, :], RQTv[c, :, t0:t0 + 512], writes=[("RQ", b)], lane=("RQ", b))
                    p.dma("sp", RK[:, b, :], RKTv[c, :, t0:t0 + 512], writes=[("RK", b)], lane=("RK", b))
                    p.dma("sp", RVt[:, b], RVv[:, tb * 4:(tb + 1) * 4, c * 128:(c + 1) * 128], writes=[("RVt", b)], lane=("RVt", b))
                    p.dma("sp", RGt[:, b], RGv[:, tb * 4:(tb + 1) * 4, c * 128:(c + 1) * 128], writes=[("RGt", b)], lane=("RGt", b))
                    p.op("pool", TT(QD[:, b, :], RQ[:, b, :], QDEC[:, c, :], ALU.mult), reads=[("RQ", b), "QDEC"], writes=[("QD", b)])
                    yp = YPS[b]
                    state = {"cur": cur}

                    def S1(j):
                        cc = j % 2
                        js = slice(j * 128, (j + 1) * 128)
                        for hh in range(2):
                            p.op("pe", MM(INPS[hh][:, cc * 128:(cc + 1) * 128], RK[64 * hh:64 * hh + 64, b, js], RQ[64 * hh:64 * hh + 64, b, js]),
                                 reads=[("RK", b), ("RQ", b)], writes=[("INP", hh)])
                        p.op("pe", MM(TKP[:, cc * 128:(cc + 1) * 128], RK[:, b, js], IDB[:]), reads=[("RK", b), "IDB"], writes=["TKP"])

                    def S2(j):
                        cc = j % 2
                        for hh in range(2):
                            p.op("dve", TT(IND[:, cc, hh, :], INPS[hh][:, cc * 128:(cc + 1) * 128], MASKT[:, 2 * c + hh, :], ALU.mult),
                                 reads=["MASKT"], writes=[("INP", hh), ("IND", cc, hh)])
                        p.op("dve", TT(KD[:, cc, :], TKP[:, cc * 128:(cc + 1) * 128], KDEC[:, 2 * c:2 * c + 2, :].rearrange("p a b -> p (a b)"), ALU.mult),
                             reads=["KDEC"], writes=["TKP", ("KD", cc)])

                    def S3(j):
                        cc = j % 2
                        js = slice(j * 128, (j + 1) * 128)
                        cur_ = state["cur"]
                        fns = [MM(yp[:, js], QD[:, b, js], SBDb[:, cur_, :], start=True, stop=False)]
                        for hh in range(2):
                            fns.append(MM(yp[:, j * 128 + 64 * hh: j * 128 + 64 * hh + 64], IND[:, cc, hh, :], RVt[:, b, j, 64 * hh:64 * hh + 64],
                                          start=False, stop=(hh == 1)))
                        p.op("pe", fns, reads=[("QD", b), ("SBDb", cur_), ("IND", cc, 0), ("IND", cc, 1), ("RVt", b)], writes=[("YPB", b)])
                        p.op("pe", MM(UP[:, cc * 128:(cc + 1) * 128], KD[:, cc, :], RVt[:, b, j, :]), reads=[("KD", cc), ("RVt", b)], writes=["UPB"])

                    def S4(j):
                        cc = j % 2
                        cur_ = state["cur"]
                        nxt = 1 - cur_
                        p.op("dve", TT(TMPU[:], UP[:, cc * 128:(cc + 1) * 128], BDM[:], ALU.mult), reads=["BDM"], writes=["UPB", "TMPU"])
                        p.op("dve", STT(SBD[:, nxt, :], SBD[:, cur_, :], CDt[:, c:c + 1], TMPU[:], ALU.mult, ALU.add),
                             reads=[("SBD", cur_), "CDt", "TMPU"], writes=[("SBD", nxt)])
                        p.op("act", ACT(SBDb[:, nxt, :], SBD[:, nxt, :], AF.Copy), reads=[("SBD", nxt)], writes=[("SBDb", nxt)])
                        state["cur"] = nxt

                    for n_ in range(6):
                        if n_ < 4:
                            S1(n_)
                        if 0 <= n_ - 1 < 4:
                            S2(n_ - 1)
                        if 0 <= n_ - 2 < 4:
                            S3(n_ - 2)
                            S4(n_ - 2)
                    cur = state["cur"]
                    YK = [("YPB", b)]
                    y3 = yp[:].rearrange("p (g e) -> p g e", e=64)
                    p.op("dve", RED(SM[:], y3, ALU.add), writes=YK + ["SM"])
                    p.op("act", ACT(YSQ[:], yp[:], AF.Square), writes=YK + ["YSQ"])
                    p.op("dve", RED(SQ[:], YSQ[:].rearrange("p (g e) -> p g e", e=64), ALU.add), reads=["YSQ"], writes=["SQ"])
                    p.op("dve", TS(MN[:], SM[:], 1.0 / 64.0, None, ALU.mult), reads=["SM"], writes=["MN"])
                    p.op("dve", TT(VR[:], MN[:], MN[:], ALU.mult), reads=["MN"], writes=["VR"])
                    p.op("dve", STT(VR[:], SQ[:], 1.0 / 64.0, VR[:], ALU.mult, ALU.subtract), reads=["SQ", "VR"], writes=["VR"])
                    p.op("act", ACT(SDv[:], VR[:], AF.Sqrt, bias=EPS2[:], scale=1.0), reads=["VR", "EPS2"], writes=["SDv"])
                    p.op("dve", RCP(RSv[:], SDv[:]), reads=["SDv"], writes=["RSv"])
                    ys3 = YS[:].rearrange("p (g e) -> p g e", e=64)
                    p.op("dve", TT(ys3, y3, MN[:].unsqueeze(2).to_broadcast([128, 8, 64]), ALU.subtract), reads=["MN"], writes=YK + ["YS"])
                    p.op("dve", TT(ys3, ys3, RSv[:].unsqueeze(2).to_broadcast([128, 8, 64]), ALU.mult), reads=["YS", "RSv"], writes=["YS"])
                    ys4 = YS[:].rearrange("p (j f) -> p j f", f=128)
                    p.op("pool", TT(ys4, ys4, GN[:, c * 128:(c + 1) * 128].unsqueeze(1).to_broadcast([128, 4, 128]), ALU.mult), reads=["YS", "GN"], writes=["YS"])
                    p.op("pool", TT(YR[:], ys4, RGt[:, b], ALU.mult), reads=["YS", ("RGt", b)], writes=["YR"])
                    fns = [MM(TRC[:, j * 128:(j + 1) * 128], YR[:, j, :], IDB[:]) for j in range(4)]
                    p.op("pe", fns, reads=["YR", "IDB"], writes=["TRC"])
                    p.op("act", ACT(YRO[:, b, :], TRC[:, 0:512], AF.Copy), writes=["TRC", ("YRO", b)])
                    p.dma("sp", CATv[6 + c, :, t0:t0 + 512], YRO[:, b, :], reads=[("YRO", b)], lane=("YRO", b))
            p.emit()

    def phase_D(l, xin, xout, moe):
        T = 1024
        NTT = T // 128
        NE = N_EXPERTS if moe else 1
        NFC = NFC_M if moe else NFC_D
        WGU = WGU_M if moe else WGU_D
        WDs = WD_M if moe else WD_D
        if moe:
            groups = [(0, 7), (7, 14), (14, 21), (21, 28)]
        else:
            groups = [(0, 6), (6, 12), (12, 17), (17, 22)]
        GMAX = 7
        with ExitStack() as es:
            sb = lambda n, *a: es.enter_context(nc.sbuf_tensor(_uniq(n), *a))
            pst = lambda n, *a: es.enter_context(nc.psum_tensor(_uniq(n), *a))
            WOUT = sb("WOUT", [128, 8, D], BF16)
            WO32 = sb("WO32", [128, D], F32)
            LNP = sb("LNP", [128, 4, D], F32)
            IDF = sb("IDF3", [128, 128], F32)
            X1 = sb("X1", [128, NTT, D], F32)
            X1T = sb("X1T", [128, 8, T], BF16)
            X1TF = sb("X1TF", [128, 2, 8, 16], F32)
            RW = sb("RW", [128, 8, N_EXPERTS], F32)
            HT = sb("HT", [128, GMAX, T], BF16)
            WD = sb("WD", [128, GMAX, D], BF16)
            WGUr = sb("WGUr", [128, 3, 2048], BF16)
            CTt = sb("CTt", [128, 4, 8, 128], BF16)
            Xt = sb("Xt", [128, 4, D], F32)
            Yt = sb("Yt", [128, 4, D], F32)
            XN = sb("XN", [128, 4, D], F32)
            BST = sb("BST", [128, 4, 12], F32)
            MV = sb("MV", [128, 4, 2], F32)
            SDl = sb("SDl", [128, 4], F32)
            RSl = sb("RSl", [128, 4], F32)
            EPSL = sb("EPSL", [128, 1], F32)
            SGt = sb("SGt", [128, 2, 512], F32)
            LGS = sb("LGS", [128, 8], F32)
            M1 = sb("M1", [128, 4], F32)
            EQ1 = sb("EQ1", [128, 8], F32)
            EQ2 = sb("EQ2", [128, 8], F32)
            LG2 = sb("LG2", [128, 8], F32)
            GT = sb("GT", [128, NTT, 8], F32)
            OUTt = sb("OUTt", [128, 2, D], F32)
            G0 = pst("G0", [128, 512], F32)
            G1 = pst("G1", [128, 512], F32)
            U0 = pst("U0", [128, 512], F32)
            U1 = pst("U1", [128, 512], F32)
            O0 = pst("O0", [128, 512], F32)
            O1 = pst("O1", [128, 512], F32)
            TR0 = pst("TR0", [128, 512], F32)
            TR1 = pst("TR1", [128, 512], F32)
            GP = [G0, G1]
            UPp = [U0, U1]
            OP = [O0, O1]
            TRp = [TR0, TR1]
            p.dma("sp", IDF[:], ident_d, writes=["IDF"], lane="IDF")
            p.dma("sp", LNP[:], ln_rep[l].rearrange("a p d -> p a d"), writes=["LNP"], lane="LNP")
            p.op("dve", MSET(EPSL[:], LN_EPS), writes=["EPSL"])
            for kc in range(8):
                p.dma("sp", WO32[:], w_out[l, kc * 128:(kc + 1) * 128, :], writes=["WO32"], lane="WO32")
                cast_op(p, kc, WOUT[:, kc, :], WO32[:], reads=["WO32"], writes=[("WOUT", kc)])
            WOK = [("WOUT", kc) for kc in range(8)]
            if moe:
                p.dma("sp", RW[:], router_w.rearrange("(c p) e -> p c e", p=128), writes=["RW"], lane="RW")
            xv = xin.rearrange("(n p) d -> n p d", p=128)
            ov = xout.rearrange("(n p) d -> n p d", p=128)
            CATv = CATT.rearrange("(c p) s -> p c s", p=128)
            oi = [0]
            ri = [0]
            gui = [0]

            def layer_norm(src, key_src, dst, key_dst, gi_, bi_, s, part=0):
                if part in (0, 1):
                    p.op("dve", lambda e: e.bn_stats(out=BST[:, s, 0:6], in_=src[:, 0:512]), reads=[key_src], writes=[("BST", s, 0)])
                    p.op("dve", lambda e: e.bn_stats(out=BST[:, s, 6:12], in_=src[:, 512:1024]), reads=[key_src], writes=[("BST", s, 1)])
                    p.op("dve", lambda e: e.bn_aggr(out=MV[:, s, :], in_=BST[:, s, :]), reads=[("BST", s, 0), ("BST", s, 1)], writes=[("MV", s)])
                    p.op("act", ACT(SDl[:, s:s + 1], MV[:, s, 1:2], AF.Ln, bias=EPSL[:], scale=1.0), reads=[("MV", s), "EPSL"], writes=[("SDl", s)])
                    p.op("act", ACT(RSl[:, s:s + 1], SDl[:, s:s + 1], AF.Exp, scale=-0.5), reads=[("SDl", s)], writes=[("RSl", s)])
                if part == 1:
                    return
                p.op("dve", TS(XN[:, s, :], src, MV[:, s, 0:1], RSl[:, s:s + 1], ALU.subtract, ALU.mult),
                     reads=[key_src, ("MV", s), ("RSl", s)], writes=[("XN", s)])
                p.op("dve", TT(XN[:, s, :], XN[:, s, :], LNP[:, gi_, :], ALU.mult), reads=[("XN", s), "LNP"], writes=[("XN", s)])
                p.op("dve", TT(dst, XN[:, s, :], LNP[:, bi_, :], ALU.add), reads=[("XN", s), "LNP"], writes=[key_dst])

            for st in range(S // T):
                tb = st * T
                def d1L(i):
                    s = i % 4
                    n = st * NTT + i
                    p.dma("sp", CTt[:, s], CATv[:, :, tb + i * 128: tb + (i + 1) * 128], writes=[("CTt", s)], lane=("CTt", s))
                    p.dma("sp", Xt[:, s, :], xv[n], writes=[("Xt", s)], lane=("Xt", s))

                def d1A(i):
                    s = i % 4
                    for half in range(2):
                        o = oi[0] % 2
                        oi[0] += 1
                        fns = [MM(OP[o][:], CTt[:, s, kc, :], WOUT[:, kc, half * 512:(half + 1) * 512], start=(kc == 0), stop=(kc == 7)) for kc in range(8)]
                        p.op("pe", fns, reads=[("CTt", s)] + WOK, writes=[("OP", o)])
                        p.op("dve", STT(Yt[:, s, half * 512:(half + 1) * 512], Xt[:, s, half * 512:(half + 1) * 512], ALPHA, OP[o][:], ALU.mult, ALU.add),
                             reads=[("Xt", s)], writes=[("OP", o), ("Yt", s)])

                def d1C(i):
                    for q4 in range(2):
                        r = ri[0] % 2
                        ri[0] += 1
                        fns = [TR(TRp[r][:, k4 * 128:(k4 + 1) * 128], X1[:, i, (q4 * 4 + k4) * 128:(q4 * 4 + k4 + 1) * 128], IDF[:]) for k4 in range(4)]
                        p.op("pe", fns, reads=[("X1", i), "IDF"], writes=[("TRp", r)])
                        src = TRp[r][:].rearrange("p (k t) -> p k t", t=128)
                        p.op("act", ACT(X1T[:, q4 * 4:(q4 + 1) * 4, i * 128:(i + 1) * 128], src, AF.Copy), writes=[("TRp", r), ("X1T", i, q4)])
                    p.op("act", ACT(X1[:, i, :], X1[:, i, :], AF.Copy, scale=ALPHA), reads=[("X1", i)], writes=[("X1", i)])

                if not moe:
                    d1L(0)
                    d1L(1)
                    for n_ in range(NTT + 3):
                        if n_ + 2 < NTT:
                            d1L(n_ + 2)
                        if n_ < NTT:
                            d1A(n_)
                        if 0 <= n_ - 1 < NTT:
                            i_ = n_ - 1
                            layer_norm(Yt[:, i_ % 4, :], ("Yt", i_ % 4), X1[:, i_, :], ("X1", i_), 0, 1, i_ % 4, part=1)
                        if 0 <= n_ - 2 < NTT:
                            i_ = n_ - 2
                            layer_norm(Yt[:, i_ % 4, :], ("Yt", i_ % 4), X1[:, i_, :], ("X1", i_), 0, 1, i_ % 4, part=2)
                        if 0 <= n_ - 3 < NTT:
                            d1C(n_ - 3)
                for i in (range(NTT) if moe else []):
                    s = i % 2
                    n = st * NTT + i
                    p.dma("sp", CTt[:, s], CATv[:, :, tb + i * 128: tb + (i + 1) * 128], writes=[("CTt", s)], lane=("CTt", s))
                    p.dma("sp", Xt[:, s, :], xv[n], writes=[("Xt", s)], lane=("Xt", s))
                    for half in range(2):
                        o = oi[0] % 2
                        oi[0] += 1
                        fns = [MM(OP[o][:], CTt[:, s, kc, :], WOUT[:, kc, half * 512:(half + 1) * 512], start=(kc == 0), stop=(kc == 7)) for kc in range(8)]
                        p.op("pe", fns, reads=[("CTt", s)] + WOK, writes=[("OP", o)])
                        p.op("dve", STT(Yt[:, s, half * 512:(half + 1) * 512], Xt[:, s, half * 512:(half + 1) * 512], ALPHA, OP[o][:], ALU.mult, ALU.add),
                             reads=[("Xt", s)], writes=[("OP", o), ("Yt", s)])
                    layer_norm(Yt[:, s, :], ("Yt", s), X1[:, i, :], ("X1", i), 0, 1, s)
                    for q4 in range(2):
                        r = ri[0] % 2
                        ri[0] += 1
                        fns = [TR(TRp[r][:, k4 * 128:(k4 + 1) * 128], X1[:, i, (q4 * 4 + k4) * 128:(q4 * 4 + k4 + 1) * 128], IDF[:]) for k4 in range(4)]
                        p.op("pe", fns, reads=[("X1", i), "IDF"], writes=[("TRp", r)])
                        src = TRp[r][:].rearrange("p (k t) -> p k t", t=128)
                        p.op("act", ACT(X1T[:, q4 * 4:(q4 + 1) * 4, i * 128:(i + 1) * 128], src, AF.Copy), writes=[("TRp", r), ("X1T", i, q4)])
                        if moe:
                            p.op("dve", CP(X1TF[:, s, q4 * 4:(q4 + 1) * 4, :], src), writes=[("TRp", r), ("X1TF", s, q4)])
                    if moe:
                        o = oi[0] % 2
                        oi[0] += 1
                        fns = [MM(OP[o][:, 0:8], X1TF[:, s, kc, :], RW[:, kc, :], start=(kc == 0), stop=(kc == 7)) for kc in range(8)]
                        p.op("pe", fns, reads=[("X1TF", s, 0), ("X1TF", s, 1), "RW"], writes=[("OP", o)])
                        p.op("dve", CP(LGS[:], OP[o][:, 0:8]), writes=[("OP", o), "LGS"])
                        p.op("dve", RED(M1[:, 0:1], LGS[:], ALU.max), reads=["LGS"], writes=["M1a"])
                        p.op("dve", TS(EQ1[:], LGS[:], M1[:, 0:1], None, ALU.is_equal), reads=["LGS", "M1a"], writes=["EQ1"])
                        p.op("dve", STT(LG2[:], EQ1[:], -1e30, LGS[:], ALU.mult, ALU.add), reads=["EQ1", "LGS"], writes=["LG2"])
                        p.op("dve", RED(M1[:, 1:2], LG2[:], ALU.max), reads=["LG2"], writes=["M1b"])
                        p.op("dve", TS(EQ2[:], LG2[:], M1[:, 1:2], None, ALU.is_equal), reads=["LG2", "M1b"], writes=["EQ2"])
                        p.op("dve", TT(M1[:, 2:3], M1[:, 1:2], M1[:, 0:1], ALU.subtract), reads=["M1a", "M1b"], writes=["M1c"])
                        p.op("act", ACT(M1[:, 2:3], M1[:, 2:3], AF.Sigmoid), reads=["M1c"], writes=["M1c"])
                        p.op("dve", TS(M1[:, 3:4], M1[:, 2:3], -1.0, 1.0, ALU.mult, ALU.add), reads=["M1c"], writes=["M1d"])
                        p.op("dve", TS(EQ1[:], EQ1[:], M1[:, 3:4], None, ALU.mult), reads=["EQ1", "M1d"], writes=["EQ1"])
                        p.op("dve", STT(GT[:, i, :], EQ2[:], M1[:, 2:3], EQ1[:], ALU.mult, ALU.add), reads=["EQ2", "M1c", "EQ1"], writes=[("GT", i)])
                    p.op("act", ACT(X1[:, i, :], X1[:, i, :], AF.Copy, scale=ALPHA), reads=[("X1", i)], writes=[("X1", i)])
                X1TK = [("X1T", i, q4) for i in range(NTT) for q4 in range(2)]
                for e in range(NE):
                    for (f0, f1) in groups:
                        ng = f1 - f0
                        for fl in range(ng):
                            fc = f0 + fl
                            r3 = gui[0] % 3
                            gui[0] += 1
                            p.dma("sp", WGUr[:, r3, :], WGU[e * NFC + fc], writes=[("WGUr", r3)], lane=("WGUr", r3))
                            if fl == 1:
                                p.dma("sp", WD[:, 0:ng, :], WDs[e * NFC + f0: e * NFC + f1].rearrange("c p d -> p c d"), writes=["WD"], lane="WD")
                            wv = WGUr[:, r3, :].rearrange("p (w k j) -> p w k j", w=2, k=8)
                            for th in range(2):
                                gp, up = GP[th], UPp[th]
                                fns = [MM(gp[:], wv[:, 0, kc, :], X1T[:, kc, th * 512:(th + 1) * 512], start=(kc == 0), stop=(kc == 7)) for kc in range(8)]
                                fns += [MM(up[:], wv[:, 1, kc, :], X1T[:, kc, th * 512:(th + 1) * 512], start=(kc == 0), stop=(kc == 7)) for kc in range(8)]
                                p.op("pe", fns, reads=[("WGUr", r3)] + X1TK, writes=[("GP", th), ("UP", th)])
                                p.op("act", ACT(SGt[:, th, :], gp[:], AF.Silu), writes=[("GP", th), ("SGt", th)])
                                p.op("dve", TT(HT[:, fl, th * 512:(th + 1) * 512], SGt[:, th, :], up[:], ALU.mult),
                                     reads=[("SGt", th)], writes=[("UP", th), ("HT", fl, th)])
                        HTK = [("HT", fl, th) for fl in range(ng) for th in range(2)]
                        for i in range(NTT):
                            for half in range(2):
                                o = oi[0] % 2
                                oi[0] += 1
                                fns = [MM(OP[o][:], HT[:, fl, i * 128:(i + 1) * 128], WD[:, fl, half * 512:(half + 1) * 512], start=(fl == 0), stop=(fl == ng - 1))
                                       for fl in range(ng)]
                                p.op("pe", fns, reads=HTK + ["WD"], writes=[("OP", o)])
                                acc = X1[:, i, half * 512:(half + 1) * 512]
                                sc = GT[:, i, e:e + 1] if moe else 1.0
                                p.op("dve", STT(acc, OP[o][:], sc, acc, ALU.mult, ALU.add), reads=[("X1", i), ("GT", i)], writes=[("OP", o), ("X1", i)])
                for n_ in range(NTT + 1):
                    if n_ < NTT:
                        layer_norm(X1[:, n_, :], ("X1", n_), OUTt[:, n_ % 2, :], ("OUTt", n_ % 2), 2, 3, n_ % 4, part=1)
                    if n_ - 1 >= 0:
                        i = n_ - 1
                        layer_norm(X1[:, i, :], ("X1", i), OUTt[:, i % 2, :], ("OUTt", i % 2), 2, 3, i % 4, part=2)
                        p.dma("sp", ov[st * NTT + i], OUTt[:, i % 2, :], reads=[("OUTt", i % 2)], lane=("OUTt", i % 2))
            p.emit()


    def phase_E(l, xin, xout):
        I32 = mybir.dt.int32
        U32 = mybir.dt.uint32
        NFC = NFC_M
        NTL = S // 128
        xv = xin.rearrange("(n p) d -> n p d", p=128)
        ov = xout.rearrange("(n p) d -> n p d", p=128)
        x1fv = X1F.rearrange("(n p) d -> n p d", p=128)
        x1bv = X1B.rearrange("(n p) d -> n p d", p=128)
        CATv = CATT.rearrange("(c p) s -> p c s", p=128)

        def ln_ops(src, key_src, dst, key_dst, LNP, gi_, bi_, s, BST, MV, SDl, RSl, EPSL, XN, part=0):
            if part in (0, 1):
                p.op("dve", lambda e: e.bn_stats(out=BST[:, s, 0:6], in_=src[:, 0:512]), reads=[key_src], writes=[("BST", s, 0)])
                p.op("dve", lambda e: e.bn_stats(out=BST[:, s, 6:12], in_=src[:, 512:1024]), reads=[key_src], writes=[("BST", s, 1)])
                p.op("dve", lambda e: e.bn_aggr(out=MV[:, s, :], in_=BST[:, s, :]), reads=[("BST", s, 0), ("BST", s, 1)], writes=[("MV", s)])
                p.op("act", ACT(SDl[:, s:s + 1], MV[:, s, 1:2], AF.Ln, bias=EPSL[:], scale=1.0), reads=[("MV", s), "EPSL"], writes=[("SDl", s)])
                p.op("act", ACT(RSl[:, s:s + 1], SDl[:, s:s + 1], AF.Exp, scale=-0.5), reads=[("SDl", s)], writes=[("RSl", s)])
            if part == 1:
                return
            p.op("dve", TS(XN[:, s, :], src, MV[:, s, 0:1], RSl[:, s:s + 1], ALU.subtract, ALU.mult),
                 reads=[key_src, ("MV", s), ("RSl", s)], writes=[("XN", s)])
            p.op("dve", TT(XN[:, s, :], XN[:, s, :], LNP[:, gi_, :], ALU.mult), reads=[("XN", s), "LNP"], writes=[("XN", s)])
            p.op("dve", TT(dst, XN[:, s, :], LNP[:, bi_, :], ALU.add), reads=[("XN", s), "LNP"], writes=[key_dst])

        RT = nc.dram_tensor(_uniq("RTAB"), [128, NTL * 4 + NBLK * 32], I32).ap()

        with ExitStack() as es:
            sb = lambda n, *a: es.enter_context(nc.sbuf_tensor(_uniq(n), *a))
            pst = lambda n, *a: es.enter_context(nc.psum_tensor(_uniq(n), *a))
            WOUT = sb("WOUT", [128, 8, D], BF16)
            WO32 = sb("WO32", [128, D], F32)
            LNP = sb("LNP", [128, 4, D], F32)
            IDF = sb("IDF", [128, 128], F32)
            RW = sb("RW", [128, 8, N_EXPERTS], F32)
            UT32 = sb("UT32", [128, 128], F32)
            UTB = sb("UTB", [128, 128], BF16)
            ONB = sb("ONB", [128, 128], BF16)
            KTH = sb("KTH", [128, 32], F32)
            BSTART = sb("BSTART", [128, NBLK], F32)
            SU = sb("SU", [128, 32], F32)
            CU = sb("CU", [128, 32], F32)
            CTt = sb("CTt", [128, 4, 8, 128], BF16)
            Xt = sb("Xt", [128, 4, D], F32)
            Yt = sb("Yt", [128, 4, D], F32)
            XN = sb("XN", [128, 4, D], F32)
            X1t = sb("X1t", [128, 4, D], F32)
            X1Bt = sb("X1Bt", [128, 4, D], BF16)
            X1TF = sb("X1TF", [128, 4, 8, 128], F32)
            BST = sb("BST", [128, 4, 12], F32)
            MV = sb("MV", [128, 4, 2], F32)
            SDl = sb("SDl", [128, 4], F32)
            RSl = sb("RSl", [128, 4], F32)
            EPSL = sb("EPSL", [128, 1], F32)
            LGSa = sb("LGS", [128, 4, 8], F32)
            LG2a = sb("LG2", [128, 4, 8], F32)
            M1a_ = sb("M1", [128, 4, 4], F32)
            MSKa = sb("MSK", [128, 4, 8], F32)
            MSKB = sb("MSKB", [128, 4, 8], BF16)
            EQ1A = sb("EQ1A", [128, NTL, 8], F32)
            EQ2A = sb("EQ2A", [128, NTL, 8], F32)
            RANKA = sb("RANKA", [128, NTL, 8], F32)
            GA = sb("GA", [128, NTL, 2], F32)
            CARRY = sb("CARRY", [128, 8], F32)
            CMPK = sb("CMPK", [128, 8, 32], F32)
            NBK = sb("NBK", [128, 8], F32)
            PADDED = sb("PADDED", [128, 8], F32)
            PSTART = sb("PSTART", [128, 8], F32)
            PEND = sb("PEND", [128, 8], F32)
            TMPA = sb("TMPA", [128, NTL, 8], F32)
            SLOTF = sb("SLOTF", [128, NTL, 2], F32)
            RTI = sb("RTI", [128, NTL * 4 + NBLK * 32], I32)
            CMPB = sb("CMPB", [128, NBLK, 8], F32)
            EB = sb("EB", [128, NBLK], F32)
            OFFU = sb("OFFU", [128, NBLK, 32], F32)
            O0 = pst("O0", [128, 512], F32)
            O1 = pst("O1", [128, 512], F32)
            TR0 = pst("TR0", [128, 512], F32)
            TR1 = pst("TR1", [128, 512], F32)
            RKP = pst("RKP", [128, 512], F32)
            OP = [O0, O1]
            TRp = [TR0, TR1]
            p.dma("sp", IDF[:], ident_d, writes=["IDF"], lane="IDF")
            p.dma("sp", LNP[:], ln_rep[l].rearrange("a p d -> p a d"), writes=["LNP"], lane="LNP")
            p.dma("sp", RW[:], router_w.rearrange("(c p) e -> p c e", p=128), writes=["RW"], lane="RW")
            p.dma("sp", UT32[:], ut_d, writes=["UT32"], lane="UT32")
            p.dma("sp", KTH[:], kth_d, writes=["KTH"], lane="KTH")
            p.dma("sp", BSTART[:], bstart_d, writes=["BSTART"], lane="BSTART")
            p.dma("sp", SU[:], su_d, writes=["SU"], lane="SU")
            p.dma("sp", CU[:], cu_d, writes=["CU"], lane="CU")
            p.op("dve", CP(UTB[:], UT32[:]), reads=["UT32"], writes=["UTB"])
            p.op("dve", MSET(ONB[:], 1.0), writes=["ONB"])
            p.op("dve", MSET(EPSL[:], LN_EPS), writes=["EPSL"])
            p.op("dve", MSET(CARRY[:], 0.0), writes=["CARRY"])
            for kc in range(8):
                p.dma("sp", WO32[:], w_out[l, kc * 128:(kc + 1) * 128, :], writes=["WO32"], lane="WO32")
                cast_op(p, kc, WOUT[:, kc, :], WO32[:], reads=["WO32"], writes=[("WOUT", kc)])
            WOK = [("WOUT", kc) for kc in range(8)]
            oi = [0]
            ri = [0]
            def stL(i):
                s = i % 4
                p.dma("sp", CTt[:, s], CATv[:, :, i * 128:(i + 1) * 128], writes=[("CTt", s)], lane=("CTt", s))
                p.dma("sp", Xt[:, s, :], xv[i], writes=[("Xt", s)], lane=("Xt", s))

            def stA(i):
                s = i % 4
                for half in range(2):
                    o = oi[0] % 2
                    oi[0] += 1
                    fns = [MM(OP[o][:], CTt[:, s, kc, :], WOUT[:, kc, half * 512:(half + 1) * 512], start=(kc == 0), stop=(kc == 7)) for kc in range(8)]
                    p.op("pe", fns, reads=[("CTt", s)] + WOK, writes=[("OP", o)])
                    p.op("dve", STT(Yt[:, s, half * 512:(half + 1) * 512], Xt[:, s, half * 512:(half + 1) * 512], ALPHA, OP[o][:], ALU.mult, ALU.add),
                         reads=[("Xt", s)], writes=[("OP", o), ("Yt", s)])

            def stB1(i):
                s = i % 4
                ln_ops(Yt[:, s, :], ("Yt", s), X1t[:, s, :], ("X1t", s), LNP, 0, 1, s, BST, MV, SDl, RSl, EPSL, XN, part=1)

            def stB(i):
                s = i % 4
                ln_ops(Yt[:, s, :], ("Yt", s), X1t[:, s, :], ("X1t", s), LNP, 0, 1, s, BST, MV, SDl, RSl, EPSL, XN, part=2)
                p.dma("sp", x1fv[i], X1t[:, s, :], reads=[("X1t", s)], lane=("X1tf", s))
                p.op("act", ACT(X1Bt[:, s, :], X1t[:, s, :], AF.Copy), reads=[("X1t", s)], writes=[("X1Bt", s)])
                p.dma("sp", x1bv[i], X1Bt[:, s, :], reads=[("X1Bt", s)], lane=("X1Bt", s))

            def stC(i):
                s = i % 4
                for q4 in range(2):
                    r = ri[0] % 2
                    ri[0] += 1
                    fns = [TR(TRp[r][:, k4 * 128:(k4 + 1) * 128], X1t[:, s, (q4 * 4 + k4) * 128:(q4 * 4 + k4 + 1) * 128], IDF[:]) for k4 in range(4)]
                    p.op("pe", fns, reads=[("X1t", s), "IDF"], writes=[("TRp", r)])
                    src = TRp[r][:].rearrange("p (k t) -> p k t", t=128)
                    p.op("dve", CP(X1TF[:, s, q4 * 4:(q4 + 1) * 4, :], src), writes=[("TRp", r), ("X1TF", s, q4)])
                o = oi[0] % 2
                oi[0] += 1
                fns = [MM(OP[o][:, 0:8], X1TF[:, s, kc, :], RW[:, kc, :], start=(kc == 0), stop=(kc == 7)) for kc in range(8)]
                p.op("pe", fns, reads=[("X1TF", s, 0), ("X1TF", s, 1), "RW"], writes=[("OP", o)])
                p.op("dve", CP(LGSa[:, s, :], OP[o][:, 0:8]), writes=[("OP", o), ("LGS", s)])

            def stD(i):
                s = i % 4
                LGS = LGSa[:, s, :]
                LG2 = LG2a[:, s, :]
                M1 = M1a_[:, s, :]
                MSK = MSKa[:, s, :]
                p.op("dve", RED(M1[:, 0:1], LGS, ALU.max), reads=[("LGS", s)], writes=[("M1a", s)])
                p.op("dve", TS(EQ1A[:, i, :], LGS, M1[:, 0:1], None, ALU.is_equal), reads=[("LGS", s), ("M1a", s)], writes=[("EQ1", i)])
                p.op("dve", STT(LG2, EQ1A[:, i, :], -1e30, LGS, ALU.mult, ALU.add), reads=[("EQ1", i), ("LGS", s)], writes=[("LG2", s)])
                p.op("dve", RED(M1[:, 1:2], LG2, ALU.max), reads=[("LG2", s)], writes=[("M1b", s)])
                p.op("dve", TS(EQ2A[:, i, :], LG2, M1[:, 1:2], None, ALU.is_equal), reads=[("LG2", s), ("M1b", s)], writes=[("EQ2", i)])
                p.op("dve", TT(M1[:, 2:3], M1[:, 1:2], M1[:, 0:1], ALU.subtract), reads=[("M1a", s), ("M1b", s)], writes=[("M1c", s)])
                p.op("act", ACT(M1[:, 3:4], M1[:, 2:3], AF.Exp, scale=1.0), reads=[("M1c", s)], writes=[("M1d", s)])

            def stD2(i):
                s = i % 4
                M1 = M1a_[:, s, :]
                MSK = MSKa[:, s, :]
                TQ = LG2a[:, s, 0:1]
                p.op("dve", TS(TQ, M1[:, 3:4], 1.0, None, ALU.add), reads=[("M1d", s), ("LG2", s)], writes=[("LG2", s)])
                p.op("dve", RCP(TQ, TQ), reads=[("LG2", s)], writes=[("LG2", s)])
                p.op("dve", TT(GA[:, i, 1:2], M1[:, 3:4], TQ, ALU.mult), reads=[("M1d", s), ("LG2", s)], writes=[("GA2", i)])
                p.op("dve", TS(GA[:, i, 0:1], GA[:, i, 1:2], -1.0, 1.0, ALU.mult, ALU.add), reads=[("GA2", i)], writes=[("GA1", i)])
                p.op("dve", TT(MSK, EQ1A[:, i, :], EQ2A[:, i, :], ALU.add), reads=[("EQ1", i), ("EQ2", i)], writes=[("MSK", s)])
                p.op("dve", CP(MSKB[:, s, :], MSK), reads=[("MSK", s)], writes=[("MSKB", s)])
                p.op("pe", [MM(RKP[:, 0:8], UTB[:], MSKB[:, s, :]), MM(RKP[:, 8:16], ONB[:], MSKB[:, s, :])], reads=["UTB", "ONB", ("MSKB", s)], writes=["RKP"])

            def stD3(i):
                p.op("dve", TT(RANKA[:, i, :], RKP[:, 0:8], CARRY[:], ALU.add), reads=["CARRY"], writes=["RKP", ("RANK", i)])
                p.op("dve", TT(CARRY[:], RKP[:, 8:16], CARRY[:], ALU.add), reads=["CARRY"], writes=["RKP", "CARRY"])

            rbank = {}
            stL(0)
            stL(1)
            for n in range(NTL + 6):
                if n + 2 < NTL:
                    stL(n + 2)
                if n < NTL:
                    stA(n)
                if 0 <= n - 1 < NTL:
                    stB1(n - 1)
                if 0 <= n - 2 < NTL:
                    stB(n - 2)
                if 0 <= n - 3 < NTL:
                    stC(n - 3)
                if 0 <= n - 4 < NTL:
                    stD(n - 4)
                if 0 <= n - 6 < NTL:
                    stD3(n - 6)
                if 0 <= n - 5 < NTL:
                    stD2(n - 5)
            AK = [("EQ1", i) for i in range(NTL)] + [("EQ2", i) for i in range(NTL)] + [("RANK", i) for i in range(NTL)]
            p.op("dve", TT(CMPK[:], CARRY[:].unsqueeze(2).to_broadcast([128, 8, 32]), KTH[:].unsqueeze(1).to_broadcast([128, 8, 32]), ALU.is_gt),
                 reads=["CARRY", "KTH"], writes=["CMPK"])
            p.op("dve", RED(NBK[:], CMPK[:], ALU.add), reads=["CMPK"], writes=["NBK"])
            p.op("dve", TS(PADDED[:], NBK[:], 512.0, None, ALU.mult), reads=["NBK"], writes=["PADDED"])
            p.op("dve", MSET(PSTART[:, 0:1], 0.0), writes=["PSTART"])
            for e_ in range(1, 8):
                p.op("dve", TT(PSTART[:, e_:e_ + 1], PSTART[:, e_ - 1:e_], PADDED[:, e_ - 1:e_], ALU.add), reads=["PSTART", "PADDED"], writes=["PSTART"])
            p.op("dve", TT(PEND[:], PSTART[:], PADDED[:], ALU.add), reads=["PSTART", "PADDED"], writes=["PEND"])
            p.op("dve", TT(RANKA[:], RANKA[:], PSTART[:].unsqueeze(1).to_broadcast([128, NTL, 8]), ALU.add), reads=AK + ["PSTART"], writes=["DEST"])
            p.op("dve", TT(TMPA[:], RANKA[:], EQ1A[:], ALU.mult), reads=["DEST"] + AK, writes=["TMPA"])
            p.op("dve", RED(SLOTF[:, :, 0], TMPA[:], ALU.add), reads=["TMPA"], writes=["SLOT0"])
            p.op("dve", TT(TMPA[:], RANKA[:], EQ2A[:], ALU.mult), reads=["DEST", "SLOT0"] + AK, writes=["TMPA"])
            p.op("dve", RED(SLOTF[:, :, 1], TMPA[:], ALU.add), reads=["TMPA"], writes=["SLOT1"])
            p.op("dve", CP(RTI[:, 0:NTL * 2], SLOTF[:].rearrange("p a b -> p (a b)")), reads=["SLOT0", "SLOT1"], writes=["RTIa"])
            GK = [("GA1", i) for i in range(NTL)] + [("GA2", i) for i in range(NTL)]
            p.op("dve", CP(RTI[:, NTL * 2:NTL * 4].bitcast(F32), GA[:].rearrange("p a b -> p (a b)")), reads=GK, writes=["RTIb"])
            p.op("dve", TT(CMPB[:], PEND[:].unsqueeze(1).to_broadcast([128, NBLK, 8]), BSTART[:].unsqueeze(2).to_broadcast([128, NBLK, 8]), ALU.is_le),
                 reads=["PEND", "BSTART"], writes=["CMPB"])
            p.op("dve", RED(EB[:], CMPB[:], ALU.add), reads=["CMPB"], writes=["EB"])
            p.op("dve", TS(EB[:], EB[:], 7.0, None, ALU.min), reads=["EB"], writes=["EB"])
            p.op("dve", TT(OFFU[:], EB[:].unsqueeze(2).to_broadcast([128, NBLK, 32]), SU[:].unsqueeze(1).to_broadcast([128, NBLK, 32]), ALU.mult),
                 reads=["EB", "SU"], writes=["OFFU"])
            p.op("dve", TT(OFFU[:], OFFU[:], CU[:].unsqueeze(1).to_broadcast([128, NBLK, 32]), ALU.add), reads=["OFFU", "CU"], writes=["OFFU"])
            p.op("dve", CP(RTI[:, NTL * 4:], OFFU[:].rearrange("p a b -> p (a b)")), reads=["OFFU"], writes=["RTIc"])
            p.dma("sp", RT, RTI[:], reads=["RTIa", "RTIb", "RTIc"], lane="RT")
            p.emit()

        with ExitStack() as es:
            sb = lambda n, *a: es.enter_context(nc.sbuf_tensor(_uniq(n), *a))
            RTI = sb("RTI", [128, NTL * 4 + NBLK * 32], I32)
            ZT = sb("ZT", [128, 8, D], BF16)
            XR = sb("XR", [128, 4, D], BF16)
            p.dma("sp", RTI[:], RT, writes=["RTI"], lane="RTI")
            p.op("dve", MSET(ZT[:], 0.0), writes=["ZT"])
            xbz = XB.rearrange("(a j p) d -> a p j d", p=128, j=8)
            for a in range(NSLOT // 1024):
                p.dma("sp", xbz[a], ZT[:], reads=["ZT"], writes=[("XBz", a)], lane=("XBz", a % 4))
            for i in range(NTL):
                s = i % 4
                p.dma("sp", XR[:, s, :], x1bv[i], writes=[("XR", s)], lane=("XR", s))
                for k in range(2):
                    idx = RTI[:, 2 * i + k: 2 * i + k + 1].bitcast(U32)
                    p.op("pool", lambda e, idx=idx, s=s: e.indirect_dma_start(out=XB, out_offset=bass.IndirectOffsetOnAxis(ap=idx, axis=0), in_=XR[:, s, :], in_offset=None),
                         reads=["RTI", ("XR", s)] + [("XBz", a) for a in range(NSLOT // 1024)], writes=[("XBs", i, k)], lane=("XRs", s, k))
            p.emit()

        with ExitStack() as es:
            sb = lambda n, *a: es.enter_context(nc.sbuf_tensor(_uniq(n), *a))
            pst = lambda n, *a: es.enter_context(nc.psum_tensor(_uniq(n), *a))
            RTI = sb("RTI", [128, NTL * 4 + NBLK * 32], I32)
            IDF = sb("IDF", [128, 128], F32)
            IDB = sb("IDB", [128, 128], BF16)
            XBt = sb("XBt", [128, 2, 4, D], BF16)
            XBT2 = sb("XBT", [128, 2, 8, 512], BF16)
            HT = sb("HT", [128, NFC, 512], BF16)
            WD = sb("WD", [128, NFC, D], BF16)
            WGUr = sb("WGUr", [128, 4, 2048], BF16)
            SGt = sb("SGt", [128, 2, 512], F32)
            YBt = sb("YBt", [128, 2, 4, D], F32)
            G0 = pst("G0", [128, 512], F32)
            G1 = pst("G1", [128, 512], F32)
            U0 = pst("U0", [128, 512], F32)
            U1 = pst("U1", [128, 512], F32)
            O0 = pst("O0", [128, 512], F32)
            O1 = pst("O1", [128, 512], F32)
            TR0 = pst("TR0", [128, 512], F32)
            TR1 = pst("TR1", [128, 512], F32)
            GP = [G0, G1]
            UPp = [U0, U1]
            OP = [O0, O1]
            TRp = [TR0, TR1]
            p.dma("sp", RTI[:], RT, writes=["RTI"], lane="RTI")
            p.dma("sp", IDF[:], ident_d, writes=["IDF"], lane="IDF")
            p.op("dve", CP(IDB[:], IDF[:]), reads=["IDF"], writes=["IDB"])
            xbv = XB.rearrange("(b j p) d -> b p j d", p=128, j=4)
            ybv = YB.rearrange("(b j p) d -> b p j d", p=128, j=4)
            regs = []
            OFF0 = NTL * 4
            gui = [0]
            oi = [0]
            ri = [0]
            gi2 = [0]
            evi = [0]

            WGUrows = WGU_M.rearrange("c p f -> (c p) f")
            WDrows = WD_M.rearrange("g p c d -> (g p) (c d)")

            def dyn_dma(out_ap, rows_ap, b, slot, reads, writes, lane):
                idx = RTI[:, OFF0 + b * 32 + slot: OFF0 + b * 32 + slot + 1].bitcast(U32)
                p.op("pool", lambda e: e.indirect_dma_start(out=out_ap, out_offset=None, in_=rows_ap, in_offset=bass.IndirectOffsetOnAxis(ap=idx, axis=0)),
                     reads=["RTI"] + list(reads), writes=writes, lane=lane)

            regs_alloc = []
            dyn_cnt = [0]
            def blk_load(b):
                s = b % 2
                p.dma("sp", XBt[:, s], xbv[b], writes=[("XBt", s)], lane=("XBt", s))

            def blk_tr(b):
                s = b % 2
                for kc in range(8):
                    r = ri[0] % 2
                    ri[0] += 1
                    fns = [MM(TRp[r][:, j * 128:(j + 1) * 128], XBt[:, s, j, kc * 128:(kc + 1) * 128], IDB[:]) for j in range(4)]
                    p.op("pe", fns, reads=[("XBt", s), "IDB"], writes=[("TRp", r)])
                    evi[0] += 1
                    if evi[0] % 2:
                        p.op("act", ACT(XBT2[:, s, kc, :], TRp[r][:], AF.Copy), writes=[("TRp", r), ("XBT", s, kc)])
                    else:
                        p.op("dve", CP(XBT2[:, s, kc, :], TRp[r][:]), writes=[("TRp", r), ("XBT", s, kc)])

            blk_load(0)
            blk_tr(0)
            for b in range(NBLK):
                s = b % 2
                XBT = XBT2[:, s]
                if b + 1 < NBLK:
                    blk_load(b + 1)
                XBTK = [("XBT", s, kc) for kc in range(8)]
                for fc in range(NFC):
                    r4 = gui[0] % 4
                    gui[0] += 1
                    dyn_dma(WGUr[:, r4, :], WGUrows, b, fc, [], [("WGUr", r4)], ("WGUr", r4))
                    if fc % 7 == 1:
                        g7 = fc // 7
                        dyn_dma(WD[:, g7 * 7:(g7 + 1) * 7, :].rearrange("p c d -> p (c d)"), WDrows, b, 28 + g7, [], [("WD", g7)], ("WD", g7))
                    wv = WGUr[:, r4, :].rearrange("p (w k j) -> p w k j", w=2, k=8)
                    th = gi2[0] % 2
                    gi2[0] += 1
                    gp, up = GP[th], UPp[th]
                    fns = [MM(gp[:], wv[:, 0, kc, :], XBT[:, kc, :], start=(kc == 0), stop=(kc == 7)) for kc in range(8)]
                    fns += [MM(up[:], wv[:, 1, kc, :], XBT[:, kc, :], start=(kc == 0), stop=(kc == 7)) for kc in range(8)]
                    p.op("pe", fns, reads=[("WGUr", r4)] + XBTK, writes=[("GP", th), ("UP", th)])
                    p.op("act", ACT(SGt[:, th, :], gp[:], AF.Silu), writes=[("GP", th), ("SGt", th)])
                    p.op("dve", TT(HT[:, fc, :], SGt[:, th, :], up[:], ALU.mult), reads=[("SGt", th)], writes=[("UP", th), ("HT", fc)])
                HTK = [("HT", fc) for fc in range(NFC)]
                WDK = [("WD", g7) for g7 in range(4)]
                if b + 1 < NBLK:
                    blk_tr(b + 1)
                for j in range(4):
                    for half in range(2):
                        o = oi[0] % 2
                        oi[0] += 1
                        fns = [MM(OP[o][:], HT[:, fc, j * 128:(j + 1) * 128], WD[:, fc, half * 512:(half + 1) * 512], start=(fc == 0), stop=(fc == NFC - 1)) for fc in range(NFC)]
                        p.op("pe", fns, reads=HTK + WDK, writes=[("OP", o)])
                        dst = YBt[:, s, j, half * 512:(half + 1) * 512]
                        if o == 0:
                            p.op("act", ACT(dst, OP[o][:], AF.Copy), writes=[("OP", o), ("YBt", s, j, half)])
                        else:
                            p.op("dve", CP(dst, OP[o][:]), writes=[("OP", o), ("YBt", s, j, half)])
                p.dma("sp", ybv[b], YBt[:, s], reads=[("YBt", s, j, h2) for j in range(4) for h2 in range(2)], lane=("YBt", s))
            p.emit()

        with ExitStack() as es:
            sb = lambda n, *a: es.enter_context(nc.sbuf_tensor(_uniq(n), *a))
            RTI = sb("RTI", [128, NTL * 4 + NBLK * 32], I32)
            LNP = sb("LNP", [128, 4, D], F32)
            Y12 = sb("Y12", [128, 4, 2, D], F32)
            X1t = sb("X1t", [128, 4, D], F32)
            ACCt = sb("ACCt", [128, 4, D], F32)
            XN = sb("XN", [128, 4, D], F32)
            OUTt = sb("OUTt", [128, 4, D], F32)
            BST = sb("BST", [128, 4, 12], F32)
            MV = sb("MV", [128, 4, 2], F32)
            SDl = sb("SDl", [128, 4], F32)
            RSl = sb("RSl", [128, 4], F32)
            EPSL = sb("EPSL", [128, 1], F32)
            p.dma("sp", RTI[:], RT, writes=["RTI"], lane="RTI")
            p.dma("sp", LNP[:], ln_rep[l].rearrange("a p d -> p a d"), writes=["LNP"], lane="LNP")
            p.op("dve", MSET(EPSL[:], LN_EPS), writes=["EPSL"])
            GAf = RTI[:, NTL * 2:NTL * 4].bitcast(F32)
            def e4_load(i):
                s = i % 4
                p.dma("sp", X1t[:, s, :], x1fv[i], writes=[("X1t", s)], lane=("X1t", s))
                for k in range(2):
                    idx = RTI[:, 2 * i + k: 2 * i + k + 1].bitcast(U32)
                    p.op("pool", lambda e, idx=idx, s=s, k=k: e.indirect_dma_start(out=Y12[:, s, k, :], out_offset=None, in_=YB, in_offset=bass.IndirectOffsetOnAxis(ap=idx, axis=0)),
                         reads=["RTI"], writes=[("Y12", s, k)], lane=("Y12", s, k))

            for n in range(NTL + 2):
                if n < NTL:
                    e4_load(n)
                i = n - 2
                if i < 0:
                    continue
                s = i % 4
                p.op("act", ACT(ACCt[:, s, :], X1t[:, s, :], AF.Copy, scale=ALPHA), reads=[("X1t", s)], writes=[("ACCt", s)])
                for k in range(2):
                    p.op("dve", STT(ACCt[:, s, :], Y12[:, s, k, :], GAf[:, 2 * i + k: 2 * i + k + 1], ACCt[:, s, :], ALU.mult, ALU.add),
                         reads=[("Y12", s, k), "RTI", ("ACCt", s)], writes=[("ACCt", s)])
                ln_ops(ACCt[:, s, :], ("ACCt", s), OUTt[:, s, :], ("OUTt", s), LNP, 2, 3, s, BST, MV, SDl, RSl, EPSL, XN)
                p.dma("sp", ov[i], OUTt[:, s, :], reads=[("OUTt", s)], lane=("OUTt", s))
            p.emit()

    conv_split = [None, None]
    if "0" in PHASES:
        if OVERLAP_CONV and "B" in PHASES:
            units, n_dense = make_conv_units()
            if LAYERS == "01":
                n0 = n_dense + (len(units) - n_dense) // 2
                conv_split = [units[:n0], units[n0:]]
            else:
                conv_split = [units, units]
        else:
            phase_convert()
    for l in range(DEPTH):
        if str(l) not in LAYERS:
            continue
        xin = x_in if l == 0 else X1S
        xout = X1S if l == 0 else out_d
        if "A" in PHASES:
            phase_A(l, xin)
        if "B" in PHASES:
            phase_B(l, conv_split[l])
        if "C" in PHASES:
            phase_C(l)
        if "D" in PHASES:
            if l % 2 == 1 and SPARSE:
                phase_E(l, xin, xout)
            else:
                phase_D(l, xin, xout, moe=(l % 2 == 1))
    p.close()
    return nc


def _t5_bucket(dist):
    n = np.maximum(dist, 0)
    max_exact = 16
    nf = np.maximum(n, 1).astype(np.float32)
    large = max_exact + (np.log(nf / max_exact) / math.log(128 / max_exact) * (32 - max_exact)).astype(np.int32)
    large = np.minimum(large, 31)
    return np.where(n < max_exact, n, large)


def host_constants(S):
    c = {}
    c["ident"] = np.eye(128, dtype=np.float32)
    half = 32
    inv = (10000.0 ** (-np.linspace(0.0, 1.0, half, dtype=np.float32))).astype(np.float32)
    pos = np.arange(S, dtype=np.float32)
    ang = pos[None, :] * inv[:, None]
    fidx = (np.arange(128) % 64) % 32
    c["cos_t"] = np.cos(ang)[fidx].astype(np.float32)
    c["sin_t"] = np.sin(ang)[fidx].astype(np.float32)
    kk = np.arange(128)[:, None]
    qq = np.arange(128)[None, :]
    c["maskneg"] = np.where(qq >= kk, 0.0, NEG).astype(np.float32)
    H = 4
    log_gamma = np.log(1.0 - 2.0 ** (-5.0 - np.arange(H, dtype=np.float64)))
    idx = np.arange(128, dtype=np.float64)
    rel = idx[None, :] - idx[:, None]
    maskt = np.zeros((128, 4, 128), np.float64)
    for h in range(H):
        maskt[:, h, :] = np.where(rel >= 0, np.exp(log_gamma[h] * np.maximum(rel, 0.0)), 0.0) * 0.125
    c["maskt"] = maskt.astype(np.float32)
    qdec = np.zeros((128, 2, 512), np.float64)
    cd = np.zeros((128, 2), np.float64)
    for cpair in range(2):
        for hh in range(2):
            h = 2 * cpair + hh
            qd = np.exp(log_gamma[h] * (idx + 1.0))
            qdec[64 * hh:64 * hh + 64, cpair, :] = np.tile(qd, 4)[None, :]
            cd[64 * hh:64 * hh + 64, cpair] = np.exp(log_gamma[h] * 128.0)
    c["qdec"] = qdec.astype(np.float32)
    c["cd"] = cd.astype(np.float32)
    kdec = np.zeros((128, 4, 64), np.float64)
    for h in range(H):
        kdec[:, h, :] = (np.exp(log_gamma[h] * (127.0 - idx)) * 0.125)[:, None]
    c["kdec"] = kdec.astype(np.float32)
    bd = np.zeros((128, 128), np.float32)
    bd[0:64, 0:64] = 1.0
    bd[64:128, 64:128] = 1.0
    c["bdmask"] = bd
    invc = np.zeros((128, 2, 2, 512), np.float32)
    wins = (2, 4, 8, 16)
    t = np.arange(512)
    for g, w in enumerate(wins):
        cch, hh = g // 2, g % 2
        invc[64 * hh:64 * hh + 64, 0, cch, :] = (1.0 / np.minimum(t + 1, w).astype(np.float32))[None, :]
        invc[64 * hh:64 * hh + 64, 1, cch, :] = np.float32(1.0 / w)
    c["invc"] = invc
    tp = np.arange(128)
    c["ut"] = (tp[:, None] < tp[None, :]).astype(np.float32)
    c["kth"] = np.broadcast_to((512.0 * np.arange(32, dtype=np.float32))[None, :], (128, 32)).copy()
    nblk = (2 * S + N_EXPERTS * 512) // 512
    c["bstart"] = np.broadcast_to((512.0 * np.arange(nblk, dtype=np.float32))[None, :], (128, nblk)).copy()
    su = np.zeros(32, np.float32)
    cu = np.zeros((128, 32), np.float32)
    su[0:28] = 28.0 * 128.0
    cu[:, 0:28] = 128.0 * np.arange(28)[None, :] + np.arange(128)[:, None]
    su[28:32] = 4.0 * 128.0
    cu[:, 28:32] = 128.0 * np.arange(4)[None, :] + np.arange(128)[:, None]
    c["su"] = np.broadcast_to(su[None, :], (128, 32)).copy()
    c["cu"] = cu
    return c


def host_layout(inputs, S):
    f = lambda a: np.ascontiguousarray(np.asarray(a, dtype=np.float32))
    sh = {}
    for k in ("w_in", "w_out", "ffn_w_gate", "ffn_w_up", "ffn_w_down", "pool_w"):
        sh[k] = f(inputs[k])
    sh["router_w"] = f(inputs["router_w"][0])
    sh["moe_w_gate"] = f(inputs["moe_w_gate"][0])
    sh["moe_w_up"] = f(inputs["moe_w_up"][0])
    sh["moe_w_down"] = f(inputs["moe_w_down"][0])
    lam = f(inputs["diff_lambda"]).reshape(DEPTH, 1, 256)
    sh["lam_rep"] = f(np.broadcast_to(lam, (DEPTH, 128, 256)))
    sh["subln_rep"] = f(np.broadcast_to(f(inputs["diff_subln_g"])[:, None, :], (DEPTH, 128, 128)))
    sh["pscale"] = f(f(inputs["pool_scale"]).reshape(DEPTH, 2, 128).transpose(0, 2, 1))
    sh["gn_rep"] = f(np.broadcast_to(f(inputs["ret_gn_g"])[:, None, :], (DEPTH, 128, 256)))
    ln = np.stack([f(inputs["ln1_g"]), f(inputs["ln1_b"]), f(inputs["ln2_g"]), f(inputs["ln2_b"])], axis=1)
    sh["ln_rep"] = f(np.broadcast_to(ln[:, :, None, :], (DEPTH, 4, 128, D)))
    rb = f(inputs["rel_bias"])
    kk = np.arange(128)[:, None]
    qq = np.arange(128)[None, :]
    b0 = _t5_bucket(qq - kk)
    b1 = _t5_bucket(128 + qq - kk)
    bt = np.stack([rb[b0], rb[b1]], axis=1)
    sh["bt"] = f(bt.transpose(0, 1, 3, 2))
    sh["cb"] = f(np.broadcast_to(rb[31][None, :], (128, 8)))
    sh.update(host_constants(S))
    if "0" not in PHASES:
        for k in ("ffn_w_gate", "ffn_w_up", "ffn_w_down", "moe_w_gate", "moe_w_up", "moe_w_down"):
            sh.pop(k)
    return sh


_CACHE = {}


def run(inputs, S, dev=False):
    B = inputs["x"].shape[0]
    key = (S, dev)
    if key not in _CACHE:
        _CACHE[key] = build(S, dev)
    nc = _CACHE[key]
    shared = host_layout(inputs, S)
    x = np.asarray(inputs["x"], dtype=np.float32)
    in_maps = []
    for b in range(B):
        m = dict(shared)
        m["x"] = np.ascontiguousarray(x[b])
        in_maps.append(m)
    res = run_bass_kernel_spmd(nc, in_maps, core_ids=list(range(B)))
    return res


def kernel(**inputs):
    S = inputs["x"].shape[1]
    res = run(inputs, S)
    out = np.stack([np.asarray(r["out"], dtype=np.float32) for r in res.results], axis=0)
    return out
```
